# Optimizing a Trainium2 kernel written in Bass

```python
import math
import numpy as np
import jax
import jax.numpy as jnp
from jax import lax

D_MODEL = 2048
BATCH = 4
SEQ = 2048
DEPTH = 4

CTX_LEN = 256
GRID_W = 64
Q_BLOCK = 128
EPS = 1e-6
ROPE_BASE = 10000.0

GQA_HEADS = 4
GQA_KV_HEADS = 2
GQA_HEAD_DIM = 128
MLA_HEADS = 4
MLA_Q_LORA = 384
MLA_KV_LORA = 256
MLA_NOPE = 64
MLA_ROPE = 32
MLA_V = 128
HY_WIDTH = 512
HY_ORDER = 2
HY_SHORT = 3
HY_BANDS = 16
HY_EMB = 2 * HY_BANDS + 1
HY_FILTER_HIDDEN = 64
HY_FAST_RATE = 15.35
HY_SLOW_RATE = 3.07
CV_WIDTH = 512
CV_KERNEL = 31
N_BRANCH = 4
BRANCH_WIDTH = 512
N_EXPERTS = 16
EXPERT_FF = 1024
CAPACITY_FACTOR = 2

GQA_KV_COLS = 2 * GQA_KV_HEADS * GQA_HEAD_DIM
MLA_KV_COLS = MLA_KV_LORA + MLA_ROPE
KV_COLS = GQA_KV_COLS + MLA_KV_COLS
Q_COLS = GQA_HEADS * GQA_HEAD_DIM + MLA_Q_LORA
HY_COLS = (HY_ORDER + 1) * HY_WIDTH
CV_COLS = 2 * CV_WIDTH
GATE_COLS = N_BRANCH * D_MODEL
IN_COLS = KV_COLS + Q_COLS + HY_COLS + CV_COLS + GATE_COLS

kernel_name = 'hybrid_hyena_gqa_mla_conformer_ec_moe_diffusion'


def rmsnorm(x, g):
    xf = x.astype(jnp.float32)
    y = xf * lax.rsqrt(jnp.mean(xf * xf, axis=-1, keepdims=True) + EPS)
    return (y * g.astype(jnp.float32)).astype(x.dtype)


def layernorm(x, g, b):
    xf = x.astype(jnp.float32)
    mu = jnp.mean(xf, axis=-1, keepdims=True)
    xc = xf - mu
    var = jnp.mean(xc * xc, axis=-1, keepdims=True)
    return (xc * lax.rsqrt(var + EPS) * g.astype(jnp.float32) + b.astype(jnp.float32)).astype(x.dtype)


def modulate(h, shift, scale):
    return h * (1.0 + scale) + shift


def split_cols(p, widths):
    return jnp.split(p, [int(o) for o in np.cumsum(widths)[:-1]], axis=-1)


def dwconv(x, w, b):
    C = x.shape[-1]
    y = lax.conv_general_dilated(x, w[:, None, :].astype(x.dtype), window_strides=(1,), padding='SAME',
                                 dimension_numbers=('NWC', 'WIO', 'NWC'), feature_group_count=C)
    return y + b.astype(x.dtype)


def _rotate(part, pos):
    m = part.shape[-1]
    inv = ROPE_BASE ** (-jnp.arange(0, m, 2, dtype=jnp.float32) / m)
    ang = pos.astype(jnp.float32)[:, None] * inv[None, :]
    cos = jnp.cos(ang)[None, :, None, :]
    sin = jnp.sin(ang)[None, :, None, :]
    pf = part.astype(jnp.float32)
    a, b = pf[..., : m // 2], pf[..., m // 2:]
    return jnp.concatenate([a * cos - b * sin, b * cos + a * sin], axis=-1).astype(part.dtype)


def axial_rope(x, pos):
    rows, cols = pos
    half = x.shape[-1] // 2
    return jnp.concatenate([_rotate(x[..., :half], rows), _rotate(x[..., half:], cols)], axis=-1)


def attend(q, k, v):
    B, Sq, Hq, dk = q.shape
    Hk, dv = k.shape[2], v.shape[-1]
    R = Hq // Hk
    nb = Sq // Q_BLOCK
    scale = dk ** -0.5
    qb = q.reshape(B, nb, Q_BLOCK, Hk, R, dk).transpose(1, 0, 2, 3, 4, 5)

    def block(qi):
        s = jnp.einsum('bqgrd,bkgd->bgrqk', qi, k, preferred_element_type=jnp.float32) * scale
        p = jax.nn.softmax(s, axis=-1).astype(v.dtype)
        return jnp.einsum('bgrqk,bkgd->bqgrd', p, v)

    o = lax.map(block, qb)
    return o.transpose(1, 0, 2, 3, 4, 5).reshape(B, Sq, Hq, dv)


def attn_keys(pkv, lp, pos):
    B, L, _ = pkv.shape
    k, v, kv_a, k_pe = split_cols(pkv, [GQA_KV_HEADS * GQA_HEAD_DIM, GQA_KV_HEADS * GQA_HEAD_DIM,
                                        MLA_KV_LORA, MLA_ROPE])
    k = rmsnorm(k.reshape(B, L, GQA_KV_HEADS, GQA_HEAD_DIM), lp['gqa_k_gain'])
    v = v.reshape(B, L, GQA_KV_HEADS, GQA_HEAD_DIM)
    kv = (rmsnorm(kv_a, lp['mla_kv_a_gain']) @ lp['mla_kv_b']).reshape(B, L, MLA_HEADS, MLA_NOPE + MLA_V)
    k_nope, v_mla = kv[..., :MLA_NOPE], kv[..., MLA_NOPE:]
    k_pe = k_pe.reshape(B, L, 1, MLA_ROPE)
    if pos is not None:
        k = axial_rope(k, pos)
        k_pe = axial_rope(k_pe, pos)
    k_mla = jnp.concatenate([k_nope, jnp.broadcast_to(k_pe, (B, L, MLA_HEADS, MLA_ROPE))], axis=-1)
    return k, v, k_mla, v_mla


def hyena_filters(L, lp):
    pos = jnp.arange(L, dtype=jnp.float32)
    t01 = pos / (L - 1)
    bands = jnp.linspace(1e-4, HY_BANDS - 1, HY_BANDS, dtype=jnp.float32)
    ang = (2.0 * math.pi / L) * pos[:, None] * bands[None, :]
    feats = jnp.concatenate([t01[:, None], jnp.cos(ang), -jnp.sin(ang)], axis=-1)
    freq = lp['hf_freq'].astype(jnp.float32)
    h = jnp.sin(freq * (feats @ lp['hf_w1'].astype(jnp.float32) + lp['hf_b1'].astype(jnp.float32)))
    h = jnp.sin(freq * (h @ lp['hf_w2'].astype(jnp.float32) + lp['hf_b2'].astype(jnp.float32)))
    h = h @ lp['hf_w3'].astype(jnp.float32)
    h = h * jnp.exp(-t01[:, None] * jnp.exp(lp['hf_log_rate'].astype(jnp.float32))[None, :])
    h = h.reshape(L, HY_ORDER, 2, HY_WIDTH)
    fwd, bwd = h[:, :, 0], h[:, :, 1]
    zero = jnp.zeros((1, HY_ORDER, HY_WIDTH), jnp.float32)
    return jnp.concatenate([fwd, zero, bwd[:0:-1]], axis=0)


def hyena_mixer(p, lp):
    B, L, _ = p.shape
    u = dwconv(p, lp['hy_conv_w'], lp['hy_conv_b'])
    v, *gates = jnp.split(u, HY_ORDER + 1, axis=-1)
    kf = jnp.fft.rfft(hyena_filters(L, lp), axis=0)
    z = v
    for n, gate in enumerate(gates):
        zf = z.astype(jnp.float32)
        conv = jnp.fft.irfft(jnp.fft.rfft(zf, n=2 * L, axis=1) * kf[None, :, n], n=2 * L, axis=1)[:, :L]
        z = gate * (conv + lp['hy_bias'][n].astype(jnp.float32) * zf).astype(p.dtype)
    return z


def conformer_conv(p, lp):
    a, b = jnp.split(p, 2, axis=-1)
    u = dwconv(a * jax.nn.sigmoid(b), lp['cv_w'], lp['cv_b'])
    return jax.nn.silu(layernorm(u, lp['cv_ln_g'], lp['cv_ln_b']))


def token_mixers(p, keys, pos, lp):
    B, L, _ = p.shape
    pq, phy, pcv, pg = split_cols(p, [Q_COLS, HY_COLS, CV_COLS, GATE_COLS])
    q, q_a = split_cols(pq, [GQA_HEADS * GQA_HEAD_DIM, MLA_Q_LORA])
    q = rmsnorm(q.reshape(B, L, GQA_HEADS, GQA_HEAD_DIM), lp['gqa_q_gain'])
    qm = (rmsnorm(q_a, lp['mla_q_a_gain']) @ lp['mla_q_b']).reshape(B, L, MLA_HEADS, MLA_NOPE + MLA_ROPE)
    q_nope, q_pe = qm[..., :MLA_NOPE], qm[..., MLA_NOPE:]
    if pos is not None:
        q = axial_rope(q, pos)
        q_pe = axial_rope(q_pe, pos)
    qm = jnp.concatenate([q_nope, q_pe], axis=-1)
    k, v, k_mla, v_mla = keys
    o_gqa = attend(q, k, v).reshape(B, L, BRANCH_WIDTH)
    o_mla = attend(qm, k_mla, v_mla).reshape(B, L, BRANCH_WIDTH)
    o_hy = hyena_mixer(phy, lp)
    o_cv = conformer_conv(pcv, lp)
    branches = jnp.stack([o_hy, o_gqa, o_mla, o_cv], axis=2)
    up = jnp.einsum('bsnw,nwd->bsnd', branches, lp['w_br'])
    gates = jax.nn.sigmoid(pg.reshape(B, L, N_BRANCH, -1))
    return jnp.einsum('bsnd,bsnd->bsd', gates, up) @ lp['w_out']


def expert_choice(h, lp):
    B, n, _ = h.shape
    cap = CAPACITY_FACTOR * n // N_EXPERTS
    aff = jax.nn.softmax(jnp.einsum('bnd,de->bne', h, lp['w_router'], preferred_element_type=jnp.float32), axis=-1)
    g, idx = lax.top_k(aff.transpose(0, 2, 1), cap)
    bidx = jnp.arange(B)[:, None, None]
    xg = h[bidx, idx]
    hid = jax.nn.silu(jnp.einsum('becd,edf->becf', xg, lp['w1'])) * jnp.einsum('becd,edf->becf', xg, lp['w3'])
    y = jnp.einsum('becf,efd->becd', hid, lp['w2']) * g[..., None].astype(h.dtype)
    return jnp.zeros_like(h).at[bidx, idx].add(y)


def setup_inputs(seed: int = 0) -> dict:
    key = jax.random.key(seed)
    ks = iter(jax.random.split(key, 40))
    D = D_MODEL

    def nrm(shape, scale):
        return jax.random.normal(next(ks), shape, jnp.float32) * scale

    def gain(shape):
        return 1.0 + nrm(shape, 0.02)

    rate0 = jnp.log(jnp.tile(jnp.linspace(HY_SLOW_RATE, HY_FAST_RATE, HY_WIDTH, dtype=jnp.float32), HY_ORDER * 2))
    return {
        'x': nrm((BATCH, SEQ, D), 1.0),
        'c': nrm((BATCH, D), 1.0),
        'ctx': nrm((BATCH, CTX_LEN, D), 1.0),
        'c_ctx': nrm((D,), 1.0),
        'w_mod': nrm((DEPTH, D, 6 * D), 0.5 * D ** -0.5),
        'b_mod': nrm((DEPTH, 6 * D), 0.01),
        'g_mix': gain((DEPTH, D)),
        'w_in': nrm((DEPTH, D, IN_COLS), D ** -0.5),
        'gqa_q_gain': gain((DEPTH, GQA_HEAD_DIM)),
        'gqa_k_gain': gain((DEPTH, GQA_HEAD_DIM)),
        'mla_q_a_gain': gain((DEPTH, MLA_Q_LORA)),
        'mla_q_b': nrm((DEPTH, MLA_Q_LORA, MLA_HEADS * (MLA_NOPE + MLA_ROPE)), MLA_Q_LORA ** -0.5),
        'mla_kv_a_gain': gain((DEPTH, MLA_KV_LORA)),
        'mla_kv_b': nrm((DEPTH, MLA_KV_LORA, MLA_HEADS * (MLA_NOPE + MLA_V)), MLA_KV_LORA ** -0.5),
        'hy_conv_w': nrm((DEPTH, HY_SHORT, HY_COLS), HY_SHORT ** -0.5),
        'hy_conv_b': nrm((DEPTH, HY_COLS), 0.01),
        'hf_w1': nrm((DEPTH, HY_EMB, HY_FILTER_HIDDEN), HY_EMB ** -0.5),
        'hf_b1': nrm((DEPTH, HY_FILTER_HIDDEN), 0.1),
        'hf_w2': nrm((DEPTH, HY_FILTER_HIDDEN, HY_FILTER_HIDDEN), HY_FILTER_HIDDEN ** -0.5),
        'hf_b2': nrm((DEPTH, HY_FILTER_HIDDEN), 0.1),
        'hf_w3': nrm((DEPTH, HY_FILTER_HIDDEN, HY_ORDER * 2 * HY_WIDTH), 0.05 * HY_FILTER_HIDDEN ** -0.5),
        'hf_freq': 1.0 + nrm((DEPTH, HY_FILTER_HIDDEN), 0.1),
        'hf_log_rate': rate0[None, :] + nrm((DEPTH, HY_ORDER * 2 * HY_WIDTH), 0.05),
        'hy_bias': nrm((DEPTH, HY_ORDER, HY_WIDTH), 0.1),
        'cv_w': nrm((DEPTH, CV_KERNEL, CV_WIDTH), CV_KERNEL ** -0.5),
        'cv_b': nrm((DEPTH, CV_WIDTH), 0.01),
        'cv_ln_g': gain((DEPTH, CV_WIDTH)),
        'cv_ln_b': nrm((DEPTH, CV_WIDTH), 0.01),
        'w_br': nrm((DEPTH, N_BRANCH, BRANCH_WIDTH, D), BRANCH_WIDTH ** -0.5),
        'w_out': nrm((DEPTH, D, D), D ** -0.5),
        'g_ffn': gain((DEPTH, D)),
        'w_router': nrm((DEPTH, D, N_EXPERTS), D ** -0.5),
        'w1': nrm((DEPTH, N_EXPERTS, D, EXPERT_FF), D ** -0.5),
        'w3': nrm((DEPTH, N_EXPERTS, D, EXPERT_FF), D ** -0.5),
        'w2': nrm((DEPTH, N_EXPERTS, EXPERT_FF, D), EXPERT_FF ** -0.5),
        'g_final': gain((D,)),
    }


def reference(x, c, ctx, c_ctx, w_mod, b_mod, g_mix, w_in, gqa_q_gain, gqa_k_gain, mla_q_a_gain, mla_q_b,
              mla_kv_a_gain, mla_kv_b, hy_conv_w, hy_conv_b, hf_w1, hf_b1, hf_w2, hf_b2, hf_w3, hf_freq,
              hf_log_rate, hy_bias, cv_w, cv_b, cv_ln_g, cv_ln_b, w_br, w_out, g_ffn, w_router, w1, w3, w2,
              g_final):
    S = x.shape[1]
    ROWS = S // GRID_W
    rows = jnp.repeat(jnp.arange(ROWS, dtype=jnp.int32), GRID_W)
    cols = jnp.tile(jnp.arange(GRID_W, dtype=jnp.int32), ROWS)
    pos = (rows, cols)
    xc = ctx
    for i in range(DEPTH):
        last = i == DEPTH - 1
        lp = {
            'gqa_q_gain': gqa_q_gain[i], 'gqa_k_gain': gqa_k_gain[i],
            'mla_q_a_gain': mla_q_a_gain[i], 'mla_q_b': mla_q_b[i],
            'mla_kv_a_gain': mla_kv_a_gain[i], 'mla_kv_b': mla_kv_b[i],
            'hy_conv_w': hy_conv_w[i], 'hy_conv_b': hy_conv_b[i],
            'hf_w1': hf_w1[i], 'hf_b1': hf_b1[i], 'hf_w2': hf_w2[i], 'hf_b2': hf_b2[i], 'hf_w3': hf_w3[i],
            'hf_freq': hf_freq[i], 'hf_log_rate': hf_log_rate[i], 'hy_bias': hy_bias[i],
            'cv_w': cv_w[i], 'cv_b': cv_b[i], 'cv_ln_g': cv_ln_g[i], 'cv_ln_b': cv_ln_b[i],
            'w_br': w_br[i], 'w_out': w_out[i],
            'w_router': w_router[i], 'w1': w1[i], 'w3': w3[i], 'w2': w2[i],
        }
        mod_x = (jax.nn.silu(c) @ w_mod[i] + b_mod[i])[:, None, :]
        mod_c = (jax.nn.silu(c_ctx) @ w_mod[i] + b_mod[i])[None, None, :]
        sh1, sc1, g1, sh2, sc2, g2 = jnp.split(mod_x, 6, axis=-1)
        csh1, csc1, cg1, csh2, csc2, cg2 = jnp.split(mod_c, 6, axis=-1)

        hx = modulate(rmsnorm(x, g_mix[i]), sh1, sc1)
        hc = modulate(rmsnorm(xc, g_mix[i]), csh1, csc1)
        pc = hc @ (w_in[i][:, :KV_COLS] if last else w_in[i])
        kc = attn_keys(pc[..., :KV_COLS], lp, None)
        px = hx @ w_in[i]
        kx = attn_keys(px[..., :KV_COLS], lp, pos)
        keys_x = tuple(jnp.concatenate([a, b], axis=1) for a, b in zip(kx, kc))

        x = x + g1 * token_mixers(px[..., KV_COLS:], keys_x, pos, lp)
        x = x + g2 * expert_choice(modulate(rmsnorm(x, g_ffn[i]), sh2, sc2), lp)
        if not last:
            xc = xc + cg1 * token_mixers(pc[..., KV_COLS:], kc, None, lp)
            xc = xc + cg2 * expert_choice(modulate(rmsnorm(xc, g_ffn[i]), csh2, csc2), lp)
    return rmsnorm(x, g_final)
```

```python
import contextlib
import math
import numpy as np
import concourse.bass as bass
import concourse.mybir as mybir
from concourse.bass_utils import run_bass_kernel_spmd

F32 = mybir.dt.float32
BF16 = mybir.dt.bfloat16
I32 = mybir.dt.int32
U32 = mybir.dt.uint32
AF = mybir.ActivationFunctionType
ALU = mybir.AluOpType
AX = mybir.AxisListType

D = 2048
SEQ = 2048
CTX = 256
NT = SEQ + CTX
NTILE = NT // 128
DEPTH = 4
KC = D // 128
IN_COLS = 12448
EPS = 1e-6
GRID_W = 64
NE = 16
FF = 1024
C_K, C_V, C_KVA, C_KPE, C_Q, C_QA, C_HY, C_CV, C_G = 0, 256, 512, 768, 800, 1312, 1696, 3232, 4256
TGROUPS = [(0, 512), (512, 512), (1024, 512), (1536, 512), (2048, 256)]


class Buf:
    __slots__ = ("name", "w", "r")

    def __init__(self, name):
        self.name = name
        self.w = None
        self.r = {}


class KB:
    ND = 8

    def __init__(self, nc, same_sync=True):
        self.nc = nc
        self.es = contextlib.ExitStack()
        self.eng = {"pe": nc.tensor, "act": nc.scalar, "dve": nc.vector, "pool": nc.gpsimd, "sp": nc.sync}
        self.sems = []
        self.csem = {}
        self.ccnt = {}
        for e in ("pe", "act", "dve", "pool"):
            self.csem[e] = self._sem("c_" + e)
            self.ccnt[e] = 0
        self.seen = {e: {} for e in self.eng}
        self.dq = {}
        self.dqn = {}
        self.dqcnt = {}
        for q in ("sp", "pool", "act"):
            self.dq[q] = [self._sem(f"d_{q}{i}") for i in range(self.ND)]
            self.dqn[q] = 0
            self.dqcnt[q] = [0] * self.ND
        self.same_sync = same_sync
        self.ninst = 0

    def _sem(self, name):
        h = self.es.enter_context(self.nc.semaphore(name))
        self.sems.append(h)
        return len(self.sems) - 1

    def sb(self, name, shape, dt):
        return self.es.enter_context(self.nc.sbuf_tensor(name, list(shape), dt))

    def _wait(self, e, ev):
        if ev is None:
            return
        si, val, src = ev
        if src == e and (e == "pe" or not self.same_sync):
            return
        if self.seen[e].get(si, 0) >= val:
            return
        self.eng[e].wait_ge(self.sems[si], val)
        self.seen[e][si] = val

    def _deps(self, e, reads, writes):
        for b in reads:
            self._wait(e, b.w)
        for b in writes:
            self._wait(e, b.w)
            for si, (val, src) in list(b.r.items()):
                self._wait(e, (si, val, src))

    def _commit(self, ev, reads, writes):
        for b in writes:
            b.w = ev
            b.r = {}
        for b in reads:
            si, val, src = ev
            b.r[si] = (val, src)

    def op(self, e, fn, reads=(), writes=()):
        self._deps(e, reads, writes)
        ins = fn(self.eng[e])
        self.ccnt[e] += 1
        ins.then_inc(self.sems[self.csem[e]], 1)
        ev = (self.csem[e], self.ccnt[e], e)
        self._commit(ev, reads, writes)
        self.ninst += 1
        return ev

    def dma(self, q, fn, reads=(), writes=()):
        e = q
        self._deps(e, reads, writes)
        slot = self.dqn[q] % self.ND
        self.dqn[q] += 1
        si = self.dq[q][slot]
        prev = 16 * self.dqcnt[q][slot]
        if prev > 0:
            self._wait(e, (si, prev, "dma"))
        ins = fn(self.eng[e])
        ins.then_inc(self.sems[si], 16)
        self.dqcnt[q][slot] += 1
        ev = (si, 16 * self.dqcnt[q][slot], "dma")
        self._commit(ev, reads, writes)
        self.ninst += 1
        return ev

    def barrier(self):
        evs = []
        for e in ("pe", "act", "dve", "pool"):
            if self.ccnt[e]:
                evs.append((self.csem[e], self.ccnt[e], "x"))
        for q in self.dq:
            for slot in range(self.ND):
                if self.dqcnt[q][slot]:
                    evs.append((self.dq[q][slot], 16 * self.dqcnt[q][slot], "x"))
        for e in self.eng:
            for ev in evs:
                self._wait(e, ev)

    def mm(self, ps, lhsT, rhs, start, stop, r, w):
        return self.op("pe", lambda g: g.matmul(ps, lhsT=lhsT, rhs=rhs, start=start, stop=stop), r, w)

    def tr(self, ps, in_, ident, r, w):
        return self.op("pe", lambda g: g.transpose(ps, in_, ident), r, w)

    def act(self, out, in_, func, r, w, scale=1.0, bias=0.0, accum=None, e="act"):
        if accum is None:
            return self.op(e, lambda g: g.activation(out=out, in_=in_, func=func, bias=bias, scale=scale), r, w)
        return self.op(e, lambda g: g.activation(out=out, in_=in_, func=func, bias=bias, scale=scale, accum_out=accum), r, w)

    def tt(self, out, a, b, op, r, w, e="dve"):
        return self.op(e, lambda g: g.tensor_tensor(out=out, in0=a, in1=b, op=op), r, w)

    def ts(self, out, a, s1, op0, r, w, s2=None, op1=None, e="dve"):
        if op1 is None:
            return self.op(e, lambda g: g.tensor_scalar(out=out, in0=a, scalar1=s1, scalar2=None, op0=op0), r, w)
        return self.op(e, lambda g: g.tensor_scalar(out=out, in0=a, scalar1=s1, scalar2=s2, op0=op0, op1=op1), r, w)

    def stt(self, out, a, s, b, op0, op1, r, w, e="dve"):
        return self.op(e, lambda g: g.scalar_tensor_tensor(out=out, in0=a, scalar=s, in1=b, op0=op0, op1=op1), r, w)

    def cp(self, out, in_, r, w, e="dve"):
        if e == "act":
            return self.op(e, lambda g: g.activation(out=out, in_=in_, func=AF.Copy), r, w)
        return self.op(e, lambda g: g.tensor_copy(out=out, in_=in_), r, w)

    def ld(self, out, in_, r, w, q="sp"):
        return self.dma(q, lambda g: g.dma_start(out=out, in_=in_), r, w)


SAME_SYNC = True


class Prog:
    def __init__(self, nlayers=DEPTH, dbg=None, same_sync=None):
        same_sync = SAME_SYNC if same_sync is None else same_sync
        self.dbg = dbg or {}
        self.nlayers = nlayers
        nc = bass.Bass("TRN2", target_bir_lowering=False)
        self.nc = nc
        self.kb = KB(nc, same_sync=same_sync)
        self.din = {}
        self.bufs = {}
        self.dbg_out = {}

    def inp(self, name, shape, dt=F32):
        t = self.nc.dram_tensor(name, list(shape), dt, kind="ExternalInput").ap()
        self.din[name] = t
        self.bufs[name] = Buf(name)
        return t

    def scratch(self, name, shape, dt):
        t = self.nc.dram_tensor(name, list(shape), dt, kind="Internal").ap()
        self.din[name] = t
        self.bufs[name] = Buf(name)
        return t

    def outp(self, name, shape, dt=F32):
        t = self.nc.dram_tensor(name, list(shape), dt, kind="ExternalOutput").ap()
        self.din[name] = t
        self.bufs[name] = Buf(name)
        return t

    def B(self, name):
        if name not in self.bufs:
            self.bufs[name] = Buf(name)
        return self.bufs[name]

    def dump(self, name, sb_ap, buf, shape, dt=F32):
        o = self.outp("dbg_" + name, shape, dt)
        self.kb.ld(o, sb_ap, [buf], [self.B("dbg_" + name)], q="sp")
        self.dbg_out[name] = "dbg_" + name


def declare_io(P):
    L = P.nlayers
    P.inp("xin", [NT, D])
    P.inp("cT", [128, KC, 2])
    P.inp("w_mod", [L, D, 6 * D])
    P.inp("b_mod2", [L, 2, 6 * D])
    P.inp("g_mixT", [L, 128, KC])
    P.inp("g_ffnT", [L, 128, KC])
    P.inp("w_in", [L, D, IN_COLS])
    P.inp("gqa_q_gain", [L, 128, 1])
    P.inp("gqa_k_gain", [L, 128, 1])
    P.inp("mla_q_a_gainT", [L, 128, 3])
    P.inp("mla_kv_a_gainT", [L, 128, 2])
    P.inp("mla_q_b", [L, 384, 384])
    P.inp("mla_kv_b", [L, 256, 768])
    P.inp("hy_conv_wT", [L, 128, 12, 3])
    P.inp("hy_conv_bT", [L, 128, 12])
    P.inp("hf_w1", [L, 33, 64])
    P.inp("hf_b1T", [L, 64, 1])
    P.inp("hf_w2", [L, 64, 64])
    P.inp("hf_b2T", [L, 64, 1])
    P.inp("hf_w3", [L, 64, 2048])
    P.inp("hf_freqT", [L, 64, 1])
    P.inp("hf_log_rate", [L, 1, 2048])
    P.inp("hy_biasT", [L, 128, 2, 4])
    P.inp("cv_wT", [L, 128, 4, 31])
    P.inp("cv_bT", [L, 128, 4])
    P.inp("cv_ln_gT", [L, 128, 4])
    P.inp("cv_ln_bT", [L, 128, 4])
    P.inp("w_brR", [L, KC, 128, 16, 128])
    P.inp("w_gateR", [L, D, KC, 512])
    P.inp("w_out", [L, D, D])
    P.inp("w_router", [L, D, NE])
    P.inp("w1", [L, NE, D, FF])
    P.inp("w3", [L, NE, D, FF])
    P.inp("w2", [L, NE, FF, D])
    P.inp("g_final", [1, D])
    P.inp("c_ident", [128, 128])
    P.inp("c_r128", [128, 128])
    P.inp("c_r32", [32, 32])
    P.inp("c_cos128", [128, NT])
    P.inp("c_sin128", [128, NT])
    P.inp("c_cos32", [32, NT])
    P.inp("c_sin32", [32, NT])
    P.inp("c_fc", [SEQ, SEQ])
    P.inp("c_fs", [SEQ, SEQ])
    P.inp("c_gc", [SEQ, SEQ])
    P.inp("c_gs", [SEQ, SEQ])
    P.inp("c_fc_c", [CTX, CTX])
    P.inp("c_fs_c", [CTX, CTX])
    P.inp("c_gc_c", [CTX, CTX])
    P.inp("c_gs_c", [CTX, CTX])
    P.inp("c_feat", [33, SEQ])
    P.inp("c_feat_c", [33, CTX])
    P.inp("c_t01", [128, 18])
    P.inp("c_iota", [128, 1])
    P.scratch("xres", [NT, D], F32)
    P.scratch("modrow", [2, 6 * D], F32)
    P.scratch("br", [4, 512, NT], BF16)
    P.scratch("xs2", [NT, D], BF16)
    P.scratch("hT_d", [128, KC, NT], BF16)
    P.scratch("mT_d", [128, KC, NT], BF16)
    for q in range(4):
        P.scratch(f"macc{q}", [NT, 512], F32)
    P.scratch("kf", [2, 16, 128, 2, 512], F32)
    P.scratch("kf_c", [2, 2, 128, 2, 512], F32)
    P.outp("out", [SEQ, D])


def load_consts(P):
    kb = P.kb
    d = P.din
    c = {}
    P.c = c

    def mk(name, shape, dt, src=None, q="sp"):
        t = kb.sb("k_" + name, shape, dt)
        c[name] = t
        b = P.B("k_" + name)
        if src is not None:
            kb.ld(t[:], d[src], [P.B(src)], [b], q=q)
        return t, b

    mk("ident_f", [128, 128], F32, "c_ident")
    mk("ident_b", [128, 128], BF16, "c_ident", q="pool")
    mk("r128", [128, 128], BF16, "c_r128", q="pool")
    mk("r32", [32, 32], BF16, "c_r32", q="pool")
    t, b = mk("ones_b", [128, 128], BF16)
    kb.op("dve", lambda g: g.memset(t[:], 1.0), [], [b])
    t2, b2 = mk("ones_f", [128, 128], F32)
    kb.op("dve", lambda g: g.memset(t2[:], 1.0), [], [b2])
    P.psf = []
    for i in range(6):
        P.psf.append((kb.es.enter_context(P.nc.psum_tensor(f"psf{i}", [128, 512], F32)), P.B(f"psf{i}")))
    P.psb = []
    for i in range(2):
        P.psb.append((kb.es.enter_context(P.nc.psum_tensor(f"psb{i}", [128, 1024], BF16)), P.B(f"psb{i}")))
    mk("modT", [128, 4, KC, 2], F32)
    mk("A", [128, 2, KC], F32)
    mk("gT", [128, KC], F32)
    kb.ld(d["xres"], d["xin"], [P.B("xin")], [P.B("xres")], q="sp")


def stage_mod(P, i):
    kb, d, c, nc = P.kb, P.din, P.c, P.nc
    kb.barrier()
    with contextlib.ExitStack() as ph:
        def sb(name, shape, dt):
            return ph.enter_context(nc.sbuf_tensor(f"m{i}_{name}", list(shape), dt))
        cT = sb("cT", [128, KC, 2], F32); bcT = Buf("cT")
        scT = sb("scT", [128, KC, 2], F32); bsc = Buf("scT")
        row = sb("row", [2, 6 * D], F32); brow = Buf("row")
        wt = [sb(f"wt{j}", [128, KC, 512], F32) for j in range(2)]
        bwt = [Buf("wt0"), Buf("wt1")]
        kb.ld(cT[:], d["cT"], [P.B("cT")], [bcT])
        kb.act(scT[:], cT[:], AF.Silu, [bcT], [bsc])
        kb.ld(row[:], d["b_mod2"][i], [P.B("b_mod2")], [brow])
        wsrc = d["w_mod"][i].rearrange("(k p) c -> p k c", p=128)
        for cg in range(24):
            w, bw = wt[cg % 2], bwt[cg % 2]
            kb.ld(w[:], wsrc[:, :, cg * 512:(cg + 1) * 512], [P.B("w_mod")], [bw])
            ps, bps = P.psf[cg % 2]
            for k in range(KC):
                kb.mm(ps[0:2, :], scT[:, k, :], w[:, k, :], k == 0, k == KC - 1, [bsc, bw], [bps])
            kb.tt(row[:, cg * 512:(cg + 1) * 512], ps[0:2, :], row[:, cg * 512:(cg + 1) * 512], ALU.add, [bps, brow], [brow])
        kb.ld(d["modrow"], row[:], [brow], [P.B("modrow")])
        ps, bps = P.psf[2]
        for vi, v in enumerate((0, 1, 3, 4)):
            for k in range(KC):
                col = (vi * KC + k) * 2
                kb.tr(ps[:, col:col + 2], row[0:2, v * D + k * 128: v * D + (k + 1) * 128], c["ident_f"][0:2, 0:2], [brow, P.B("k_ident_f")], [bps])
        kb.cp(c["modT"][:].rearrange("p v k j -> p (v k j)"), ps[:, 0:128], [bps], [P.B("k_modT")])
    kb.barrier()


def norm_stage(P, i, gname, vsh, vsc, consume, xs_dram=None):
    kb, d, c, nc = P.kb, P.din, P.c, P.nc
    with contextlib.ExitStack() as ph:
        def sb(name, shape, dt):
            return ph.enter_context(nc.sbuf_tensor(f"n{i}{gname}_{name}", list(shape), dt))
        gT, bg = c["gT"], P.B("k_gT")
        A, bA = c["A"], P.B("k_A")
        modT, bm = c["modT"], P.B("k_modT")
        kb.ld(gT[:], d[gname][i], [P.B(gname)], [bg])
        for j in range(2):
            kb.tt(A[:, j, :], gT[:], modT[:, vsc, :, j], ALU.mult, [bg, bm], [bA])
            kb.tt(A[:, j, :], A[:, j, :], gT[:], ALU.add, [bA, bg], [bA])
        xt = [sb(f"xt{j}", [128, D], F32) for j in range(2)]; bxt = [Buf("a"), Buf("b")]
        junk = sb("junk", [128, D], BF16); bj = Buf("junk")
        xs = [sb(f"xs{j}", [128, D], BF16) for j in range(8)]; bxs = [Buf(f"xs{j}") for j in range(8)]
        st = [sb(f"st{j}", [128, 4], F32) for j in range(2)]; bst = [Buf("a"), Buf("b")]
        hts = [sb(f"ht{j}", [128, KC, 512], BF16) for j in range(2)]; bht = [Buf("a"), Buf("b")]
        for gi, g4 in enumerate(range(0, NTILE, 4)):
            nt = min(4, NTILE - g4)
            j = 0 if g4 < 16 else 1
            for tl in range(nt):
                tt = g4 + tl
                p = tt % 2
                xi = (gi % 2) * 4 + tl
                kb.ld(xt[p][:], d["xres"][tt * 128:(tt + 1) * 128, :], [P.B("xres")], [bxt[p]])
                kb.op("dve", lambda g: g.memset(st[p][:], 0.0), [], [bst[p]])
                kb.act(junk[:], xt[p][:], AF.Square, [bxt[p], bst[p]], [bj, bst[p]], accum=st[p][:, 0:1])
                kb.act(st[p][:, 1:2], st[p][:, 0:1], AF.Sqrt, [bst[p]], [bst[p]], scale=1.0 / D, bias=EPS)
                kb.op("dve", lambda g: g.reciprocal(out=st[p][:, 2:3], in_=st[p][:, 1:2]), [bst[p]], [bst[p]])
                kb.ts(xs[xi][:], xt[p][:], st[p][:, 2:3], ALU.mult, [bxt[p], bst[p]], [bxs[xi]])
                if xs_dram is not None:
                    kb.ld(d[xs_dram][tt * 128:(tt + 1) * 128, :], xs[xi][:], [bxs[xi]], [P.B(xs_dram)], q="act")
            h_, bh_ = hts[gi % 2], bht[gi % 2]
            for k in range(KC):
                ps, bps = P.psb[k % 2]
                off = ((k // 2) % 2) * 512
                for tl in range(nt):
                    xi = (gi % 2) * 4 + tl
                    kb.tr(ps[:, off + tl * 128:off + (tl + 1) * 128], xs[xi][:, k * 128:(k + 1) * 128], c["ident_b"][:], [bxs[xi], P.B("k_ident_b")], [bps])
                kb.act(h_[:, k, :nt * 128], ps[:, off:off + nt * 128], AF.Identity, [bps, bA, bm], [bh_], scale=A[:, j, k:k + 1], bias=modT[:, vsh, k, j:j + 1])
            consume(g4 * 128, nt * 128, h_, bh_)


def _fm(v, k):
    sh = v.shape[:-1]
    return np.ascontiguousarray(np.swapaxes(v.reshape(*sh, k, 128), -1, -2))


_CONST_CACHE = {}


def const_tables():
    if _CONST_CACHE:
        return _CONST_CACHE
    c = {}
    c["c_ident"] = np.eye(128, dtype=np.float32)
    def perm(n):
        q = n // 4
        m = np.zeros((n, n), np.float32)
        for i in range(n):
            blk = i // q
            partner = i + q if blk % 2 == 0 else i - q
            m[partner, i] = 1.0
        return m
    c["c_r128"] = perm(128)
    c["c_r32"] = perm(32)
    rows = np.repeat(np.arange(SEQ // GRID_W), GRID_W).astype(np.float64)
    cols = np.tile(np.arange(GRID_W), SEQ // GRID_W).astype(np.float64)
    def rope_tab(hd):
        half = hd // 2
        m = half
        inv = 10000.0 ** (-np.arange(0, m, 2, dtype=np.float64) / m)
        cos = np.ones((hd, NT), np.float64)
        sin = np.zeros((hd, NT), np.float64)
        for p in range(hd):
            axis_pos = rows if p < half else cols
            q = p % half
            f = q % (m // 2)
            ang = axis_pos * inv[f]
            cos[p, :SEQ] = np.cos(ang)
            sgn = -1.0 if q < m // 2 else 1.0
            sin[p, :SEQ] = sgn * np.sin(ang)
        return cos.astype(np.float32), sin.astype(np.float32)
    c["c_cos128"], c["c_sin128"] = rope_tab(128)
    c["c_cos32"], c["c_sin32"] = rope_tab(32)
    def dft(L):
        N = 2 * L
        t = np.arange(L, dtype=np.float64)
        f = np.arange(L, dtype=np.float64)
        ang = 2.0 * np.pi * np.outer(t, f) / N
        fc = np.cos(ang)
        fs = -np.sin(ang)
        fs[:, 0] = (-1.0) ** t
        w = np.full(L, 2.0); w[0] = 1.0
        gc = (w[:, None] / N) * np.cos(ang.T)
        gs = -(2.0 / N) * np.sin(ang.T)
        gs[0, :] = (1.0 / N) * (-1.0) ** t
        return [a.astype(np.float32) for a in (fc, fs, gc, gs)]
    c["c_fc"], c["c_fs"], c["c_gc"], c["c_gs"] = dft(SEQ)
    c["c_fc_c"], c["c_fs_c"], c["c_gc_c"], c["c_gs_c"] = dft(CTX)
    def feats(L):
        pos = np.arange(L, dtype=np.float32)
        t01 = pos / np.float32(L - 1)
        bands = np.linspace(1e-4, 15, 16, dtype=np.float32)
        ang = (np.float32(2.0 * math.pi / L) * pos[:, None] * bands[None, :]).astype(np.float32)
        f = np.concatenate([t01[:, None], np.cos(ang), -np.sin(ang)], axis=-1).astype(np.float32)
        return np.ascontiguousarray(f.T), t01
    c["c_feat"], t01 = feats(SEQ)
    c["c_feat_c"], t01c = feats(CTX)
    c["c_t01"] = np.ascontiguousarray(np.concatenate([t01, t01c]).reshape(18, 128).T)
    c["c_iota"] = np.arange(128, dtype=np.float32).reshape(128, 1)
    _CONST_CACHE.update(c)
    return c


def prep_shared(inputs, nlayers):
    L = nlayers
    f = lambda n: np.ascontiguousarray(np.asarray(inputs[n], dtype=np.float32)[:L])
    s = {}
    s["w_mod"] = f("w_mod")
    s["b_mod2"] = np.ascontiguousarray(np.repeat(f("b_mod")[:, None, :], 2, axis=1))
    s["g_mixT"] = _fm(f("g_mix"), KC)
    s["g_ffnT"] = _fm(f("g_ffn"), KC)
    s["w_in"] = f("w_in")
    s["gqa_q_gain"] = f("gqa_q_gain").reshape(L, 128, 1)
    s["gqa_k_gain"] = f("gqa_k_gain").reshape(L, 128, 1)
    s["mla_q_a_gainT"] = _fm(f("mla_q_a_gain"), 3)
    s["mla_kv_a_gainT"] = _fm(f("mla_kv_a_gain"), 2)
    s["mla_q_b"] = f("mla_q_b")
    s["mla_kv_b"] = f("mla_kv_b")
    s["hy_conv_wT"] = np.ascontiguousarray(np.transpose(f("hy_conv_w").reshape(L, 3, 12, 128), (0, 3, 2, 1)))
    s["hy_conv_bT"] = _fm(f("hy_conv_b"), 12)
    s["hf_w1"] = f("hf_w1")
    s["hf_b1T"] = f("hf_b1").reshape(L, 64, 1)
    s["hf_w2"] = f("hf_w2")
    s["hf_b2T"] = f("hf_b2").reshape(L, 64, 1)
    s["hf_w3"] = f("hf_w3")
    s["hf_freqT"] = f("hf_freq").reshape(L, 64, 1)
    s["hf_log_rate"] = f("hf_log_rate").reshape(L, 1, 2048)
    s["hy_biasT"] = np.ascontiguousarray(np.transpose(f("hy_bias").reshape(L, 2, 4, 128), (0, 3, 1, 2)))
    s["cv_wT"] = np.ascontiguousarray(np.transpose(f("cv_w").reshape(L, 31, 4, 128), (0, 3, 2, 1)))
    s["cv_bT"] = _fm(f("cv_b"), 4)
    s["cv_ln_gT"] = _fm(f("cv_ln_g"), 4)
    s["cv_ln_bT"] = _fm(f("cv_ln_b"), 4)
    for n in ("w_out", "w_router", "w1", "w3", "w2"):
        s[n] = f(n)
    s["w_brR"] = np.ascontiguousarray(np.transpose(f("w_br").reshape(L, 4, 4, 128, KC, 128), (0, 4, 3, 1, 2, 5)).reshape(L, KC, 128, 16, 128))
    s["w_gateR"] = np.ascontiguousarray(np.transpose(s["w_in"][:, :, C_G:].reshape(L, D, 4, KC, 128), (0, 1, 3, 2, 4)).reshape(L, D, KC, 512))
    s["g_final"] = np.asarray(inputs["g_final"], np.float32).reshape(1, D)
    s.update(const_tables())
    return s


def prep_core(inputs, b):
    m = {}
    m["xin"] = np.ascontiguousarray(np.concatenate([np.asarray(inputs["x"][b], np.float32), np.asarray(inputs["ctx"][b], np.float32)], axis=0))
    cv = np.stack([np.asarray(inputs["c"][b], np.float32), np.asarray(inputs["c_ctx"], np.float32)], axis=0)
    m["cT"] = np.ascontiguousarray(np.transpose(cv.reshape(2, KC, 128), (2, 1, 0)))
    return m


def rms_fm(P, chunks, N, gains, dim, outs, bout, wk):
    kb, c = P.kb, P.c
    sq, bsq, rs, brs = wk["sq"], wk["bsq"], wk["rs"], wk["brs"]
    ss, bss = P.psf[3]
    n = chunks[0][0].shape[0]
    for ci, (ps, bps) in enumerate(chunks):
        kb.act(sq[:n, ci, :N], ps, AF.Square, [bps], [bsq])
    for ci in range(len(chunks)):
        kb.mm(ss[:, :N], c["ones_b"][:n, :], sq[:n, ci, :N], ci == 0, ci == len(chunks) - 1, [bsq, P.B("k_ones_b")], [bss])
    kb.act(rs[:, :N], ss[:, :N], AF.Ln, [bss], [brs], scale=1.0 / dim, bias=EPS)
    kb.act(rs[:, :N], rs[:, :N], AF.Exp, [brs], [brs], scale=-0.5)
    for ci, (ps, bps) in enumerate(chunks):
        gap, bg = gains[ci]
        kb.stt(outs[ci], ps, gap, rs[:n, :N], ALU.mult, ALU.mult, [bps, bg, brs], [bout])


def rope_fm(P, xn, bxn, n, N, cos, sin, bcs, out, bout, wk):
    kb, c = P.kb, P.c
    rot, brot = P.psf[4]
    R = c["r128"] if n == 128 else c["r32"]
    bR = P.B("k_r128" if n == 128 else "k_r32")
    kb.mm(rot[:n, :N], R[:, :], xn, True, True, [bxn, bR], [brot])
    t1, bt1, t2, bt2 = wk["t1"], wk["bt1"], wk["t2"], wk["bt2"]
    kb.tt(t1[:n, :N], xn, cos, ALU.mult, [bxn, bcs], [bt1])
    kb.tt(t2[:n, :N], rot[:n, :N], sin, ALU.mult, [brot, bcs], [bt2])
    kb.tt(out, t1[:n, :N], t2[:n, :N], ALU.add, [bt1, bt2], [bout])


def proj_fm(P, ps, bps, W, bW, col0, ncols, hTg, bh, N):
    for k in range(KC):
        P.kb.mm(ps[:ncols, :N], W[:, k, col0:col0 + ncols], hTg[:, k, :N], k == 0, k == KC - 1, [bW, bh], [bps])


def attn_stage(P, i):
    kb, d, c, nc = P.kb, P.din, P.c, P.nc
    kb.barrier()
    with contextlib.ExitStack() as ph:
        def sb(name, shape, dt):
            return ph.enter_context(nc.sbuf_tensor(f"a{i}_{name}", list(shape), dt)), Buf(name)
        W, bW = sb("W", [128, KC, 896], BF16)
        kvb, bkvb = sb("kvb", [128, 2, 768], BF16)
        qb, bqb = sb("qb", [128, 3, 384], BF16)
        gq, bgq = sb("gq", [128, 1], F32)
        gk, bgk = sb("gk", [128, 1], F32)
        gqa, bgqa = sb("gqa", [128, 3], F32)
        gkva, bgkva = sb("gkva", [128, 2], F32)
        kT, bkT = sb("kT", [128, 2, NT], BF16)
        vg, bvg = sb("vg", [128, NTILE, 256], BF16)
        kcat, bkcat = sb("kcat", [128, 4, NT], BF16)
        vm, bvm = sb("vm", [128, NTILE, 512], BF16)
        hTg = [sb(f"hTg{j}", [128, KC, 512], BF16) for j in range(2)]
        cs128 = [sb(f"cs128_{j}", [128, 2, 512], F32) for j in range(2)]
        cs32 = [sb(f"cs32_{j}", [32, 2, 512], F32) for j in range(2)]
        wk = {}
        wk["sq"], wk["bsq"] = sb("sq", [128, 3, 512], BF16)
        wk["rs"], wk["brs"] = sb("rs", [128, 512], F32)
        wk["t1"], wk["bt1"] = sb("t1", [128, 512], F32)
        wk["t2"], wk["bt2"] = sb("t2", [128, 512], F32)
        xn, bxn = sb("xn", [128, 3, 512], BF16)
        kp, bkp = sb("kp", [32, 512], BF16)
        qT, bqT = sb("qT", [128, 4, 512], BF16)
        qcat, bqcat = sb("qcat", [128, 4, 512], BF16)
        pT = [sb(f"pT{j}", [128, 512], BF16) for j in range(3)]
        rd, brd = sb("rd", [128, 512], F32)
        dacc = [sb(f"dacc{j}", [128, 512], F32) for j in range(2)]
        oT = [sb(f"oT{j}", [128, 512], BF16) for j in range(2)]

        kb.op("dve", lambda g: g.memset(kcat[:], 0.0), [], [bkcat])
        kb.op("dve", lambda g: g.memset(qcat[:], 0.0), [], [bqcat])
        kb.ld(gq[:], d["gqa_q_gain"][i], [P.B("gqa_q_gain")], [bgq])
        kb.ld(gk[:], d["gqa_k_gain"][i], [P.B("gqa_k_gain")], [bgk])
        kb.ld(gqa[:], d["mla_q_a_gainT"][i], [P.B("mla_q_a_gainT")], [bgqa])
        kb.ld(gkva[:], d["mla_kv_a_gainT"][i], [P.B("mla_kv_a_gainT")], [bgkva])
        kb.ld(kvb[:], d["mla_kv_b"][i].rearrange("(k p) c -> p k c", p=128), [P.B("mla_kv_b")], [bkvb], q="pool")
        kb.ld(qb[:], d["mla_q_b"][i].rearrange("(k p) c -> p k c", p=128), [P.B("mla_q_b")], [bqb], q="pool")
        wsrc = d["w_in"][i].rearrange("(k p) c -> p k c", p=128)
        hsrc = d["hT_d"]

        def load_group(gi):
            t0, N = TGROUPS[gi]
            h, bh = hTg[gi % 2]
            kb.ld(h[:, :, :N], hsrc[:, :, t0:t0 + N], [P.B("hT_d")], [bh])
            a, ba = cs128[gi % 2]
            kb.ld(a[:, 0, :N], d["c_cos128"][:, t0:t0 + N], [P.B("c_cos128")], [ba])
            kb.ld(a[:, 1, :N], d["c_sin128"][:, t0:t0 + N], [P.B("c_sin128")], [ba])
            a2, ba2 = cs32[gi % 2]
            kb.ld(a2[:, 0, :N], d["c_cos32"][:, t0:t0 + N], [P.B("c_cos32")], [ba2])
            kb.ld(a2[:, 1, :N], d["c_sin32"][:, t0:t0 + N], [P.B("c_sin32")], [ba2])
            return h, bh, a, ba, a2, ba2

        for k in range(KC):
            kb.ld(W[:, k, 0:800], wsrc[:, k, 0:800], [P.B("w_in")], [bW], q="pool")
        for gi, (t0, N) in enumerate(TGROUPS):
            h, bh, a, ba, a2, ba2 = load_group(gi)
            for g in range(2):
                ps, bps = P.psf[g]
                proj_fm(P, ps, bps, W, bW, C_K + g * 128, 128, h, bh, N)
                rms_fm(P, [(ps[:, :N], bps)], N, [(gk[:, 0:1], bgk)], 128, [xn[:, 0, :N]], bxn, wk)
                rope_fm(P, xn[:, 0, :N], bxn, 128, N, a[:, 0, :N], a[:, 1, :N], ba, kT[:, g, t0:t0 + N], bkT, wk)
            for tl in range(N // 128):
                tt = t0 // 128 + tl
                ps, bps = P.psf[tl % 2]
                for k in range(KC):
                    kb.mm(ps[:, 0:256], h[:, k, tl * 128:(tl + 1) * 128], W[:, k, C_V:C_V + 256], k == 0, k == KC - 1, [bh, bW], [bps])
                kb.act(vg[:, tt, :], ps[:, 0:256], AF.Copy, [bps], [bvg])
            chunks = []
            for cc in range(2):
                ps, bps = P.psf[cc]
                proj_fm(P, ps, bps, W, bW, C_KVA + cc * 128, 128, h, bh, N)
                chunks.append((ps[:, :N], bps))
            rms_fm(P, chunks, N, [(gkva[:, cc:cc + 1], bgkva) for cc in range(2)], 256, [xn[:, cc, :N] for cc in range(2)], bxn, wk)
            for hh in range(4):
                ps, bps = P.psf[hh % 2]
                for cc in range(2):
                    kb.mm(ps[:64, :N], kvb[:, cc, hh * 192:hh * 192 + 64], xn[:, cc, :N], cc == 0, cc == 1, [bkvb, bxn], [bps])
                kb.act(kcat[0:64, hh, t0:t0 + N], ps[:64, :N], AF.Copy, [bps], [bkcat])
            for tl in range(N // 128):
                tt = t0 // 128 + tl
                ps, bps = P.psf[tl % 2]
                for hh in range(4):
                    for cc in range(2):
                        kb.mm(ps[:, hh * 128:(hh + 1) * 128], xn[:, cc, tl * 128:(tl + 1) * 128], kvb[:, cc, hh * 192 + 64:hh * 192 + 192], cc == 0, cc == 1, [bkvb, bxn], [bps])
                kb.act(vm[:, tt, :], ps[:, :], AF.Copy, [bps], [bvm])
            ps, bps = P.psf[0]
            proj_fm(P, ps, bps, W, bW, C_KPE, 32, h, bh, N)
            kb.act(kp[:, :N], ps[:32, :N], AF.Copy, [bps], [bkp])
            rope_fm(P, kp[:, :N], bkp, 32, N, a2[:, 0, :N], a2[:, 1, :N], ba2, kcat[64:96, 0, t0:t0 + N], bkcat, wk)
            for hh in range(1, 4):
                kb.cp(kcat[64:96, hh, t0:t0 + N], kcat[64:96, 0, t0:t0 + N], [bkcat], [bkcat])

        for k in range(KC):
            kb.ld(W[:, k, 0:896], wsrc[:, k, C_Q:C_Q + 896], [P.B("w_in")], [bW], q="pool")
        for gi, (t0, N) in enumerate(TGROUPS):
            h, bh, a, ba, a2, ba2 = load_group(gi)
            for hh in range(4):
                ps, bps = P.psf[hh % 2]
                proj_fm(P, ps, bps, W, bW, hh * 128, 128, h, bh, N)
                rms_fm(P, [(ps[:, :N], bps)], N, [(gq[:, 0:1], bgq)], 128, [xn[:, 0, :N]], bxn, wk)
                rope_fm(P, xn[:, 0, :N], bxn, 128, N, a[:, 0, :N], a[:, 1, :N], ba, qT[:, hh, :N], bqT, wk)
            chunks = []
            for cc in range(3):
                ps, bps = P.psf[cc]
                proj_fm(P, ps, bps, W, bW, 512 + cc * 128, 128, h, bh, N)
                chunks.append((ps[:, :N], bps))
            rms_fm(P, chunks, N, [(gqa[:, cc:cc + 1], bgqa) for cc in range(3)], 384, [xn[:, cc, :N] for cc in range(3)], bxn, wk)
            for hh in range(4):
                ps, bps = P.psf[hh % 2]
                for cc in range(3):
                    kb.mm(ps[:64, :N], qb[:, cc, hh * 96:hh * 96 + 64], xn[:, cc, :N], cc == 0, cc == 2, [bqb, bxn], [bps])
                kb.act(qcat[0:64, hh, :N], ps[:64, :N], AF.Copy, [bps], [bqcat])
                ps2, bps2 = P.psf[2]
                for cc in range(3):
                    kb.mm(ps2[:32, :N], qb[:, cc, hh * 96 + 64:hh * 96 + 96], xn[:, cc, :N], cc == 0, cc == 2, [bqb, bxn], [bps2])
                kb.act(kp[:, :N], ps2[:32, :N], AF.Copy, [bps2], [bkp])
                rope_fm(P, kp[:, :N], bkp, 32, N, a2[:, 0, :N], a2[:, 1, :N], ba2, qcat[64:96, hh, :N], bqcat, wk)
            kts = list(range(NTILE)) if gi < 4 else [16, 17]
            for br_i, nh in ((1, 4), (2, 4)):
                for hh in range(nh):
                    o_ps, bo = P.psf[2 + 2 * (hh % 2)]
                    d_ps, bd = P.psf[5]
                    SB = (0, 1, 3)
                    def emit_s(ki):
                        kt = kts[ki]
                        s_ps, bs = P.psf[SB[ki % 3]]
                        if br_i == 1:
                            kb.mm(s_ps[:, :N], kT[:, hh // 2, kt * 128:(kt + 1) * 128], qT[:, hh, :N], True, True, [bkT, bqT], [bs])
                        else:
                            kb.mm(s_ps[:, :N], kcat[:, hh, kt * 128:(kt + 1) * 128], qcat[:, hh, :N], True, True, [bkcat, bqcat], [bs])
                        p_, bp = pT[ki % 3]
                        kb.act(p_[:, :N], s_ps[:, :N], AF.Exp, [bs], [bp], scale=(128.0 ** -0.5 if br_i == 1 else 96.0 ** -0.5))
                    emit_s(0)
                    emit_s(1)
                    for ki, kt in enumerate(kts):
                        if ki + 2 < len(kts):
                            emit_s(ki + 2)
                        if br_i == 1:
                            vv = vg[:, kt, (hh // 2) * 128:(hh // 2 + 1) * 128]
                            bv = bvg
                        else:
                            vv = vm[:, kt, hh * 128:(hh + 1) * 128]
                            bv = bvm
                        p_, bp = pT[ki % 3]
                        kb.mm(o_ps[:, :N], vv, p_[:, :N], ki == 0, ki == len(kts) - 1, [bv, bp], [bo])
                        da, bda = dacc[hh % 2]
                        if ki == 0:
                            kb.cp(da[:, :N], p_[:, :N], [bp], [bda])
                        else:
                            kb.tt(da[:, :N], da[:, :N], p_[:, :N], ALU.add, [bda, bp], [bda])
                    kb.mm(d_ps[:, :N], c["ones_f"][:, :], da[:, :N], True, True, [P.B("k_ones_f"), bda], [bd])
                    kb.act(rd[:, :N], d_ps[:, :N], AF.Ln, [bd], [brd])
                    kb.act(rd[:, :N], rd[:, :N], AF.Exp, [brd], [brd], scale=-1.0)
                    o_, bo_ = oT[hh % 2]
                    kb.tt(o_[:, :N], o_ps[:, :N], rd[:, :N], ALU.mult, [bo, brd], [bo_])
                    kb.ld(d["br"][br_i, hh * 128:(hh + 1) * 128, t0:t0 + N], o_[:, :N], [bo_], [P.B("br")], q="act")
    kb.barrier()


def norm1_stage(P, i):
    kb, d = P.kb, P.din
    kb.barrier()

    def consume(t0, ntok, ht, bht):
        kb.ld(d["hT_d"][:, :, t0:t0 + ntok], ht[:, :, :ntok], [bht], [P.B("hT_d")], q="act")
    norm_stage(P, i, "g_mixT", 0, 1, consume)
    kb.barrier()


SEGS = [("lat", 0, SEQ, 16, "", 0), ("ctx", SEQ, CTX, 2, "_c", 16)]


def sin3(P, out, arg, n, N, bufs, wk):
    kb = P.kb
    s, bs, s2, bs2 = wk["s"], wk["bs"], wk["s2"], wk["bs2"]
    kb.act(s[:n, :N], arg, AF.Sin, bufs, [bs], scale=1.0 / 3.0)
    kb.tt(s2[:n, :N], s[:n, :N], s[:n, :N], ALU.mult, [bs], [bs2])
    kb.ts(s2[:n, :N], s2[:n, :N], -4.0, ALU.mult, [bs2], [bs2], s2=3.0, op1=ALU.add)
    return kb.tt(out, s[:n, :N], s2[:n, :N], ALU.mult, [bs, bs2], wk["outb"])


def hy_filters(P, i):
    kb, d, c, nc = P.kb, P.din, P.c, P.nc
    kb.barrier()
    with contextlib.ExitStack() as ph:
        def sb(name, shape, dt):
            return ph.enter_context(nc.sbuf_tensor(f"f{i}_{name}", list(shape), dt)), Buf(name)
        w1, bw1 = sb("w1", [33, 64], F32)
        w2, bw2 = sb("w2", [64, 64], F32)
        w3, bw3 = sb("w3", [64, 2048], F32)
        b1, bb1 = sb("b1", [64, 1], F32)
        b2, bb2 = sb("b2", [64, 1], F32)
        fq, bfq = sb("fq", [64, 1], F32)
        rate, brate = sb("rate", [128, 2048], F32)
        t01, bt01 = sb("t01", [128, 18], F32)
        feat, bfeat = sb("feat", [33, SEQ], F32)
        h1, bh1 = sb("h1", [64, SEQ], F32)
        h2, bh2 = sb("h2", [64, SEQ], F32)
        arg, barg = sb("arg", [64, 512], F32)
        wk = {}
        wk["s"], wk["bs"] = sb("s", [64, 512], F32)
        wk["s2"], wk["bs2"] = sb("s2", [64, 512], F32)
        dec, bdec = sb("dec", [128, 2048], F32)
        filt, bfilt = sb("filt", [128, 16, 2048], BF16)
        tab = [sb(f"tab{j}", [128, 16, 128], BF16) for j in range(4)]
        res = [sb(f"res{j}", [128, 2, 2, 512], F32) for j in range(2)]
        tmp, btmp = sb("tmp", [128, 512], F32)
        kb.ld(w1[:], d["hf_w1"][i], [P.B("hf_w1")], [bw1])
        kb.ld(w2[:], d["hf_w2"][i], [P.B("hf_w2")], [bw2])
        kb.ld(w3[:], d["hf_w3"][i], [P.B("hf_w3")], [bw3])
        kb.ld(b1[:], d["hf_b1T"][i], [P.B("hf_b1T")], [bb1])
        kb.ld(b2[:], d["hf_b2T"][i], [P.B("hf_b2T")], [bb2])
        kb.ld(fq[:], d["hf_freqT"][i], [P.B("hf_freqT")], [bfq])
        kb.ld(rate[:], d["hf_log_rate"][i].partition_broadcast(128), [P.B("hf_log_rate")], [brate])
        kb.act(rate[:], rate[:], AF.Exp, [brate], [brate])
        kb.ld(t01[:], d["c_t01"], [P.B("c_t01")], [bt01])
        kb.ts(t01[:], t01[:], -1.0, ALU.mult, [bt01], [bt01])
        for (sname, toff, L, npt, suf, ttoff) in SEGS:
            kb.ld(feat[:, :L], d["c_feat" + suf], [P.B("c_feat" + suf)], [bfeat])
            G = min(L, 512)
            for g0 in range(0, L, G):
                ps, bps = P.psf[0]
                kb.mm(ps[:64, :G], w1[:, :], feat[:, g0:g0 + G], True, True, [bw1, bfeat], [bps])
                kb.ts(arg[:, :G], ps[:64, :G], b1[:, 0:1], ALU.add, [bps, bb1, bfq], [barg], s2=fq[:, 0:1], op1=ALU.mult)
                wk["outb"] = [bh1]
                sin3(P, h1[:, g0:g0 + G], arg[:, :G], 64, G, [barg], wk)
                ps, bps = P.psf[1]
                kb.mm(ps[:64, :G], w2[:, :], h1[:, g0:g0 + G], True, True, [bw2, bh1], [bps])
                kb.ts(arg[:, :G], ps[:64, :G], b2[:, 0:1], ALU.add, [bps, bb2, bfq], [barg], s2=fq[:, 0:1], op1=ALU.mult)
                wk["outb"] = [bh2]
                sin3(P, h2[:, g0:g0 + G], arg[:, :G], 64, G, [barg], wk)
            for pt in range(npt):
                kb.act(dec[:], rate[:], AF.Exp, [brate, bt01], [bdec], scale=t01[:, ttoff + pt:ttoff + pt + 1])
                for cg in range(4):
                    ps, bps = P.psf[cg % 2]
                    kb.mm(ps[:, :], h2[:, pt * 128:(pt + 1) * 128], w3[:, cg * 512:(cg + 1) * 512], True, True, [bh2, bw3], [bps])
                    kb.tt(filt[:, pt, cg * 512:(cg + 1) * 512], ps[:, :], dec[:, cg * 512:(cg + 1) * 512], ALU.mult, [bps, bdec], [bfilt])
            kb.op("dve", lambda g: g.memset(filt[0:1, 0, 512:1024], 0.0), [], [bfilt])
            kb.op("dve", lambda g: g.memset(filt[0:1, 0, 1536:2048], 0.0), [], [bfilt])
            for pt in range(npt):
                for o in range(2):
                    f_ = filt[:, pt, o * 1024:o * 1024 + 512]
                    b_ = filt[:, pt, o * 1024 + 512:o * 1024 + 1024]
                    kb.tt(b_, f_, b_, ALU.subtract, [bfilt], [bfilt])
                    kb.stt(f_, f_, 2.0, b_, ALU.mult, ALU.subtract, [bfilt], [bfilt])
            kf = d["kf" + suf]
            for ft in range(npt):
                r_, br_ = res[ft % 2]
                for cs, tn in enumerate(("c_fc", "c_fs")):
                    tb, btb = tab[2 * (ft % 2) + cs]
                    src = d[tn + suf].rearrange("(pt p) f -> p pt f", p=128)
                    kb.ld(tb[:, :npt, :], src[:, :, ft * 128:(ft + 1) * 128], [P.B(tn + suf)], [btb], q="pool")
                    for o in range(2):
                        pX, bX = P.psf[2 * o + cs]
                        c0 = o * 1024 + cs * 512
                        for pt in range(npt):
                            kb.mm(pX[:, :], tb[:, pt, :], filt[:, pt, c0:c0 + 512], pt == 0, pt == npt - 1, [btb, bfilt], [bX])
                        kb.cp(r_[:, cs, o, :], pX[:, :], [bX], [br_], e="act")
                        if cs == 1 and ft == 0:
                            pN, bN = P.psf[4]
                            for pt in range(npt):
                                kb.mm(pN[0:1, :], tb[:, pt, 0:1], filt[:, pt, o * 1024:o * 1024 + 512], pt == 0, pt == npt - 1, [btb, bfilt], [bN])
                            kb.cp(r_[0:1, 1, o, :], pN[0:1, :], [bN], [br_], e="act")
                kb.ld(kf[0, ft], r_[:, 0], [br_], [P.B("kf" + suf)], q="act")
                kb.ld(kf[1, ft], r_[:, 1], [br_], [P.B("kf" + suf)], q="act")
    kb.barrier()


def hy_stage(P, i):
    kb, d, c, nc = P.kb, P.din, P.c, P.nc
    kb.barrier()
    PADW = NT + 4
    with contextlib.ExitStack() as ph:
        def sb(name, shape, dt):
            return ph.enter_context(nc.sbuf_tensor(f"h{i}_{name}", list(shape), dt)), Buf(name)
        u, bu = sb("u", [128, 12, NT], BF16)
        cw, bcw = sb("cw", [128, 12, 3], F32)
        cb, bcb = sb("cb", [128, 12], F32)
        hb, bhb = sb("hb", [128, 2, 4], F32)
        kb.ld(cw[:], d["hy_conv_wT"][i], [P.B("hy_conv_wT")], [bcw])
        kb.ld(cb[:], d["hy_conv_bT"][i], [P.B("hy_conv_bT")], [bcb])
        kb.ld(hb[:], d["hy_biasT"][i], [P.B("hy_biasT")], [bhb])
        wsrc = d["w_in"][i].rearrange("(k p) c -> p k c", p=128)
        with contextlib.ExitStack() as ph2:
            def sb2(name, shape, dt):
                return ph2.enter_context(nc.sbuf_tensor(f"h{i}_{name}", list(shape), dt)), Buf(name)
            Ws = [sb2(f"W{j}", [128, KC, 512], BF16) for j in range(2)]
            hTg = [sb2(f"hTg{j}", [128, KC, 512], BF16) for j in range(2)]
            praw, bpraw = sb2("praw", [128, 4, PADW], F32)
            acc, bacc = sb2("acc", [128, SEQ], F32)
            kb.op("dve", lambda g: g.memset(praw[:], 0.0), [], [bpraw])
            for part in range(3):
                W, bW = Ws[part % 2]
                for k in range(KC):
                    kb.ld(W[:, k, :], wsrc[:, k, C_HY + part * 512:C_HY + (part + 1) * 512], [P.B("w_in")], [bW], q="pool")
                for gi, (t0, N) in enumerate(TGROUPS):
                    h, bh = hTg[gi % 2]
                    kb.ld(h[:, :, :N], d["hT_d"][:, :, t0:t0 + N], [P.B("hT_d")], [bh])
                    off = 1 + t0 if t0 < SEQ else 3 + t0
                    for cc in range(4):
                        ps, bps = P.psf[cc % 2]
                        proj_fm(P, ps, bps, W, bW, cc * 128, 128, h, bh, N)
                        kb.act(praw[:, cc, off:off + N], ps[:, :N], AF.Copy, [bps], [bpraw])
                for cc in range(4):
                    ch = part * 4 + cc
                    for (toff, L, poff) in ((0, SEQ, 1), (SEQ, CTX, SEQ + 3)):
                        kb.ts(acc[:, :L], praw[:, cc, poff - 1:poff - 1 + L], cw[:, ch, 0:1], ALU.mult, [bpraw, bcw, bcb], [bacc], s2=cb[:, ch:ch + 1], op1=ALU.add)
                        kb.stt(acc[:, :L], praw[:, cc, poff:poff + L], cw[:, ch, 1:2], acc[:, :L], ALU.mult, ALU.add, [bpraw, bcw, bacc], [bacc])
                        kb.stt(u[:, ch, toff:toff + L], praw[:, cc, poff + 1:poff + 1 + L], cw[:, ch, 2:3], acc[:, :L], ALU.mult, ALU.add, [bpraw, bcw, bacc], [bu])
        kb.barrier()
        if "hy_u" in P.dbg:
            P.dump("hy_u", u[:], bu, [128, 12, NT], BF16)
        z, bz = sb("z", [128, 4, SEQ], BF16)
        zt, bzt = sb("zt", [128, 16, 512], BF16)
        Yr, bYr = sb("Yr", [128, 16, 512], BF16)
        Yi, bYi = sb("Yi", [128, 16, 512], BF16)
        tabF = [sb(f"tabF{j}", [128, 16, 128], BF16) for j in range(4)]
        tabG = [sb(f"tabG{j}", [128, 16, 512], BF16) for j in range(2)]
        kfr, bkfr = sb("kfr", [128, 512], F32)
        kfi, bkfi = sb("kfi", [128, 512], F32)
        t1, bt1 = sb("t1", [128, 512], F32)
        t2, bt2 = sb("t2", [128, 512], F32)
        ob, bob = sb("ob", [128, 512], BF16)
        for (sname, toff, L, npt, suf, ttoff) in SEGS:
            kf = d["kf" + suf]
            for n in range(2):
                src_ap = (lambda cc, a, b: u[:, cc, toff + a:toff + b]) if n == 0 else (lambda cc, a, b: z[:, cc, a:b])
                bsrc = bu if n == 0 else bz
                for pt in range(npt):
                    ps, bps = P.psb[pt % 2]
                    for cc in range(4):
                        kb.tr(ps[:, cc * 128:(cc + 1) * 128], src_ap(cc, pt * 128, (pt + 1) * 128), c["ident_b"][:], [bsrc, P.B("k_ident_b")], [bps])
                    kb.cp(zt[:, pt, :], ps[:, 0:512], [bps], [bzt], e="act")
                for ft in range(npt):
                    tf = [tabF[2 * (ft % 2)], tabF[2 * (ft % 2) + 1]]
                    for cs, tn in enumerate(("c_fc", "c_fs")):
                        tb, btb = tf[cs]
                        src = d[tn + suf].rearrange("(pt p) f -> p pt f", p=128)
                        kb.ld(tb[:, :npt, :], src[:, :, ft * 128:(ft + 1) * 128], [P.B(tn + suf)], [btb], q="pool")
                    kb.ld(kfr[:], kf[0, ft, :, n, :], [P.B("kf" + suf)], [bkfr])
                    kb.ld(kfi[:], kf[1, ft, :, n, :], [P.B("kf" + suf)], [bkfi])
                    zr, bzr = P.psf[2 * (ft % 2)]
                    zi, bzi = P.psf[2 * (ft % 2) + 1]
                    for pt in range(npt):
                        kb.mm(zr[:, :], tf[0][0][:, pt, :], zt[:, pt, :], pt == 0, pt == npt - 1, [tf[0][1], bzt], [bzr])
                    for pt in range(npt):
                        kb.mm(zi[:, :], tf[1][0][:, pt, :], zt[:, pt, :], pt == 0, pt == npt - 1, [tf[1][1], bzt], [bzi])
                    kb.tt(t1[:], zr[:, :], kfi[:], ALU.mult, [bzr, bkfi], [bt1])
                    kb.tt(t2[:], zi[:, :], kfr[:], ALU.mult, [bzi, bkfr], [bt2])
                    kb.tt(Yi[:, ft, :], t1[:], t2[:], ALU.add, [bt1, bt2], [bYi])
                    kb.tt(t1[:], zr[:, :], kfr[:], ALU.mult, [bzr, bkfr], [bt1])
                    kb.tt(t2[:], zi[:, :], kfi[:], ALU.mult, [bzi, bkfi], [bt2])
                    kb.tt(Yr[:, ft, :], t1[:], t2[:], ALU.subtract, [bt1, bt2], [bYr])
                    if ft == 0:
                        kb.cp(Yr[0:1, 0, :], t1[0:1, :], [bt1], [bYr])
                        kb.cp(Yi[0:1, 0, :], t2[0:1, :], [bt2], [bYi])
                G = min(L, 512)
                for gidx, g0 in enumerate(range(0, L, G)):
                    for cs, tn in enumerate(("c_gc", "c_gs")):
                        tb, btb = tabG[cs]
                        src = d[tn + suf].rearrange("(ft p) t -> p ft t", p=128)
                        kb.ld(tb[:, :npt, :G], src[:, :, g0:g0 + G], [P.B(tn + suf)], [btb], q="pool")
                    for cc in range(4):
                        ps, bps = P.psf[2 + cc]
                        for ft in range(npt):
                            kb.mm(ps[:, :G], Yr[:, ft, cc * 128:(cc + 1) * 128], tabG[0][0][:, ft, :G], ft == 0, False, [bYr, tabG[0][1]], [bps])
                    for cc in range(4):
                        ps, bps = P.psf[2 + cc]
                        for ft in range(npt):
                            kb.mm(ps[:, :G], Yi[:, ft, cc * 128:(cc + 1) * 128], tabG[1][0][:, ft, :G], False, ft == npt - 1, [bYi, tabG[1][1]], [bps])
                    for cc in range(4):
                        ps, bps = P.psf[2 + cc]
                        kb.stt(t1[:, :G], src_ap(cc, g0, g0 + G), hb[:, n, cc:cc + 1], ps[:, :G], ALU.mult, ALU.add, [bsrc, bhb, bps], [bt1])
                        gate = u[:, 4 * (n + 1) + cc, toff + g0:toff + g0 + G]
                        if n == 0:
                            kb.tt(z[:, cc, g0:g0 + G], t1[:, :G], gate, ALU.mult, [bt1, bu], [bz])
                        else:
                            kb.tt(ob[:, :G], t1[:, :G], gate, ALU.mult, [bt1, bu], [bob])
                            kb.ld(d["br"][0, cc * 128:(cc + 1) * 128, toff + g0:toff + g0 + G], ob[:, :G], [bob], [P.B("br")], q="act")
                    if n == 0:
                        pass
                if n == 0:
                    pass
    kb.barrier()


def conf_stage(P, i):
    kb, d, c, nc = P.kb, P.din, P.c, P.nc
    kb.barrier()
    LOFF, COFF, TOT = 15, SEQ + 45, SEQ + 45 + CTX + 15
    with contextlib.ExitStack() as ph:
        def sb(name, shape, dt):
            return ph.enter_context(nc.sbuf_tensor(f"c{i}_{name}", list(shape), dt)), Buf(name)
        W, bW = sb("W", [128, KC, 1024], BF16)
        hTg = [sb(f"hTg{j}", [128, KC, 512], BF16) for j in range(2)]
        glu, bglu = sb("glu", [128, 4, TOT], BF16)
        dgm, bdgm = sb("dgm", [128, 4, 31, 128], BF16)
        uu, buu = sb("uu", [128, 4, NT], F32)
        buus = [Buf(f"uu{j}") for j in range(4)]
        cw, bcw = sb("cw", [128, 4, 31], F32)
        cb, bcb = sb("cb", [128, 4], F32)
        lg, blg = sb("lg", [128, 4], F32)
        lb, blb = sb("lb", [128, 4], F32)
        sg, bsg = sb("sg", [128, 512], F32)
        usq, busq = sb("usq", [128, 4, 512], F32)
        mean, bmean = sb("mean", [128, 512], F32)
        var, bvar = sb("var", [128, 512], F32)
        y, by = sb("y", [128, 512], F32)
        ob, bob = sb("ob", [128, 512], BF16)
        kb.ld(cw[:], d["cv_wT"][i], [P.B("cv_wT")], [bcw])
        kb.ld(cb[:], d["cv_bT"][i], [P.B("cv_bT")], [bcb])
        kb.ld(lg[:], d["cv_ln_gT"][i], [P.B("cv_ln_gT")], [blg])
        kb.ld(lb[:], d["cv_ln_bT"][i], [P.B("cv_ln_bT")], [blb])
        wsrc = d["w_in"][i].rearrange("(k p) c -> p k c", p=128)
        for k in range(KC):
            kb.ld(W[:, k, :], wsrc[:, k, C_CV:C_CV + 1024], [P.B("w_in")], [bW], q="pool")
        kb.op("dve", lambda g: g.memset(glu[:], 0.0), [], [bglu])
        for gi, (t0, N) in enumerate(TGROUPS):
            h, bh = hTg[gi % 2]
            kb.ld(h[:, :, :N], d["hT_d"][:, :, t0:t0 + N], [P.B("hT_d")], [bh])
            off = LOFF + t0 if t0 < SEQ else COFF
            for cc in range(4):
                pa, bpa = P.psf[0]
                pb, bpb = P.psf[1]
                proj_fm(P, pa, bpa, W, bW, cc * 128, 128, h, bh, N)
                proj_fm(P, pb, bpb, W, bW, 512 + cc * 128, 128, h, bh, N)
                kb.act(sg[:, :N], pb[:, :N], AF.Sigmoid, [bpb], [bsg])
                kb.tt(glu[:, cc, off:off + N], pa[:, :N], sg[:, :N], ALU.mult, [bpa, bsg], [bglu])
        for cc in range(4):
            for j in range(31):
                kb.ts(dgm[:, cc, j, :], c["ident_b"][:], cw[:, cc, j:j + 1], ALU.mult, [P.B("k_ident_b"), bcw], [bdgm])
        it = 0
        for cc in range(4):
            for (toff, L, poff) in ((0, SEQ, LOFF), (SEQ, CTX, COFF)):
                G = min(L, 512)
                for g0 in range(0, L, G):
                    ps, bps = P.psf[2 + it % 4]
                    it += 1
                    for j in range(31):
                        a0 = poff - 15 + j + g0
                        kb.mm(ps[:, :G], dgm[:, cc, j, :], glu[:, cc, a0:a0 + G], j == 0, j == 30, [bdgm, bglu], [bps])
                    kb.act(uu[:, cc, toff + g0:toff + g0 + G], ps[:, :G], AF.Identity, [bps, bcb], [buus[cc]], bias=cb[:, cc:cc + 1])
        for gi, (t0, N) in enumerate(TGROUPS):
            s_ps, bs = P.psf[0]
            q_ps, bq = P.psf[1]
            for cc in range(4):
                kb.act(usq[:, cc, :N], uu[:, cc, t0:t0 + N], AF.Square, [buus[cc]], [busq])
            for cc in range(4):
                kb.mm(s_ps[:, :N], c["ones_f"][:, :], uu[:, cc, t0:t0 + N], cc == 0, cc == 3, [P.B("k_ones_f"), buus[cc]], [bs])
            for cc in range(4):
                kb.mm(q_ps[:, :N], c["ones_f"][:, :], usq[:, cc, :N], cc == 0, cc == 3, [P.B("k_ones_f"), busq], [bq])
            kb.ts(mean[:, :N], s_ps[:, :N], 1.0 / 512, ALU.mult, [bs], [bmean])
            kb.tt(var[:, :N], mean[:, :N], mean[:, :N], ALU.mult, [bmean], [bvar])
            kb.stt(var[:, :N], q_ps[:, :N], 1.0 / 512, var[:, :N], ALU.mult, ALU.subtract, [bq, bvar], [bvar])
            kb.act(var[:, :N], var[:, :N], AF.Ln, [bvar], [bvar], bias=EPS)
            kb.act(var[:, :N], var[:, :N], AF.Exp, [bvar], [bvar], scale=-0.5)
            for cc in range(4):
                kb.tt(y[:, :N], uu[:, cc, t0:t0 + N], mean[:, :N], ALU.subtract, [buus[cc], bmean], [by])
                kb.tt(y[:, :N], y[:, :N], var[:, :N], ALU.mult, [by, bvar], [by])
                kb.act(ob[:, :N], y[:, :N], AF.Silu, [by, blg, blb], [bob], scale=lg[:, cc:cc + 1], bias=lb[:, cc:cc + 1])
                kb.ld(d["br"][3, cc * 128:(cc + 1) * 128, t0:t0 + N], ob[:, :N], [bob], [P.B("br")], q="act")
    kb.barrier()


def merge_stage(P, i):
    kb, d, c, nc = P.kb, P.din, P.c, P.nc
    kb.barrier()
    with contextlib.ExitStack() as ph:
        def sb(name, shape, dt):
            return ph.enter_context(nc.sbuf_tensor(f"g{i}_{name}", list(shape), dt)), Buf(name)
        hT, bh = sb("hT", [128, KC, NT], BF16)
        brs, bbr = sb("brs", [128, 16, NT], BF16)
        Wg = [sb(f"Wg{j}", [128, KC, 512], BF16) for j in range(2)]
        wbr = [sb(f"wbr{j}", [128, 16, 128], BF16) for j in range(2)]
        sgt = [sb(f"sgt{j}", [128, 512], F32) for j in range(2)]
        macc, bmacc = sb("macc", [128, 512], F32)
        tmp, btmp = sb("tmp", [128, 512], F32)
        mTk = [sb(f"mTk{j}", [128, 512], BF16) for j in range(2)]
        bhg = [Buf(f"hTg{g}") for g in range(len(TGROUPS))]
        bbg = [Buf(f"brg{g}") for g in range(len(TGROUPS))]
        for gi, (t0, N) in enumerate(TGROUPS):
            kb.ld(hT[:, :, t0:t0 + N], d["hT_d"][:, :, t0:t0 + N], [P.B("hT_d")], [bhg[gi]])
            for n in range(4):
                kb.ld(brs[:, n * 4:(n + 1) * 4, t0:t0 + N], d["br"][n, :, t0:t0 + N].rearrange("(wc p) t -> p wc t", p=128), [P.B("br")], [bbg[gi]])
        wgsrc = d["w_gateR"][i].rearrange("(kc p) k c -> p kc k c", p=128)
        it = 0
        for k in range(KC):
            W_, bW_ = Wg[k % 2]
            wb_, bwb_ = wbr[k % 2]
            kb.ld(W_[:], wgsrc[:, :, k, :], [P.B("w_gateR")], [bW_], q="pool")
            kb.ld(wb_[:], d["w_brR"][i, k], [P.B("w_brR")], [bwb_], q="pool")
            for gi, (t0, N) in enumerate(TGROUPS):
                m_, bm_ = mTk[it % 2]
                it += 1
                for n in range(4):
                    pg, bpg = P.psf[n % 2]
                    up, bup = P.psf[2 + n % 2]
                    sg_, bsg_ = sgt[n % 2]
                    for kc in range(KC):
                        kb.mm(pg[:, :N], W_[:, kc, n * 128:(n + 1) * 128], hT[:, kc, t0:t0 + N], kc == 0, kc == KC - 1, [bW_, bhg[gi]], [bpg])
                    for wc in range(4):
                        kb.mm(up[:, :N], wb_[:, n * 4 + wc, :], brs[:, n * 4 + wc, t0:t0 + N], wc == 0, wc == 3, [bwb_, bbg[gi]], [bup])
                    kb.act(sg_[:, :N], pg[:, :N], AF.Sigmoid, [bpg], [bsg_])
                    if n == 0:
                        kb.tt(macc[:, :N], sg_[:, :N], up[:, :N], ALU.mult, [bsg_, bup], [bmacc])
                    else:
                        kb.tt(tmp[:, :N], sg_[:, :N], up[:, :N], ALU.mult, [bsg_, bup], [btmp])
                        if n < 3:
                            kb.tt(macc[:, :N], macc[:, :N], tmp[:, :N], ALU.add, [bmacc, btmp], [bmacc])
                        else:
                            kb.tt(m_[:, :N], macc[:, :N], tmp[:, :N], ALU.add, [bmacc, btmp], [bm_])
                kb.ld(d["mT_d"][:, k, t0:t0 + N], m_[:, :N], [bm_], [P.B("mT_d")], q="act")
    kb.barrier()
    with contextlib.ExitStack() as ph:
        def sb(name, shape, dt):
            return ph.enter_context(nc.sbuf_tensor(f"o{i}_{name}", list(shape), dt)), Buf(name)
        wo, bwo = sb("wo", [128, KC, D], BF16)
        g1bc, bg1 = sb("g1bc", [128, 2, D], F32)
        mTg = [sb(f"mTg{j}", [128, KC, 512], BF16) for j in range(2)]
        xt = [sb(f"xt{j}", [128, D], F32) for j in range(2)]
        xo = [sb(f"xo{j}", [128, D], F32) for j in range(2)]
        wosrc = d["w_out"][i].rearrange("(kc p) c -> p kc c", p=128)
        for dg in range(4):
            kb.ld(wo[:, :, dg * 512:(dg + 1) * 512], wosrc[:, :, dg * 512:(dg + 1) * 512], [P.B("w_out")], [bwo], q="pool")
        for j in range(2):
            kb.ld(g1bc[:, j, :], d["modrow"][j:j + 1, 2 * D:3 * D].partition_broadcast(128), [P.B("modrow")], [bg1])
        for gi, (t0, N) in enumerate(TGROUPS):
            j = 0 if t0 < SEQ else 1
            m_, bm_ = mTg[gi % 2]
            kb.ld(m_[:, :, :N], d["mT_d"][:, :, t0:t0 + N], [P.B("mT_d")], [bm_])
            for tl in range(N // 128):
                r0 = t0 + tl * 128
                x_, bx_ = xt[tl % 2]
                o_, bo_ = xo[tl % 2]
                kb.ld(x_[:], d["xres"][r0:r0 + 128, :], [P.B("xres")], [bx_])
                for dg in range(4):
                    ps, bps = P.psf[dg % 4]
                    for k in range(KC):
                        kb.mm(ps[:, :], m_[:, k, tl * 128:(tl + 1) * 128], wo[:, k, dg * 512:(dg + 1) * 512], k == 0, k == KC - 1, [bm_, bwo], [bps])
                    kb.tt(o_[:, dg * 512:(dg + 1) * 512], ps[:, :], g1bc[:, j, dg * 512:(dg + 1) * 512], ALU.mult, [bps, bg1], [bo_])
                kb.tt(o_[:], o_[:], x_[:], ALU.add, [bo_, bx_], [bo_])
                kb.ld(d["xres"][r0:r0 + 128, :], o_[:], [bo_], [P.B("xres")], q="act")
    kb.barrier()


def moe_stage(P, i):
    kb, d, c, nc = P.kb, P.din, P.c, P.nc
    kb.barrier()
    NS = 288
    with contextlib.ExitStack() as ph:
        def sb(name, shape, dt):
            return ph.enter_context(nc.sbuf_tensor(f"e{i}_{name}", list(shape), dt)), Buf(name)
        wr, bwr = sb("wr", [128, KC, NE], BF16)
        affT, baffT = sb("affT", [16, NT], F32)
        kb.ld(wr[:], d["w_router"][i].rearrange("(k p) e -> p k e", p=128), [P.B("w_router")], [bwr], q="pool")
        w13 = [sb(f"w13_{j}", [128, KC, 2, 512], BF16) for j in range(2)]
        w2b = [sb(f"w2_{j}", [128, 8, 1024], BF16) for j in range(2)]

        def load13(e, fh):
            w_, bw_ = w13[fh]
            w1src = d["w1"][i, e].rearrange("(k p) f -> p k f", p=128)
            w3src = d["w3"][i, e].rearrange("(k p) f -> p k f", p=128)
            kb.ld(w_[:, :, 0, :], w1src[:, :, fh * 512:(fh + 1) * 512], [P.B("w1")], [bw_], q="pool")
            kb.ld(w_[:, :, 1, :], w3src[:, :, fh * 512:(fh + 1) * 512], [P.B("w3")], [bw_], q="pool")

        def load2(e, dh):
            w_, bw_ = w2b[dh]
            w2src = d["w2"][i, e].rearrange("(k p) c -> p k c", p=128)
            kb.ld(w_[:], w2src[:, :, dh * 1024:(dh + 1) * 1024], [P.B("w2")], [bw_], q="pool")

        load13(0, 0)
        load13(0, 1)
        load2(0, 0)
        load2(0, 1)
        with contextlib.ExitStack() as ph1:
            def sb1(name, shape, dt):
                return ph1.enter_context(nc.sbuf_tensor(f"e{i}_{name}", list(shape), dt)), Buf(name)
            sm = [sb1(f"sm{j}", [128, 4], F32) for j in range(2)]
            ee = [sb1(f"ee{j}", [128, NE], F32) for j in range(2)]

            def consume(t0, ntok, ht, bht):
                for tl in range(ntok // 128):
                    tt = t0 // 128 + tl
                    p = tt % 2
                    lg, blg = P.psf[p]
                    for k in range(KC):
                        kb.mm(lg[:, 0:NE], ht[:, k, tl * 128:(tl + 1) * 128], wr[:, k, :], k == 0, k == KC - 1, [bht, bwr], [blg])
                    s_, bs_ = sm[p]
                    e_, be_ = ee[p]
                    kb.op("dve", lambda g: g.reduce_max(out=s_[:, 0:1], in_=lg[:, 0:NE], axis=AX.X), [blg], [bs_])
                    kb.ts(s_[:, 1:2], s_[:, 0:1], -1.0, ALU.mult, [bs_], [bs_])
                    kb.op("dve", lambda g: g.memset(s_[:, 2:3], 0.0), [], [bs_])
                    kb.act(e_[:], lg[:, 0:NE], AF.Exp, [blg, bs_], [be_, bs_], bias=s_[:, 1:2], accum=s_[:, 2:3])
                    kb.op("dve", lambda g: g.reciprocal(out=s_[:, 3:4], in_=s_[:, 2:3]), [bs_], [bs_])
                    kb.ts(e_[:], e_[:], s_[:, 3:4], ALU.mult, [be_, bs_], [be_])
                    tp, btp = P.psf[2 + p]
                    kb.tr(tp[0:16, 0:128], e_[:, :], c["ident_f"][:, :], [be_, P.B("k_ident_f")], [btp])
                    kb.cp(affT[:, tt * 128:(tt + 1) * 128], tp[0:16, 0:128], [btp], [baffT], e="act")
            norm_stage(P, i, "g_ffnT", 2, 3, consume, xs_dram="xs2")
        kb.barrier()
        if "aff" in P.dbg:
            P.dump(f"aff{i}", affT[:], baffT, [16, NT])
        wa, bwa = sb("wa", [16, SEQ], F32)
        wb, bwb = sb("wb", [16, SEQ], F32)
        vals, bvals = sb("vals", [16, NS], F32)
        idxu, bidxu = sb("idxu", [16, NS], U32)
        idxf, bidxf = sb("idxf", [16, NS], F32)
        gT, bgT = sb("gT", [128, 3, NE], F32)
        idxT, bidxT = sb("idxT", [128, 3, NE], I32)
        for (toff, L, rounds, soff) in ((0, SEQ, 32, 0), (SEQ, CTX, 4, 256)):
            cur, bcur = affT[:, toff:toff + L], baffT
            for r in range(rounds):
                v8 = vals[:, soff + r * 8:soff + (r + 1) * 8]
                kb.op("dve", lambda g: g.max(out=v8, in_=cur), [bcur], [bvals])
                kb.op("dve", lambda g: g.max_index(out=idxu[:, soff + r * 8:soff + (r + 1) * 8], in_max=v8, in_values=cur), [bcur, bvals], [bidxu])
                nxt, bnxt = (wa, bwa) if r % 2 == 0 else (wb, bwb)
                if r < rounds - 1:
                    kb.op("dve", lambda g: g.match_replace(out=nxt[:, :L], in_to_replace=v8, in_values=cur, imm_value=-1.0), [bcur, bvals], [bnxt])
                    cur, bcur = nxt[:, :L], bnxt
        kb.cp(idxf[:], idxu[:], [bidxu], [bidxf])
        kb.ts(idxf[:, 256:NS], idxf[:, 256:NS], float(SEQ), ALU.add, [bidxf], [bidxf])
        kb.ts(idxf[:], idxf[:], 0.0, ALU.max, [bidxf], [bidxf], s2=float(NT - 1), op1=ALU.min)
        for ct, (c0, n) in enumerate(((0, 128), (128, 128), (256, 32))):
            tp, btp = P.psf[ct % 2]
            kb.tr(tp[0:n, 0:16], vals[:, c0:c0 + n], c["ident_f"][0:16, 0:16], [bvals, P.B("k_ident_f")], [btp])
            kb.cp(gT[0:n, ct, :], tp[0:n, 0:16], [btp], [bgT])
            tp2, btp2 = P.psf[2 + ct % 2]
            kb.tr(tp2[0:n, 0:16], idxf[:, c0:c0 + n], c["ident_f"][0:16, 0:16], [bidxf, P.B("k_ident_f")], [btp2])
            kb.cp(idxT[0:n, ct, :], tp2[0:n, 0:16], [btp2], [bidxT])
        if "idx" in P.dbg:
            P.dump(f"idx{i}", idxf[:], bidxf, [16, NS])
            P.dump(f"vals{i}", vals[:], bvals, [16, NS])
            P.dump(f"idxT{i}", idxT[:], bidxT, [128, 3, NE], I32)
        g2bc, bg2 = sb("g2bc", [128, 2, D], F32)
        for j in range(2):
            kb.ld(g2bc[:, j, :], d["modrow"][j:j + 1, 5 * D:6 * D].partition_broadcast(128), [P.B("modrow")], [bg2])
        xg = [sb(f"xg{j}", [128, 3, D], BF16) for j in range(1)]
        xgT, bxgT = sb("xgT", [128, KC, NS], BF16)
        hidT, bhid = sb("hidT", [128, 8, NS], BF16)
        sl, bsl = sb("sl", [128, NS], F32)
        yo, byo = sb("yo", [128, 3, D], F32)
        A, bA, modT, bm = c["A"], P.B("k_A"), c["modT"], P.B("k_modT")
        CT = ((0, 128, 0), (128, 128, 0), (256, 32, 1))
        for q in range(4):
            kb.ld(d[f"macc{q}"], d["xres"][:, q * 512:(q + 1) * 512], [P.B("xres")], [P.B(f"macc{q}")])
        x_, bx_ = xg[0]

        def gather(e):
            for ct, (c0, n, j) in enumerate(CT):
                kb.dma("pool", lambda g, ct=ct, n=n: g.indirect_dma_start(
                    out=x_[0:n, ct, :], out_offset=None, in_=d["xs2"][:, :],
                    in_offset=bass.IndirectOffsetOnAxis(ap=idxT[0:n, ct, e:e + 1], axis=0)), [P.B("xs2"), bidxT], [bx_])

        gather(0)
        for e in range(NE):
            for ct, (c0, n, j) in enumerate(CT):
                for k in range(KC):
                    ps, bps = P.psb[k % 2]
                    sl_ = ps[:, (k // 2 % 8) * 128:(k // 2 % 8) * 128 + n]
                    kb.tr(sl_, x_[0:n, ct, k * 128:(k + 1) * 128], c["ident_b"][0:n, 0:n], [bx_, P.B("k_ident_b")], [bps])
                    kb.act(xgT[:, k, c0:c0 + n], sl_, AF.Identity, [bps, bA, bm], [bxgT], scale=A[:, j, k:k + 1], bias=modT[:, 2, k, j:j + 1])
            if e + 1 < NE:
                gather(e + 1)
            for fh in range(2):
                w_, bw_ = w13[fh]
                for fi in range(4):
                    h1, bh1 = P.psf[fi % 2]
                    h3, bh3 = P.psf[2 + fi % 2]
                    for k in range(KC):
                        kb.mm(h1[:, :NS], w_[:, k, 0, fi * 128:(fi + 1) * 128], xgT[:, k, :], k == 0, k == KC - 1, [bw_, bxgT], [bh1])
                    for k in range(KC):
                        kb.mm(h3[:, :NS], w_[:, k, 1, fi * 128:(fi + 1) * 128], xgT[:, k, :], k == 0, k == KC - 1, [bw_, bxgT], [bh3])
                    kb.act(sl[:], h1[:, :NS], AF.Silu, [bh1], [bsl])
                    kb.tt(hidT[:, fh * 4 + fi, :], sl[:], h3[:, :NS], ALU.mult, [bsl, bh3], [bhid])
                if e + 1 < NE:
                    load13(e + 1, fh)
            for dh in range(2):
                w_, bw_ = w2b[dh]
                for ct, (c0, n, j) in enumerate(CT):
                    for dgi in range(2):
                        y, by = P.psf[4 + dgi]
                        for f in range(8):
                            kb.mm(y[0:n, :], hidT[:, f, c0:c0 + n], w_[:, f, dgi * 512:(dgi + 1) * 512], f == 0, f == 7, [bhid, bw_], [by])
                        col = dh * 1024 + dgi * 512
                        kb.stt(yo[0:n, ct, col:col + 512], y[0:n, :], gT[0:n, ct, e:e + 1], g2bc[0:n, j, col:col + 512], ALU.mult, ALU.mult, [by, bgT, bg2], [byo])
                if e + 1 < NE:
                    load2(e + 1, dh)
            for ct, (c0, n, j) in enumerate(CT):
                for q in range(4):
                    kb.dma("pool", lambda g, ct=ct, n=n, q=q: g.indirect_dma_start(
                        out=d[f"macc{q}"][:, :], out_offset=bass.IndirectOffsetOnAxis(ap=idxT[0:n, ct, e:e + 1], axis=0),
                        in_=yo[0:n, ct, q * 512:(q + 1) * 512], in_offset=None, compute_op=ALU.add), [byo, bidxT], [P.B(f"macc{q}")])
        for q in range(4):
            kb.ld(d["xres"][:, q * 512:(q + 1) * 512], d[f"macc{q}"], [P.B(f"macc{q}")], [P.B("xres")])
    kb.barrier()


def final_stage(P):
    kb, d, c, nc = P.kb, P.din, P.c, P.nc
    kb.barrier()
    with contextlib.ExitStack() as ph:
        def sb(name, shape, dt):
            return ph.enter_context(nc.sbuf_tensor(f"z_{name}", list(shape), dt)), Buf(name)
        gbc, bg = sb("gbc", [128, D], F32)
        kb.ld(gbc[:], d["g_final"][0:1, :].partition_broadcast(128), [P.B("g_final")], [bg])
        xt = [sb(f"xt{j}", [128, D], F32) for j in range(2)]
        ot = [sb(f"ot{j}", [128, D], F32) for j in range(2)]
        junk, bj = sb("junk", [128, D], BF16)
        st = [sb(f"st{j}", [128, 4], F32) for j in range(2)]
        for tt in range(SEQ // 128):
            p = tt % 2
            x_, bx_ = xt[p]
            o_, bo_ = ot[p]
            s_, bs_ = st[p]
            kb.ld(x_[:], d["xres"][tt * 128:(tt + 1) * 128, :], [P.B("xres")], [bx_])
            kb.op("dve", lambda g: g.memset(s_[:], 0.0), [], [bs_])
            kb.act(junk[:], x_[:], AF.Square, [bx_, bs_], [bj, bs_], accum=s_[:, 0:1])
            kb.act(s_[:, 1:2], s_[:, 0:1], AF.Sqrt, [bs_], [bs_], scale=1.0 / D, bias=EPS)
            kb.op("dve", lambda g: g.reciprocal(out=s_[:, 2:3], in_=s_[:, 1:2]), [bs_], [bs_])
            kb.stt(o_[:], x_[:], s_[:, 2:3], gbc[:], ALU.mult, ALU.mult, [bx_, bs_, bg], [bo_])
            kb.ld(d["out"][tt * 128:(tt + 1) * 128, :], o_[:], [bo_], [P.B("out")], q="act")
    kb.barrier()


def build_program(nlayers=DEPTH, dbg=None, upto=None):
    P = Prog(nlayers=nlayers, dbg=dbg)
    declare_io(P)
    load_consts(P)
    for i in range(nlayers):
        stage_mod(P, i)
        norm1_stage(P, i)
        attn_stage(P, i)
        hy_filters(P, i)
        hy_stage(P, i)
        conf_stage(P, i)
        if upto == "mixers":
            break
        merge_stage(P, i)
        if P.dbg.get("xmid") and i == 0:
            o = P.outp("dbg_xmid", [NT, D])
            P.kb.ld(o, P.din["xres"], [P.B("xres")], [P.B("dbg_xmid")])
        if upto == "merge":
            break
        moe_stage(P, i)
        if P.dbg.get("xmid") and i == 0:
            o = P.outp("dbg_xl0", [NT, D])
            P.kb.ld(o, P.din["xres"], [P.B("xres")], [P.B("dbg_xl0")])
    final_stage(P)
    return P


_PROG = {}


def kernel(**inputs):
    if "p" not in _PROG:
        _PROG["p"] = build_program(DEPTH)
    P = _PROG["p"]
    shared = prep_shared(inputs, DEPTH)
    in_maps = []
    for core in range(8):
        m = dict(shared)
        m.update(prep_core(inputs, core % 4))
        in_maps.append({k: v for k, v in m.items() if k in P.din})
    res = run_bass_kernel_spmd(P.nc, in_maps, core_ids=list(range(8)))
    out = np.stack([np.asarray(res.results[b]["out"], dtype=np.float32) for b in range(4)], axis=0)
    return out
```

```python
import contextlib
import math
import numpy as np
import concourse.bass as bass
import concourse.mybir as mybir
from concourse.bass_utils import run_bass_kernel_spmd

F32 = mybir.dt.float32
BF16 = mybir.dt.bfloat16
I32 = mybir.dt.int32
U32 = mybir.dt.uint32
AF = mybir.ActivationFunctionType
ALU = mybir.AluOpType
AX = mybir.AxisListType

D = 2048
SEQ = 2048
CTX = 256
NT = SEQ + CTX
NTILE = NT // 128
DEPTH = 4
KC = D // 128
IN_COLS = 12448
EPS = 1e-6
GRID_W = 64
NE = 16
FF = 1024
C_K, C_V, C_KVA, C_KPE, C_Q, C_QA, C_HY, C_CV, C_G = 0, 256, 512, 768, 800, 1312, 1696, 3232, 4256
TGROUPS = [(0, 512), (512, 512), (1024, 512), (1536, 512), (2048, 256)]


class Buf:
    __slots__ = ("name", "w", "r")

    def __init__(self, name):
        self.name = name
        self.w = None
        self.r = {}


class KB:
    ND = 8

    def __init__(self, nc, same_sync=True):
        self.nc = nc
        self.es = contextlib.ExitStack()
        self.eng = {"pe": nc.tensor, "act": nc.scalar, "dve": nc.vector, "pool": nc.gpsimd, "sp": nc.sync}
        self.sems = []
        self.csem = {}
        self.ccnt = {}
        for e in ("pe", "act", "dve", "pool"):
            self.csem[e] = self._sem("c_" + e)
            self.ccnt[e] = 0
        self.seen = {e: {} for e in self.eng}
        self.dq = {}
        self.dqn = {}
        self.dqcnt = {}
        for q in ("sp", "pool", "act"):
            self.dq[q] = [self._sem(f"d_{q}{i}") for i in range(self.ND)]
            self.dqn[q] = 0
            self.dqcnt[q] = [0] * self.ND
        self.same_sync = same_sync
        self.ninst = 0

    def _sem(self, name):
        h = self.es.enter_context(self.nc.semaphore(name))
        self.sems.append(h)
        return len(self.sems) - 1

    def sb(self, name, shape, dt):
        return self.es.enter_context(self.nc.sbuf_tensor(name, list(shape), dt))

    def _wait(self, e, ev):
        if ev is None:
            return
        si, val, src = ev
        if src == e and (e == "pe" or not self.same_sync):
            return
        if self.seen[e].get(si, 0) >= val:
            return
        self.eng[e].wait_ge(self.sems[si], val)
        self.seen[e][si] = val

    def _deps(self, e, reads, writes):
        for b in reads:
            self._wait(e, b.w)
        for b in writes:
            self._wait(e, b.w)
            for si, (val, src) in list(b.r.items()):
                self._wait(e, (si, val, src))

    def _commit(self, ev, reads, writes):
        for b in writes:
            b.w = ev
            b.r = {}
        for b in reads:
            si, val, src = ev
            b.r[si] = (val, src)

    def op(self, e, fn, reads=(), writes=()):
        self._deps(e, reads, writes)
        ins = fn(self.eng[e])
        self.ccnt[e] += 1
        ins.then_inc(self.sems[self.csem[e]], 1)
        ev = (self.csem[e], self.ccnt[e], e)
        self._commit(ev, reads, writes)
        self.ninst += 1
        return ev

    def dma(self, q, fn, reads=(), writes=()):
        e = q
        self._deps(e, reads, writes)
        slot = self.dqn[q] % self.ND
        self.dqn[q] += 1
        si = self.dq[q][slot]
        prev = 16 * self.dqcnt[q][slot]
        if prev > 0:
            self._wait(e, (si, prev, "dma"))
        ins = fn(self.eng[e])
        ins.then_inc(self.sems[si], 16)
        self.dqcnt[q][slot] += 1
        ev = (si, 16 * self.dqcnt[q][slot], "dma")
        self._commit(ev, reads, writes)
        self.ninst += 1
        return ev

    def barrier(self):
        evs = []
        for e in ("pe", "act", "dve", "pool"):
            if self.ccnt[e]:
                evs.append((self.csem[e], self.ccnt[e], "x"))
        for q in self.dq:
            for slot in range(self.ND):
                if self.dqcnt[q][slot]:
                    evs.append((self.dq[q][slot], 16 * self.dqcnt[q][slot], "x"))
        for e in self.eng:
            for ev in evs:
                self._wait(e, ev)

    def mm(self, ps, lhsT, rhs, start, stop, r, w):
        return self.op("pe", lambda g: g.matmul(ps, lhsT=lhsT, rhs=rhs, start=start, stop=stop), r, w)

    def tr(self, ps, in_, ident, r, w):
        return self.op("pe", lambda g: g.transpose(ps, in_, ident), r, w)

    def act(self, out, in_, func, r, w, scale=1.0, bias=0.0, accum=None, e="act"):
        if accum is None:
            return self.op(e, lambda g: g.activation(out=out, in_=in_, func=func, bias=bias, scale=scale), r, w)
        return self.op(e, lambda g: g.activation(out=out, in_=in_, func=func, bias=bias, scale=scale, accum_out=accum), r, w)

    def tt(self, out, a, b, op, r, w, e="dve"):
        return self.op(e, lambda g: g.tensor_tensor(out=out, in0=a, in1=b, op=op), r, w)

    def ts(self, out, a, s1, op0, r, w, s2=None, op1=None, e="dve"):
        if op1 is None:
            return self.op(e, lambda g: g.tensor_scalar(out=out, in0=a, scalar1=s1, scalar2=None, op0=op0), r, w)
        return self.op(e, lambda g: g.tensor_scalar(out=out, in0=a, scalar1=s1, scalar2=s2, op0=op0, op1=op1), r, w)

    def stt(self, out, a, s, b, op0, op1, r, w, e="dve"):
        return self.op(e, lambda g: g.scalar_tensor_tensor(out=out, in0=a, scalar=s, in1=b, op0=op0, op1=op1), r, w)

    def cp(self, out, in_, r, w, e="dve"):
        if e == "act":
            return self.op(e, lambda g: g.activation(out=out, in_=in_, func=AF.Copy), r, w)
        return self.op(e, lambda g: g.tensor_copy(out=out, in_=in_), r, w)

    def ld(self, out, in_, r, w, q="sp"):
        return self.dma(q, lambda g: g.dma_start(out=out, in_=in_), r, w)


SAME_SYNC = True


class Prog:
    def __init__(self, nlayers=DEPTH, dbg=None, same_sync=None):
        same_sync = SAME_SYNC if same_sync is None else same_sync
        self.dbg = dbg or {}
        self.nlayers = nlayers
        nc = bass.Bass("TRN2", target_bir_lowering=False)
        self.nc = nc
        self.kb = KB(nc, same_sync=same_sync)
        self.din = {}
        self.bufs = {}
        self.dbg_out = {}

    def inp(self, name, shape, dt=F32):
        t = self.nc.dram_tensor(name, list(shape), dt, kind="ExternalInput").ap()
        self.din[name] = t
        self.bufs[name] = Buf(name)
        return t

    def scratch(self, name, shape, dt):
        t = self.nc.dram_tensor(name, list(shape), dt, kind="Internal").ap()
        self.din[name] = t
        self.bufs[name] = Buf(name)
        return t

    def outp(self, name, shape, dt=F32):
        t = self.nc.dram_tensor(name, list(shape), dt, kind="ExternalOutput").ap()
        self.din[name] = t
        self.bufs[name] = Buf(name)
        return t

    def B(self, name):
        if name not in self.bufs:
            self.bufs[name] = Buf(name)
        return self.bufs[name]

    def dump(self, name, sb_ap, buf, shape, dt=F32):
        o = self.outp("dbg_" + name, shape, dt)
        self.kb.ld(o, sb_ap, [buf], [self.B("dbg_" + name)], q="sp")
        self.dbg_out[name] = "dbg_" + name


def declare_io(P):
    L = P.nlayers
    P.inp("xin", [NT, D])
    P.inp("cT", [128, KC, 2])
    P.inp("w_mod", [L, D, 6 * D])
    P.inp("b_mod2", [L, 2, 6 * D])
    P.inp("g_mixT", [L, 128, KC])
    P.inp("g_ffnT", [L, 128, KC])
    P.inp("w_in", [L, D, IN_COLS])
    P.inp("gqa_q_gain", [L, 128, 1])
    P.inp("gqa_k_gain", [L, 128, 1])
    P.inp("mla_q_a_gainT", [L, 128, 3])
    P.inp("mla_kv_a_gainT", [L, 128, 2])
    P.inp("mla_q_b", [L, 384, 384])
    P.inp("mla_kv_b", [L, 256, 768])
    P.inp("hy_conv_wT", [L, 128, 12, 3])
    P.inp("hy_conv_bT", [L, 128, 12])
    P.inp("hf_w1", [L, 33, 64])
    P.inp("hf_b1T", [L, 64, 1])
    P.inp("hf_w2", [L, 64, 64])
    P.inp("hf_b2T", [L, 64, 1])
    P.inp("hf_w3", [L, 64, 2048])
    P.inp("hf_freqT", [L, 64, 1])
    P.inp("hf_log_rate", [L, 1, 2048])
    P.inp("hy_biasT", [L, 128, 2, 4])
    P.inp("cv_wT", [L, 128, 4, 31])
    P.inp("cv_bT", [L, 128, 4])
    P.inp("cv_ln_gT", [L, 128, 4])
    P.inp("cv_ln_bT", [L, 128, 4])
    P.inp("w_brR", [L, KC, 128, 16, 128])
    P.inp("w_gateR", [L, D, KC, 512])
    P.inp("w_out", [L, D, D])
    P.inp("w_router", [L, D, NE])
    P.inp("w1", [L, NE, D, FF])
    P.inp("w3", [L, NE, D, FF])
    P.inp("w2", [L, NE, FF, D])
    P.inp("g_final", [1, D])
    P.inp("c_ident", [128, 128])
    P.inp("c_r128", [128, 128])
    P.inp("c_r32", [32, 32])
    P.inp("c_cos128", [128, NT])
    P.inp("c_sin128", [128, NT])
    P.inp("c_cos32", [32, NT])
    P.inp("c_sin32", [32, NT])
    P.inp("c_fc", [SEQ, SEQ])
    P.inp("c_fs", [SEQ, SEQ])
    P.inp("c_gc", [SEQ, SEQ])
    P.inp("c_gs", [SEQ, SEQ])
    P.inp("c_fc_c", [CTX, CTX])
    P.inp("c_fs_c", [CTX, CTX])
    P.inp("c_gc_c", [CTX, CTX])
    P.inp("c_gs_c", [CTX, CTX])
    P.inp("c_feat", [33, SEQ])
    P.inp("c_feat_c", [33, CTX])
    P.inp("c_t01", [128, 18])
    P.inp("c_iota", [128, 1])
    P.scratch("xres", [NT, D], F32)
    P.scratch("modrow", [2, 6 * D], F32)
    P.scratch("br", [4, 512, NT], BF16)
    P.scratch("xs2", [NT, D], BF16)
    P.scratch("hT_d", [128, KC, NT], BF16)
    P.scratch("mT_d", [128, KC, NT], BF16)
    for q in range(4):
        P.scratch(f"macc{q}", [NT, 512], F32)
    P.scratch("kf", [2, 16, 128, 2, 512], F32)
    P.scratch("kf_c", [2, 2, 128, 2, 512], F32)
    P.outp("out", [SEQ, D])


def load_consts(P):
    kb = P.kb
    d = P.din
    c = {}
    P.c = c

    def mk(name, shape, dt, src=None, q="sp"):
        t = kb.sb("k_" + name, shape, dt)
        c[name] = t
        b = P.B("k_" + name)
        if src is not None:
            kb.ld(t[:], d[src], [P.B(src)], [b], q=q)
        return t, b

    mk("ident_f", [128, 128], F32, "c_ident")
    mk("ident_b", [128, 128], BF16, "c_ident", q="pool")
    mk("r128", [128, 128], BF16, "c_r128", q="pool")
    mk("r32", [32, 32], BF16, "c_r32", q="pool")
    t, b = mk("ones_b", [128, 128], BF16)
    kb.op("dve", lambda g: g.memset(t[:], 1.0), [], [b])
    t2, b2 = mk("ones_f", [128, 128], F32)
    kb.op("dve", lambda g: g.memset(t2[:], 1.0), [], [b2])
    P.psf = []
    for i in range(6):
        P.psf.append((kb.es.enter_context(P.nc.psum_tensor(f"psf{i}", [128, 512], F32)), P.B(f"psf{i}")))
    P.psb = []
    for i in range(2):
        P.psb.append((kb.es.enter_context(P.nc.psum_tensor(f"psb{i}", [128, 1024], BF16)), P.B(f"psb{i}")))
    mk("modT", [128, 4, KC, 2], F32)
    mk("A", [128, 2, KC], F32)
    mk("gT", [128, KC], F32)
    kb.ld(d["xres"], d["xin"], [P.B("xin")], [P.B("xres")], q="sp")


def stage_mod(P, i):
    kb, d, c, nc = P.kb, P.din, P.c, P.nc
    kb.barrier()
    with contextlib.ExitStack() as ph:
        def sb(name, shape, dt):
            return ph.enter_context(nc.sbuf_tensor(f"m{i}_{name}", list(shape), dt))
        cT = sb("cT", [128, KC, 2], F32); bcT = Buf("cT")
        scT = sb("scT", [128, KC, 2], F32); bsc = Buf("scT")
        row = sb("row", [2, 6 * D], F32); brow = Buf("row")
        wt = [sb(f"wt{j}", [128, KC, 512], F32) for j in range(2)]
        bwt = [Buf("wt0"), Buf("wt1")]
        kb.ld(cT[:], d["cT"], [P.B("cT")], [bcT])
        kb.act(scT[:], cT[:], AF.Silu, [bcT], [bsc])
        kb.ld(row[:], d["b_mod2"][i], [P.B("b_mod2")], [brow])
        wsrc = d["w_mod"][i].rearrange("(k p) c -> p k c", p=128)
        for cg in range(24):
            w, bw = wt[cg % 2], bwt[cg % 2]
            kb.ld(w[:], wsrc[:, :, cg * 512:(cg + 1) * 512], [P.B("w_mod")], [bw])
            ps, bps = P.psf[cg % 2]
            for k in range(KC):
                kb.mm(ps[0:2, :], scT[:, k, :], w[:, k, :], k == 0, k == KC - 1, [bsc, bw], [bps])
            kb.tt(row[:, cg * 512:(cg + 1) * 512], ps[0:2, :], row[:, cg * 512:(cg + 1) * 512], ALU.add, [bps, brow], [brow])
        kb.ld(d["modrow"], row[:], [brow], [P.B("modrow")])
        ps, bps = P.psf[2]
        for vi, v in enumerate((0, 1, 3, 4)):
            for k in range(KC):
                col = (vi * KC + k) * 2
                kb.tr(ps[:, col:col + 2], row[0:2, v * D + k * 128: v * D + (k + 1) * 128], c["ident_f"][0:2, 0:2], [brow, P.B("k_ident_f")], [bps])
        kb.cp(c["modT"][:].rearrange("p v k j -> p (v k j)"), ps[:, 0:128], [bps], [P.B("k_modT")])
    kb.barrier()


def norm_stage(P, i, gname, vsh, vsc, consume, xs_dram=None):
    kb, d, c, nc = P.kb, P.din, P.c, P.nc
    with contextlib.ExitStack() as ph:
        def sb(name, shape, dt):
            return ph.enter_context(nc.sbuf_tensor(f"n{i}{gname}_{name}", list(shape), dt))
        gT, bg = c["gT"], P.B("k_gT")
        A, bA = c["A"], P.B("k_A")
        modT, bm = c["modT"], P.B("k_modT")
        kb.ld(gT[:], d[gname][i], [P.B(gname)], [bg])
        for j in range(2):
            kb.tt(A[:, j, :], gT[:], modT[:, vsc, :, j], ALU.mult, [bg, bm], [bA])
            kb.tt(A[:, j, :], A[:, j, :], gT[:], ALU.add, [bA, bg], [bA])
        xt = [sb(f"xt{j}", [128, D], F32) for j in range(2)]; bxt = [Buf("a"), Buf("b")]
        junk = sb("junk", [128, D], BF16); bj = Buf("junk")
        xs = [sb(f"xs{j}", [128, D], BF16) for j in range(8)]; bxs = [Buf(f"xs{j}") for j in range(8)]
        st = [sb(f"st{j}", [128, 4], F32) for j in range(2)]; bst = [Buf("a"), Buf("b")]
        hts = [sb(f"ht{j}", [128, KC, 512], BF16) for j in range(2)]; bht = [Buf("a"), Buf("b")]
        for gi, g4 in enumerate(range(0, NTILE, 4)):
            nt = min(4, NTILE - g4)
            j = 0 if g4 < 16 else 1
            for tl in range(nt):
                tt = g4 + tl
                p = tt % 2
                xi = (gi % 2) * 4 + tl
                kb.ld(xt[p][:], d["xres"][tt * 128:(tt + 1) * 128, :], [P.B("xres")], [bxt[p]])
                kb.op("dve", lambda g: g.memset(st[p][:], 0.0), [], [bst[p]])
                kb.act(junk[:], xt[p][:], AF.Square, [bxt[p], bst[p]], [bj, bst[p]], accum=st[p][:, 0:1])
                kb.act(st[p][:, 1:2], st[p][:, 0:1], AF.Sqrt, [bst[p]], [bst[p]], scale=1.0 / D, bias=EPS)
                kb.op("dve", lambda g: g.reciprocal(out=st[p][:, 2:3], in_=st[p][:, 1:2]), [bst[p]], [bst[p]])
                kb.ts(xs[xi][:], xt[p][:], st[p][:, 2:3], ALU.mult, [bxt[p], bst[p]], [bxs[xi]])
                if xs_dram is not None:
                    kb.ld(d[xs_dram][tt * 128:(tt + 1) * 128, :], xs[xi][:], [bxs[xi]], [P.B(xs_dram)], q="act")
            h_, bh_ = hts[gi % 2], bht[gi % 2]
            for k in range(KC):
                ps, bps = P.psb[k % 2]
                off = ((k // 2) % 2) * 512
                for tl in range(nt):
                    xi = (gi % 2) * 4 + tl
                    kb.tr(ps[:, off + tl * 128:off + (tl + 1) * 128], xs[xi][:, k * 128:(k + 1) * 128], c["ident_b"][:], [bxs[xi], P.B("k_ident_b")], [bps])
                kb.act(h_[:, k, :nt * 128], ps[:, off:off + nt * 128], AF.Identity, [bps, bA, bm], [bh_], scale=A[:, j, k:k + 1], bias=modT[:, vsh, k, j:j + 1])
            consume(g4 * 128, nt * 128, h_, bh_)


def _fm(v, k):
    sh = v.shape[:-1]
    return np.ascontiguousarray(np.swapaxes(v.reshape(*sh, k, 128), -1, -2))


_CONST_CACHE = {}


def const_tables():
    if _CONST_CACHE:
        return _CONST_CACHE
    c = {}
    c["c_ident"] = np.eye(128, dtype=np.float32)
    def perm(n):
        q = n // 4
        m = np.zeros((n, n), np.float32)
        for i in range(n):
            blk = i // q
            partner = i + q if blk % 2 == 0 else i - q
            m[partner, i] = 1.0
        return m
    c["c_r128"] = perm(128)
    c["c_r32"] = perm(32)
    rows = np.repeat(np.arange(SEQ // GRID_W), GRID_W).astype(np.float64)
    cols = np.tile(np.arange(GRID_W), SEQ // GRID_W).astype(np.float64)
    def rope_tab(hd):
        half = hd // 2
        m = half
        inv = 10000.0 ** (-np.arange(0, m, 2, dtype=np.float64) / m)
        cos = np.ones((hd, NT), np.float64)
        sin = np.zeros((hd, NT), np.float64)
        for p in range(hd):
            axis_pos = rows if p < half else cols
            q = p % half
            f = q % (m // 2)
            ang = axis_pos * inv[f]
            cos[p, :SEQ] = np.cos(ang)
            sgn = -1.0 if q < m // 2 else 1.0
            sin[p, :SEQ] = sgn * np.sin(ang)
        return cos.astype(np.float32), sin.astype(np.float32)
    c["c_cos128"], c["c_sin128"] = rope_tab(128)
    c["c_cos32"], c["c_sin32"] = rope_tab(32)
    def dft(L):
        N = 2 * L
        t = np.arange(L, dtype=np.float64)
        f = np.arange(L, dtype=np.float64)
        ang = 2.0 * np.pi * np.outer(t, f) / N
        fc = np.cos(ang)
        fs = -np.sin(ang)
        fs[:, 0] = (-1.0) ** t
        w = np.full(L, 2.0); w[0] = 1.0
        gc = (w[:, None] / N) * np.cos(ang.T)
        gs = -(2.0 / N) * np.sin(ang.T)
        gs[0, :] = (1.0 / N) * (-1.0) ** t
        return [a.astype(np.float32) for a in (fc, fs, gc, gs)]
    c["c_fc"], c["c_fs"], c["c_gc"], c["c_gs"] = dft(SEQ)
    c["c_fc_c"], c["c_fs_c"], c["c_gc_c"], c["c_gs_c"] = dft(CTX)
    def feats(L):
        pos = np.arange(L, dtype=np.float32)
        t01 = pos / np.float32(L - 1)
        bands = np.linspace(1e-4, 15, 16, dtype=np.float32)
        ang = (np.float32(2.0 * math.pi / L) * pos[:, None] * bands[None, :]).astype(np.float32)
        f = np.concatenate([t01[:, None], np.cos(ang), -np.sin(ang)], axis=-1).astype(np.float32)
        return np.ascontiguousarray(f.T), t01
    c["c_feat"], t01 = feats(SEQ)
    c["c_feat_c"], t01c = feats(CTX)
    c["c_t01"] = np.ascontiguousarray(np.concatenate([t01, t01c]).reshape(18, 128).T)
    c["c_iota"] = np.arange(128, dtype=np.float32).reshape(128, 1)
    _CONST_CACHE.update(c)
    return c


def prep_shared(inputs, nlayers):
    L = nlayers
    f = lambda n: np.ascontiguousarray(np.asarray(inputs[n], dtype=np.float32)[:L])
    s = {}
    s["w_mod"] = f("w_mod")
    s["b_mod2"] = np.ascontiguousarray(np.repeat(f("b_mod")[:, None, :], 2, axis=1))
    s["g_mixT"] = _fm(f("g_mix"), KC)
    s["g_ffnT"] = _fm(f("g_ffn"), KC)
    s["w_in"] = f("w_in")
    s["gqa_q_gain"] = f("gqa_q_gain").reshape(L, 128, 1)
    s["gqa_k_gain"] = f("gqa_k_gain").reshape(L, 128, 1)
    s["mla_q_a_gainT"] = _fm(f("mla_q_a_gain"), 3)
    s["mla_kv_a_gainT"] = _fm(f("mla_kv_a_gain"), 2)
    s["mla_q_b"] = f("mla_q_b")
    s["mla_kv_b"] = f("mla_kv_b")
    s["hy_conv_wT"] = np.ascontiguousarray(np.transpose(f("hy_conv_w").reshape(L, 3, 12, 128), (0, 3, 2, 1)))
    s["hy_conv_bT"] = _fm(f("hy_conv_b"), 12)
    s["hf_w1"] = f("hf_w1")
    s["hf_b1T"] = f("hf_b1").reshape(L, 64, 1)
    s["hf_w2"] = f("hf_w2")
    s["hf_b2T"] = f("hf_b2").reshape(L, 64, 1)
    s["hf_w3"] = f("hf_w3")
    s["hf_freqT"] = f("hf_freq").reshape(L, 64, 1)
    s["hf_log_rate"] = f("hf_log_rate").reshape(L, 1, 2048)
    s["hy_biasT"] = np.ascontiguousarray(np.transpose(f("hy_bias").reshape(L, 2, 4, 128), (0, 3, 1, 2)))
    s["cv_wT"] = np.ascontiguousarray(np.transpose(f("cv_w").reshape(L, 31, 4, 128), (0, 3, 2, 1)))
    s["cv_bT"] = _fm(f("cv_b"), 4)
    s["cv_ln_gT"] = _fm(f("cv_ln_g"), 4)
    s["cv_ln_bT"] = _fm(f("cv_ln_b"), 4)
    for n in ("w_out", "w_router", "w1", "w3", "w2"):
        s[n] = f(n)
    s["w_brR"] = np.ascontiguousarray(np.transpose(f("w_br").reshape(L, 4, 4, 128, KC, 128), (0, 4, 3, 1, 2, 5)).reshape(L, KC, 128, 16, 128))
    s["w_gateR"] = np.ascontiguousarray(np.transpose(s["w_in"][:, :, C_G:].reshape(L, D, 4, KC, 128), (0, 1, 3, 2, 4)).reshape(L, D, KC, 512))
    s["g_final"] = np.asarray(inputs["g_final"], np.float32).reshape(1, D)
    s.update(const_tables())
    return s


def prep_core(inputs, b):
    m = {}
    m["xin"] = np.ascontiguousarray(np.concatenate([np.asarray(inputs["x"][b], np.float32), np.asarray(inputs["ctx"][b], np.float32)], axis=0))
    cv = np.stack([np.asarray(inputs["c"][b], np.float32), np.asarray(inputs["c_ctx"], np.float32)], axis=0)
    m["cT"] = np.ascontiguousarray(np.transpose(cv.reshape(2, KC, 128), (2, 1, 0)))
    return m


def rms_fm(P, chunks, N, gains, dim, outs, bout, wk):
    kb, c = P.kb, P.c
    sq, bsq, rs, brs = wk["sq"], wk["bsq"], wk["rs"], wk["brs"]
    ss, bss = P.psf[3]
    n = chunks[0][0].shape[0]
    for ci, (ps, bps) in enumerate(chunks):
        kb.act(sq[:n, ci, :N], ps, AF.Square, [bps], [bsq])
    for ci in range(len(chunks)):
        kb.mm(ss[:, :N], c["ones_b"][:n, :], sq[:n, ci, :N], ci == 0, ci == len(chunks) - 1, [bsq, P.B("k_ones_b")], [bss])
    kb.act(rs[:, :N], ss[:, :N], AF.Ln, [bss], [brs], scale=1.0 / dim, bias=EPS)
    kb.act(rs[:, :N], rs[:, :N], AF.Exp, [brs], [brs], scale=-0.5)
    for ci, (ps, bps) in enumerate(chunks):
        gap, bg = gains[ci]
        kb.stt(outs[ci], ps, gap, rs[:n, :N], ALU.mult, ALU.mult, [bps, bg, brs], [bout])


def rope_fm(P, xn, bxn, n, N, cos, sin, bcs, out, bout, wk):
    kb, c = P.kb, P.c
    rot, brot = P.psf[4]
    R = c["r128"] if n == 128 else c["r32"]
    bR = P.B("k_r128" if n == 128 else "k_r32")
    kb.mm(rot[:n, :N], R[:, :], xn, True, True, [bxn, bR], [brot])
    t1, bt1, t2, bt2 = wk["t1"], wk["bt1"], wk["t2"], wk["bt2"]
    kb.tt(t1[:n, :N], xn, cos, ALU.mult, [bxn, bcs], [bt1])
    kb.tt(t2[:n, :N], rot[:n, :N], sin, ALU.mult, [brot, bcs], [bt2])
    kb.tt(out, t1[:n, :N], t2[:n, :N], ALU.add, [bt1, bt2], [bout])


def proj_fm(P, ps, bps, W, bW, col0, ncols, hTg, bh, N):
    for k in range(KC):
        P.kb.mm(ps[:ncols, :N], W[:, k, col0:col0 + ncols], hTg[:, k, :N], k == 0, k == KC - 1, [bW, bh], [bps])


def attn_stage(P, i):
    kb, d, c, nc = P.kb, P.din, P.c, P.nc
    kb.barrier()
    with contextlib.ExitStack() as ph:
        def sb(name, shape, dt):
            return ph.enter_context(nc.sbuf_tensor(f"a{i}_{name}", list(shape), dt)), Buf(name)
        W, bW = sb("W", [128, KC, 896], BF16)
        kvb, bkvb = sb("kvb", [128, 2, 768], BF16)
        qb, bqb = sb("qb", [128, 3, 384], BF16)
        gq, bgq = sb("gq", [128, 1], F32)
        gk, bgk = sb("gk", [128, 1], F32)
        gqa, bgqa = sb("gqa", [128, 3], F32)
        gkva, bgkva = sb("gkva", [128, 2], F32)
        kT, bkT = sb("kT", [128, 2, NT], BF16)
        vg, bvg = sb("vg", [128, NTILE, 256], BF16)
        kcat, bkcat = sb("kcat", [128, 4, NT], BF16)
        vm, bvm = sb("vm", [128, NTILE, 512], BF16)
        hTg = [sb(f"hTg{j}", [128, KC, 512], BF16) for j in range(2)]
        cs128 = [sb(f"cs128_{j}", [128, 2, 512], F32) for j in range(2)]
        cs32 = [sb(f"cs32_{j}", [32, 2, 512], F32) for j in range(2)]
        wk = {}
        wk["sq"], wk["bsq"] = sb("sq", [128, 3, 512], BF16)
        wk["rs"], wk["brs"] = sb("rs", [128, 512], F32)
        wk["t1"], wk["bt1"] = sb("t1", [128, 512], F32)
        wk["t2"], wk["bt2"] = sb("t2", [128, 512], F32)
        xn, bxn = sb("xn", [128, 3, 512], BF16)
        kp, bkp = sb("kp", [32, 512], BF16)
        qT, bqT = sb("qT", [128, 4, 512], BF16)
        qcat, bqcat = sb("qcat", [128, 4, 512], BF16)
        pT = [sb(f"pT{j}", [128, 512], BF16) for j in range(3)]
        rd, brd = sb("rd", [128, 512], F32)
        dacc = [sb(f"dacc{j}", [128, 512], F32) for j in range(2)]
        oT = [sb(f"oT{j}", [128, 512], BF16) for j in range(2)]

        kb.op("dve", lambda g: g.memset(kcat[:], 0.0), [], [bkcat])
        kb.op("dve", lambda g: g.memset(qcat[:], 0.0), [], [bqcat])
        kb.ld(gq[:], d["gqa_q_gain"][i], [P.B("gqa_q_gain")], [bgq])
        kb.ld(gk[:], d["gqa_k_gain"][i], [P.B("gqa_k_gain")], [bgk])
        kb.ld(gqa[:], d["mla_q_a_gainT"][i], [P.B("mla_q_a_gainT")], [bgqa])
        kb.ld(gkva[:], d["mla_kv_a_gainT"][i], [P.B("mla_kv_a_gainT")], [bgkva])
        kb.ld(kvb[:], d["mla_kv_b"][i].rearrange("(k p) c -> p k c", p=128), [P.B("mla_kv_b")], [bkvb], q="pool")
        kb.ld(qb[:], d["mla_q_b"][i].rearrange("(k p) c -> p k c", p=128), [P.B("mla_q_b")], [bqb], q="pool")
        wsrc = d["w_in"][i].rearrange("(k p) c -> p k c", p=128)
        hsrc = d["hT_d"]

        def load_group(gi):
            t0, N = TGROUPS[gi]
            h, bh = hTg[gi % 2]
            kb.ld(h[:, :, :N], hsrc[:, :, t0:t0 + N], [P.B("hT_d")], [bh])
            a, ba = cs128[gi % 2]
            kb.ld(a[:, 0, :N], d["c_cos128"][:, t0:t0 + N], [P.B("c_cos128")], [ba])
            kb.ld(a[:, 1, :N], d["c_sin128"][:, t0:t0 + N], [P.B("c_sin128")], [ba])
            a2, ba2 = cs32[gi % 2]
            kb.ld(a2[:, 0, :N], d["c_cos32"][:, t0:t0 + N], [P.B("c_cos32")], [ba2])
            kb.ld(a2[:, 1, :N], d["c_sin32"][:, t0:t0 + N], [P.B("c_sin32")], [ba2])
            return h, bh, a, ba, a2, ba2

        for k in range(KC):
            kb.ld(W[:, k, 0:800], wsrc[:, k, 0:800], [P.B("w_in")], [bW], q="pool")
        for gi, (t0, N) in enumerate(TGROUPS):
            h, bh, a, ba, a2, ba2 = load_group(gi)
            for g in range(2):
                ps, bps = P.psf[g]
                proj_fm(P, ps, bps, W, bW, C_K + g * 128, 128, h, bh, N)

            def vtile(tl):
                tt = t0 // 128 + tl
                ps, bps = P.psf[2] if tl % 2 == 0 else P.psf[5]
                for k in range(KC):
                    kb.mm(ps[:, 0:256], h[:, k, tl * 128:(tl + 1) * 128], W[:, k, C_V:C_V + 256], k == 0, k == KC - 1, [bh, bW], [bps])
                kb.act(vg[:, tt, :], ps[:, 0:256], AF.Copy, [bps], [bvg])
            ntl = N // 128
            for g in range(2):
                ps, bps = P.psf[g]
                rms_fm(P, [(ps[:, :N], bps)], N, [(gk[:, 0:1], bgk)], 128, [xn[:, g, :N]], bxn, wk)
                for tl in range(g * ntl // 2, (g + 1) * ntl // 2):
                    vtile(tl)
                rope_fm(P, xn[:, g, :N], bxn, 128, N, a[:, 0, :N], a[:, 1, :N], ba, kT[:, g, t0:t0 + N], bkT, wk)
            chunks = []
            for cc in range(2):
                ps, bps = P.psf[cc]
                proj_fm(P, ps, bps, W, bW, C_KVA + cc * 128, 128, h, bh, N)
                chunks.append((ps[:, :N], bps))
            rms_fm(P, chunks, N, [(gkva[:, cc:cc + 1], bgkva) for cc in range(2)], 256, [xn[:, cc, :N] for cc in range(2)], bxn, wk)
            for hh in range(4):
                ps, bps = P.psf[hh % 2]
                for cc in range(2):
                    kb.mm(ps[:64, :N], kvb[:, cc, hh * 192:hh * 192 + 64], xn[:, cc, :N], cc == 0, cc == 1, [bkvb, bxn], [bps])
                kb.act(kcat[0:64, hh, t0:t0 + N], ps[:64, :N], AF.Copy, [bps], [bkcat])
            for tl in range(N // 128):
                tt = t0 // 128 + tl
                ps, bps = P.psf[tl % 2]
                for hh in range(4):
                    for cc in range(2):
                        kb.mm(ps[:, hh * 128:(hh + 1) * 128], xn[:, cc, tl * 128:(tl + 1) * 128], kvb[:, cc, hh * 192 + 64:hh * 192 + 192], cc == 0, cc == 1, [bkvb, bxn], [bps])
                kb.act(vm[:, tt, :], ps[:, :], AF.Copy, [bps], [bvm])
            ps, bps = P.psf[0]
            proj_fm(P, ps, bps, W, bW, C_KPE, 32, h, bh, N)
            kb.act(kp[:, :N], ps[:32, :N], AF.Copy, [bps], [bkp])
            rope_fm(P, kp[:, :N], bkp, 32, N, a2[:, 0, :N], a2[:, 1, :N], ba2, kcat[64:96, 0, t0:t0 + N], bkcat, wk)
            for hh in range(1, 4):
                kb.cp(kcat[64:96, hh, t0:t0 + N], kcat[64:96, 0, t0:t0 + N], [bkcat], [bkcat])

        for k in range(KC):
            kb.ld(W[:, k, 0:896], wsrc[:, k, C_Q:C_Q + 896], [P.B("w_in")], [bW], q="pool")
        for gi, (t0, N) in enumerate(TGROUPS):
            h, bh, a, ba, a2, ba2 = load_group(gi)
            QB = (0, 1, 2)
            ps, bps = P.psf[QB[0]]
            proj_fm(P, ps, bps, W, bW, 0, 128, h, bh, N)
            for hh in range(4):
                if hh + 1 < 4:
                    psn, bpsn = P.psf[QB[(hh + 1) % 3]]
                    proj_fm(P, psn, bpsn, W, bW, (hh + 1) * 128, 128, h, bh, N)
                ps, bps = P.psf[QB[hh % 3]]
                rms_fm(P, [(ps[:, :N], bps)], N, [(gq[:, 0:1], bgq)], 128, [xn[:, hh % 3, :N]], bxn, wk)
                rope_fm(P, xn[:, hh % 3, :N], bxn, 128, N, a[:, 0, :N], a[:, 1, :N], ba, qT[:, hh, :N], bqT, wk)
            chunks = []
            for cc in range(3):
                ps, bps = P.psf[cc]
                proj_fm(P, ps, bps, W, bW, 512 + cc * 128, 128, h, bh, N)
                chunks.append((ps[:, :N], bps))
            rms_fm(P, chunks, N, [(gqa[:, cc:cc + 1], bgqa) for cc in range(3)], 384, [xn[:, cc, :N] for cc in range(3)], bxn, wk)
            for hh in range(4):
                ps, bps = P.psf[hh % 2]
                for cc in range(3):
                    kb.mm(ps[:64, :N], qb[:, cc, hh * 96:hh * 96 + 64], xn[:, cc, :N], cc == 0, cc == 2, [bqb, bxn], [bps])
                kb.act(qcat[0:64, hh, :N], ps[:64, :N], AF.Copy, [bps], [bqcat])
                ps2, bps2 = P.psf[2]
                for cc in range(3):
                    kb.mm(ps2[:32, :N], qb[:, cc, hh * 96 + 64:hh * 96 + 96], xn[:, cc, :N], cc == 0, cc == 2, [bqb, bxn], [bps2])
                kb.act(kp[:, :N], ps2[:32, :N], AF.Copy, [bps2], [bkp])
                rope_fm(P, kp[:, :N], bkp, 32, N, a2[:, 0, :N], a2[:, 1, :N], ba2, qcat[64:96, hh, :N], bqcat, wk)
            kts = list(range(NTILE)) if gi < 4 else [16, 17]
            for br_i, nh in ((1, 4), (2, 4)):
                for hh in range(nh):
                    o_ps, bo = P.psf[2 + 2 * (hh % 2)]
                    d_ps, bd = P.psf[5]
                    SB = (0, 1, 3)
                    def emit_s(ki):
                        kt = kts[ki]
                        s_ps, bs = P.psf[SB[ki % 3]]
                        if br_i == 1:
                            kb.mm(s_ps[:, :N], kT[:, hh // 2, kt * 128:(kt + 1) * 128], qT[:, hh, :N], True, True, [bkT, bqT], [bs])
                        else:
                            kb.mm(s_ps[:, :N], kcat[:, hh, kt * 128:(kt + 1) * 128], qcat[:, hh, :N], True, True, [bkcat, bqcat], [bs])
                        p_, bp = pT[ki % 3]
                        kb.act(p_[:, :N], s_ps[:, :N], AF.Exp, [bs], [bp], scale=(128.0 ** -0.5 if br_i == 1 else 96.0 ** -0.5))
                    emit_s(0)
                    emit_s(1)
                    for ki, kt in enumerate(kts):
                        if ki + 2 < len(kts):
                            emit_s(ki + 2)
                        if br_i == 1:
                            vv = vg[:, kt, (hh // 2) * 128:(hh // 2 + 1) * 128]
                            bv = bvg
                        else:
                            vv = vm[:, kt, hh * 128:(hh + 1) * 128]
                            bv = bvm
                        p_, bp = pT[ki % 3]
                        kb.mm(o_ps[:, :N], vv, p_[:, :N], ki == 0, ki == len(kts) - 1, [bv, bp], [bo])
                        da, bda = dacc[hh % 2]
                        if ki == 0:
                            kb.cp(da[:, :N], p_[:, :N], [bp], [bda])
                        else:
                            kb.tt(da[:, :N], da[:, :N], p_[:, :N], ALU.add, [bda, bp], [bda])
                    kb.mm(d_ps[:, :N], c["ones_f"][:, :], da[:, :N], True, True, [P.B("k_ones_f"), bda], [bd])
                    kb.act(rd[:, :N], d_ps[:, :N], AF.Ln, [bd], [brd])
                    kb.act(rd[:, :N], rd[:, :N], AF.Exp, [brd], [brd], scale=-1.0)
                    o_, bo_ = oT[hh % 2]
                    kb.tt(o_[:, :N], o_ps[:, :N], rd[:, :N], ALU.mult, [bo, brd], [bo_])
                    kb.ld(d["br"][br_i, hh * 128:(hh + 1) * 128, t0:t0 + N], o_[:, :N], [bo_], [P.B("br")], q="act")
    kb.barrier()


def norm1_stage(P, i):
    kb, d = P.kb, P.din
    kb.barrier()

    def consume(t0, ntok, ht, bht):
        kb.ld(d["hT_d"][:, :, t0:t0 + ntok], ht[:, :, :ntok], [bht], [P.B("hT_d")], q="act")
    norm_stage(P, i, "g_mixT", 0, 1, consume)
    kb.barrier()


SEGS = [("lat", 0, SEQ, 16, "", 0), ("ctx", SEQ, CTX, 2, "_c", 16)]


def sin3(P, out, arg, n, N, bufs, wk):
    kb = P.kb
    s, bs, s2, bs2 = wk["s"], wk["bs"], wk["s2"], wk["bs2"]
    kb.act(s[:n, :N], arg, AF.Sin, bufs, [bs], scale=1.0 / 3.0)
    kb.tt(s2[:n, :N], s[:n, :N], s[:n, :N], ALU.mult, [bs], [bs2])
    kb.ts(s2[:n, :N], s2[:n, :N], -4.0, ALU.mult, [bs2], [bs2], s2=3.0, op1=ALU.add)
    return kb.tt(out, s[:n, :N], s2[:n, :N], ALU.mult, [bs, bs2], wk["outb"])


def hy_filters(P, i):
    kb, d, c, nc = P.kb, P.din, P.c, P.nc
    kb.barrier()
    with contextlib.ExitStack() as ph:
        def sb(name, shape, dt):
            return ph.enter_context(nc.sbuf_tensor(f"f{i}_{name}", list(shape), dt)), Buf(name)
        w1, bw1 = sb("w1", [33, 64], F32)
        w2, bw2 = sb("w2", [64, 64], F32)
        w3, bw3 = sb("w3", [64, 2048], F32)
        b1, bb1 = sb("b1", [64, 1], F32)
        b2, bb2 = sb("b2", [64, 1], F32)
        fq, bfq = sb("fq", [64, 1], F32)
        rate, brate = sb("rate", [128, 2048], F32)
        t01, bt01 = sb("t01", [128, 18], F32)
        feat, bfeat = sb("feat", [33, SEQ], F32)
        h1, bh1 = sb("h1", [64, SEQ], F32)
        h2, bh2 = sb("h2", [64, SEQ], F32)
        arg, barg = sb("arg", [64, 512], F32)
        wk = {}
        wk["s"], wk["bs"] = sb("s", [64, 512], F32)
        wk["s2"], wk["bs2"] = sb("s2", [64, 512], F32)
        dec, bdec = sb("dec", [128, 2048], F32)
        filt, bfilt = sb("filt", [128, 16, 2048], BF16)
        tab = [sb(f"tab{j}", [128, 16, 128], BF16) for j in range(4)]
        res = [sb(f"res{j}", [128, 2, 2, 512], F32) for j in range(2)]
        tmp, btmp = sb("tmp", [128, 512], F32)
        kb.ld(w1[:], d["hf_w1"][i], [P.B("hf_w1")], [bw1])
        kb.ld(w2[:], d["hf_w2"][i], [P.B("hf_w2")], [bw2])
        kb.ld(w3[:], d["hf_w3"][i], [P.B("hf_w3")], [bw3])
        kb.ld(b1[:], d["hf_b1T"][i], [P.B("hf_b1T")], [bb1])
        kb.ld(b2[:], d["hf_b2T"][i], [P.B("hf_b2T")], [bb2])
        kb.ld(fq[:], d["hf_freqT"][i], [P.B("hf_freqT")], [bfq])
        kb.ld(rate[:], d["hf_log_rate"][i].partition_broadcast(128), [P.B("hf_log_rate")], [brate])
        kb.act(rate[:], rate[:], AF.Exp, [brate], [brate])
        kb.ld(t01[:], d["c_t01"], [P.B("c_t01")], [bt01])
        kb.ts(t01[:], t01[:], -1.0, ALU.mult, [bt01], [bt01])
        for (sname, toff, L, npt, suf, ttoff) in SEGS:
            kb.ld(feat[:, :L], d["c_feat" + suf], [P.B("c_feat" + suf)], [bfeat])
            G = min(L, 512)
            for g0 in range(0, L, G):
                ps, bps = P.psf[0]
                kb.mm(ps[:64, :G], w1[:, :], feat[:, g0:g0 + G], True, True, [bw1, bfeat], [bps])
                kb.ts(arg[:, :G], ps[:64, :G], b1[:, 0:1], ALU.add, [bps, bb1, bfq], [barg], s2=fq[:, 0:1], op1=ALU.mult)
                wk["outb"] = [bh1]
                sin3(P, h1[:, g0:g0 + G], arg[:, :G], 64, G, [barg], wk)
                ps, bps = P.psf[1]
                kb.mm(ps[:64, :G], w2[:, :], h1[:, g0:g0 + G], True, True, [bw2, bh1], [bps])
                kb.ts(arg[:, :G], ps[:64, :G], b2[:, 0:1], ALU.add, [bps, bb2, bfq], [barg], s2=fq[:, 0:1], op1=ALU.mult)
                wk["outb"] = [bh2]
                sin3(P, h2[:, g0:g0 + G], arg[:, :G], 64, G, [barg], wk)
            for pt in range(npt):
                kb.act(dec[:], rate[:], AF.Exp, [brate, bt01], [bdec], scale=t01[:, ttoff + pt:ttoff + pt + 1])
                for cg in range(4):
                    ps, bps = P.psf[cg % 2]
                    kb.mm(ps[:, :], h2[:, pt * 128:(pt + 1) * 128], w3[:, cg * 512:(cg + 1) * 512], True, True, [bh2, bw3], [bps])
                    kb.tt(filt[:, pt, cg * 512:(cg + 1) * 512], ps[:, :], dec[:, cg * 512:(cg + 1) * 512], ALU.mult, [bps, bdec], [bfilt])
            kb.op("dve", lambda g: g.memset(filt[0:1, 0, 512:1024], 0.0), [], [bfilt])
            kb.op("dve", lambda g: g.memset(filt[0:1, 0, 1536:2048], 0.0), [], [bfilt])
            for pt in range(npt):
                for o in range(2):
                    f_ = filt[:, pt, o * 1024:o * 1024 + 512]
                    b_ = filt[:, pt, o * 1024 + 512:o * 1024 + 1024]
                    kb.tt(b_, f_, b_, ALU.subtract, [bfilt], [bfilt])
                    kb.stt(f_, f_, 2.0, b_, ALU.mult, ALU.subtract, [bfilt], [bfilt])
            kf = d["kf" + suf]
            for ft in range(npt):
                r_, br_ = res[ft % 2]
                for cs, tn in enumerate(("c_fc", "c_fs")):
                    tb, btb = tab[2 * (ft % 2) + cs]
                    src = d[tn + suf].rearrange("(pt p) f -> p pt f", p=128)
                    kb.ld(tb[:, :npt, :], src[:, :, ft * 128:(ft + 1) * 128], [P.B(tn + suf)], [btb], q="pool")
                    for o in range(2):
                        pX, bX = P.psf[2 * o + cs]
                        c0 = o * 1024 + cs * 512
                        for pt in range(npt):
                            kb.mm(pX[:, :], tb[:, pt, :], filt[:, pt, c0:c0 + 512], pt == 0, pt == npt - 1, [btb, bfilt], [bX])
                        kb.cp(r_[:, cs, o, :], pX[:, :], [bX], [br_], e="act")
                        if cs == 1 and ft == 0:
                            pN, bN = P.psf[4]
                            for pt in range(npt):
                                kb.mm(pN[0:1, :], tb[:, pt, 0:1], filt[:, pt, o * 1024:o * 1024 + 512], pt == 0, pt == npt - 1, [btb, bfilt], [bN])
                            kb.cp(r_[0:1, 1, o, :], pN[0:1, :], [bN], [br_], e="act")
                kb.ld(kf[0, ft], r_[:, 0], [br_], [P.B("kf" + suf)], q="act")
                kb.ld(kf[1, ft], r_[:, 1], [br_], [P.B("kf" + suf)], q="act")
    kb.barrier()


def hy_stage(P, i):
    kb, d, c, nc = P.kb, P.din, P.c, P.nc
    kb.barrier()
    PADW = NT + 4
    with contextlib.ExitStack() as ph:
        def sb(name, shape, dt):
            return ph.enter_context(nc.sbuf_tensor(f"h{i}_{name}", list(shape), dt)), Buf(name)
        u, bu = sb("u", [128, 12, NT], BF16)
        cw, bcw = sb("cw", [128, 12, 3], F32)
        cb, bcb = sb("cb", [128, 12], F32)
        hb, bhb = sb("hb", [128, 2, 4], F32)
        kb.ld(cw[:], d["hy_conv_wT"][i], [P.B("hy_conv_wT")], [bcw])
        kb.ld(cb[:], d["hy_conv_bT"][i], [P.B("hy_conv_bT")], [bcb])
        kb.ld(hb[:], d["hy_biasT"][i], [P.B("hy_biasT")], [bhb])
        wsrc = d["w_in"][i].rearrange("(k p) c -> p k c", p=128)
        with contextlib.ExitStack() as ph2:
            def sb2(name, shape, dt):
                return ph2.enter_context(nc.sbuf_tensor(f"h{i}_{name}", list(shape), dt)), Buf(name)
            Ws = [sb2(f"W{j}", [128, KC, 512], BF16) for j in range(2)]
            hTg = [sb2(f"hTg{j}", [128, KC, 512], BF16) for j in range(2)]
            praw, bpraw = sb2("praw", [128, 4, PADW], F32)
            acc, bacc = sb2("acc", [128, SEQ], F32)
            kb.op("dve", lambda g: g.memset(praw[:], 0.0), [], [bpraw])
            for part in range(3):
                W, bW = Ws[part % 2]
                for k in range(KC):
                    kb.ld(W[:, k, :], wsrc[:, k, C_HY + part * 512:C_HY + (part + 1) * 512], [P.B("w_in")], [bW], q="pool")
                for gi, (t0, N) in enumerate(TGROUPS):
                    h, bh = hTg[gi % 2]
                    kb.ld(h[:, :, :N], d["hT_d"][:, :, t0:t0 + N], [P.B("hT_d")], [bh])
                    off = 1 + t0 if t0 < SEQ else 3 + t0
                    for cc in range(4):
                        ps, bps = P.psf[cc % 2]
                        proj_fm(P, ps, bps, W, bW, cc * 128, 128, h, bh, N)
                        kb.act(praw[:, cc, off:off + N], ps[:, :N], AF.Copy, [bps], [bpraw])
                for cc in range(4):
                    ch = part * 4 + cc
                    for (toff, L, poff) in ((0, SEQ, 1), (SEQ, CTX, SEQ + 3)):
                        kb.ts(acc[:, :L], praw[:, cc, poff - 1:poff - 1 + L], cw[:, ch, 0:1], ALU.mult, [bpraw, bcw, bcb], [bacc], s2=cb[:, ch:ch + 1], op1=ALU.add)
                        kb.stt(acc[:, :L], praw[:, cc, poff:poff + L], cw[:, ch, 1:2], acc[:, :L], ALU.mult, ALU.add, [bpraw, bcw, bacc], [bacc])
                        kb.stt(u[:, ch, toff:toff + L], praw[:, cc, poff + 1:poff + 1 + L], cw[:, ch, 2:3], acc[:, :L], ALU.mult, ALU.add, [bpraw, bcw, bacc], [bu])
        kb.barrier()
        if "hy_u" in P.dbg:
            P.dump("hy_u", u[:], bu, [128, 12, NT], BF16)
        z, bz = sb("z", [128, 4, SEQ], BF16)
        zt, bzt = sb("zt", [128, 16, 512], BF16)
        Yr, bYr = sb("Yr", [128, 16, 512], BF16)
        Yi, bYi = sb("Yi", [128, 16, 512], BF16)
        tabF = [sb(f"tabF{j}", [128, 16, 128], BF16) for j in range(4)]
        tabG = [sb(f"tabG{j}", [128, 16, 512], BF16) for j in range(2)]
        kfr, bkfr = sb("kfr", [128, 512], F32)
        kfi, bkfi = sb("kfi", [128, 512], F32)
        t1, bt1 = sb("t1", [128, 512], F32)
        t2, bt2 = sb("t2", [128, 512], F32)
        ob, bob = sb("ob", [128, 512], BF16)
        for (sname, toff, L, npt, suf, ttoff) in SEGS:
            kf = d["kf" + suf]
            for n in range(2):
                src_ap = (lambda cc, a, b: u[:, cc, toff + a:toff + b]) if n == 0 else (lambda cc, a, b: z[:, cc, a:b])
                bsrc = bu if n == 0 else bz
                for pt in range(npt):
                    ps, bps = P.psb[pt % 2]
                    for cc in range(4):
                        kb.tr(ps[:, cc * 128:(cc + 1) * 128], src_ap(cc, pt * 128, (pt + 1) * 128), c["ident_b"][:], [bsrc, P.B("k_ident_b")], [bps])
                    kb.cp(zt[:, pt, :], ps[:, 0:512], [bps], [bzt], e="act")
                for ft in range(npt):
                    tf = [tabF[2 * (ft % 2)], tabF[2 * (ft % 2) + 1]]
                    for cs, tn in enumerate(("c_fc", "c_fs")):
                        tb, btb = tf[cs]
                        src = d[tn + suf].rearrange("(pt p) f -> p pt f", p=128)
                        kb.ld(tb[:, :npt, :], src[:, :, ft * 128:(ft + 1) * 128], [P.B(tn + suf)], [btb], q="pool")
                    kb.ld(kfr[:], kf[0, ft, :, n, :], [P.B("kf" + suf)], [bkfr])
                    kb.ld(kfi[:], kf[1, ft, :, n, :], [P.B("kf" + suf)], [bkfi])
                    zr, bzr = P.psf[2 * (ft % 2)]
                    zi, bzi = P.psf[2 * (ft % 2) + 1]
                    for pt in range(npt):
                        kb.mm(zr[:, :], tf[0][0][:, pt, :], zt[:, pt, :], pt == 0, pt == npt - 1, [tf[0][1], bzt], [bzr])
                    for pt in range(npt):
                        kb.mm(zi[:, :], tf[1][0][:, pt, :], zt[:, pt, :], pt == 0, pt == npt - 1, [tf[1][1], bzt], [bzi])
                    kb.tt(t1[:], zr[:, :], kfi[:], ALU.mult, [bzr, bkfi], [bt1])
                    kb.tt(t2[:], zi[:, :], kfr[:], ALU.mult, [bzi, bkfr], [bt2])
                    kb.tt(Yi[:, ft, :], t1[:], t2[:], ALU.add, [bt1, bt2], [bYi])
                    kb.tt(t1[:], zr[:, :], kfr[:], ALU.mult, [bzr, bkfr], [bt1])
                    kb.tt(t2[:], zi[:, :], kfi[:], ALU.mult, [bzi, bkfi], [bt2])
                    kb.tt(Yr[:, ft, :], t1[:], t2[:], ALU.subtract, [bt1, bt2], [bYr])
                    if ft == 0:
                        kb.cp(Yr[0:1, 0, :], t1[0:1, :], [bt1], [bYr])
                        kb.cp(Yi[0:1, 0, :], t2[0:1, :], [bt2], [bYi])
                G = min(L, 512)
                for gidx, g0 in enumerate(range(0, L, G)):
                    for cs, tn in enumerate(("c_gc", "c_gs")):
                        tb, btb = tabG[cs]
                        src = d[tn + suf].rearrange("(ft p) t -> p ft t", p=128)
                        kb.ld(tb[:, :npt, :G], src[:, :, g0:g0 + G], [P.B(tn + suf)], [btb], q="pool")
                    for cc in range(4):
                        ps, bps = P.psf[2 + cc]
                        for ft in range(npt):
                            kb.mm(ps[:, :G], Yr[:, ft, cc * 128:(cc + 1) * 128], tabG[0][0][:, ft, :G], ft == 0, False, [bYr, tabG[0][1]], [bps])
                    for cc in range(4):
                        ps, bps = P.psf[2 + cc]
                        for ft in range(npt):
                            kb.mm(ps[:, :G], Yi[:, ft, cc * 128:(cc + 1) * 128], tabG[1][0][:, ft, :G], False, ft == npt - 1, [bYi, tabG[1][1]], [bps])
                    for cc in range(4):
                        ps, bps = P.psf[2 + cc]
                        kb.stt(t1[:, :G], src_ap(cc, g0, g0 + G), hb[:, n, cc:cc + 1], ps[:, :G], ALU.mult, ALU.add, [bsrc, bhb, bps], [bt1])
                        gate = u[:, 4 * (n + 1) + cc, toff + g0:toff + g0 + G]
                        if n == 0:
                            kb.tt(z[:, cc, g0:g0 + G], t1[:, :G], gate, ALU.mult, [bt1, bu], [bz])
                        else:
                            kb.tt(ob[:, :G], t1[:, :G], gate, ALU.mult, [bt1, bu], [bob])
                            kb.ld(d["br"][0, cc * 128:(cc + 1) * 128, toff + g0:toff + g0 + G], ob[:, :G], [bob], [P.B("br")], q="act")
                    if n == 0:
                        pass
                if n == 0:
                    pass
    kb.barrier()


def conf_stage(P, i):
    kb, d, c, nc = P.kb, P.din, P.c, P.nc
    kb.barrier()
    LOFF, COFF, TOT = 15, SEQ + 45, SEQ + 45 + CTX + 15
    with contextlib.ExitStack() as ph:
        def sb(name, shape, dt):
            return ph.enter_context(nc.sbuf_tensor(f"c{i}_{name}", list(shape), dt)), Buf(name)
        W, bW = sb("W", [128, KC, 1024], BF16)
        hTg = [sb(f"hTg{j}", [128, KC, 512], BF16) for j in range(2)]
        glu, bglu = sb("glu", [128, 4, TOT], BF16)
        dgm, bdgm = sb("dgm", [128, 4, 31, 128], BF16)
        uu, buu = sb("uu", [128, 4, NT], F32)
        buus = [Buf(f"uu{j}") for j in range(4)]
        cw, bcw = sb("cw", [128, 4, 31], F32)
        cb, bcb = sb("cb", [128, 4], F32)
        lg, blg = sb("lg", [128, 4], F32)
        lb, blb = sb("lb", [128, 4], F32)
        sg, bsg = sb("sg", [128, 512], F32)
        usq, busq = sb("usq", [128, 4, 512], F32)
        mean, bmean = sb("mean", [128, 512], F32)
        var, bvar = sb("var", [128, 512], F32)
        y, by = sb("y", [128, 512], F32)
        ob, bob = sb("ob", [128, 512], BF16)
        kb.ld(cw[:], d["cv_wT"][i], [P.B("cv_wT")], [bcw])
        kb.ld(cb[:], d["cv_bT"][i], [P.B("cv_bT")], [bcb])
        kb.ld(lg[:], d["cv_ln_gT"][i], [P.B("cv_ln_gT")], [blg])
        kb.ld(lb[:], d["cv_ln_bT"][i], [P.B("cv_ln_bT")], [blb])
        wsrc = d["w_in"][i].rearrange("(k p) c -> p k c", p=128)
        for k in range(KC):
            kb.ld(W[:, k, :], wsrc[:, k, C_CV:C_CV + 1024], [P.B("w_in")], [bW], q="pool")
        kb.op("dve", lambda g: g.memset(glu[:], 0.0), [], [bglu])
        for gi, (t0, N) in enumerate(TGROUPS):
            h, bh = hTg[gi % 2]
            kb.ld(h[:, :, :N], d["hT_d"][:, :, t0:t0 + N], [P.B("hT_d")], [bh])
            off = LOFF + t0 if t0 < SEQ else COFF
            for cc in range(4):
                pa, bpa = P.psf[0]
                pb, bpb = P.psf[1]
                proj_fm(P, pa, bpa, W, bW, cc * 128, 128, h, bh, N)
                proj_fm(P, pb, bpb, W, bW, 512 + cc * 128, 128, h, bh, N)
                kb.act(sg[:, :N], pb[:, :N], AF.Sigmoid, [bpb], [bsg])
                kb.tt(glu[:, cc, off:off + N], pa[:, :N], sg[:, :N], ALU.mult, [bpa, bsg], [bglu])
        for cc in range(4):
            for j in range(31):
                kb.ts(dgm[:, cc, j, :], c["ident_b"][:], cw[:, cc, j:j + 1], ALU.mult, [P.B("k_ident_b"), bcw], [bdgm])
        it = 0
        for cc in range(4):
            for (toff, L, poff) in ((0, SEQ, LOFF), (SEQ, CTX, COFF)):
                G = min(L, 512)
                for g0 in range(0, L, G):
                    ps, bps = P.psf[2 + it % 4]
                    it += 1
                    for j in range(31):
                        a0 = poff - 15 + j + g0
                        kb.mm(ps[:, :G], dgm[:, cc, j, :], glu[:, cc, a0:a0 + G], j == 0, j == 30, [bdgm, bglu], [bps])
                    kb.act(uu[:, cc, toff + g0:toff + g0 + G], ps[:, :G], AF.Identity, [bps, bcb], [buus[cc]], bias=cb[:, cc:cc + 1])
        for gi, (t0, N) in enumerate(TGROUPS):
            s_ps, bs = P.psf[0]
            q_ps, bq = P.psf[1]
            for cc in range(4):
                kb.act(usq[:, cc, :N], uu[:, cc, t0:t0 + N], AF.Square, [buus[cc]], [busq])
            for cc in range(4):
                kb.mm(s_ps[:, :N], c["ones_f"][:, :], uu[:, cc, t0:t0 + N], cc == 0, cc == 3, [P.B("k_ones_f"), buus[cc]], [bs])
            for cc in range(4):
                kb.mm(q_ps[:, :N], c["ones_f"][:, :], usq[:, cc, :N], cc == 0, cc == 3, [P.B("k_ones_f"), busq], [bq])
            kb.ts(mean[:, :N], s_ps[:, :N], 1.0 / 512, ALU.mult, [bs], [bmean])
            kb.tt(var[:, :N], mean[:, :N], mean[:, :N], ALU.mult, [bmean], [bvar])
            kb.stt(var[:, :N], q_ps[:, :N], 1.0 / 512, var[:, :N], ALU.mult, ALU.subtract, [bq, bvar], [bvar])
            kb.act(var[:, :N], var[:, :N], AF.Ln, [bvar], [bvar], bias=EPS)
            kb.act(var[:, :N], var[:, :N], AF.Exp, [bvar], [bvar], scale=-0.5)
            for cc in range(4):
                kb.tt(y[:, :N], uu[:, cc, t0:t0 + N], mean[:, :N], ALU.subtract, [buus[cc], bmean], [by])
                kb.tt(y[:, :N], y[:, :N], var[:, :N], ALU.mult, [by, bvar], [by])
                kb.act(ob[:, :N], y[:, :N], AF.Silu, [by, blg, blb], [bob], scale=lg[:, cc:cc + 1], bias=lb[:, cc:cc + 1])
                kb.ld(d["br"][3, cc * 128:(cc + 1) * 128, t0:t0 + N], ob[:, :N], [bob], [P.B("br")], q="act")
    kb.barrier()


def merge_stage(P, i):
    kb, d, c, nc = P.kb, P.din, P.c, P.nc
    kb.barrier()
    with contextlib.ExitStack() as ph:
        def sb(name, shape, dt):
            return ph.enter_context(nc.sbuf_tensor(f"g{i}_{name}", list(shape), dt)), Buf(name)
        hT, bh = sb("hT", [128, KC, NT], BF16)
        brs, bbr = sb("brs", [128, 16, NT], BF16)
        Wg = [sb(f"Wg{j}", [128, KC, 512], BF16) for j in range(2)]
        wbr = [sb(f"wbr{j}", [128, 16, 128], BF16) for j in range(2)]
        sgt = [sb(f"sgt{j}", [128, 512], F32) for j in range(2)]
        macc, bmacc = sb("macc", [128, 512], F32)
        tmp, btmp = sb("tmp", [128, 512], F32)
        mTk = [sb(f"mTk{j}", [128, 512], BF16) for j in range(2)]
        bhg = [Buf(f"hTg{g}") for g in range(len(TGROUPS))]
        bbg = [Buf(f"brg{g}") for g in range(len(TGROUPS))]
        for gi, (t0, N) in enumerate(TGROUPS):
            kb.ld(hT[:, :, t0:t0 + N], d["hT_d"][:, :, t0:t0 + N], [P.B("hT_d")], [bhg[gi]])
            for n in range(4):
                kb.ld(brs[:, n * 4:(n + 1) * 4, t0:t0 + N], d["br"][n, :, t0:t0 + N].rearrange("(wc p) t -> p wc t", p=128), [P.B("br")], [bbg[gi]])
        wgsrc = d["w_gateR"][i].rearrange("(kc p) k c -> p kc k c", p=128)
        it = 0
        for k in range(KC):
            W_, bW_ = Wg[k % 2]
            wb_, bwb_ = wbr[k % 2]
            kb.ld(W_[:], wgsrc[:, :, k, :], [P.B("w_gateR")], [bW_], q="pool")
            kb.ld(wb_[:], d["w_brR"][i, k], [P.B("w_brR")], [bwb_], q="pool")
            for gi, (t0, N) in enumerate(TGROUPS):
                m_, bm_ = mTk[it % 2]
                it += 1
                for n in range(4):
                    pg, bpg = P.psf[n % 2]
                    up, bup = P.psf[2 + n % 2]
                    sg_, bsg_ = sgt[n % 2]
                    for kc in range(KC):
                        kb.mm(pg[:, :N], W_[:, kc, n * 128:(n + 1) * 128], hT[:, kc, t0:t0 + N], kc == 0, kc == KC - 1, [bW_, bhg[gi]], [bpg])
                    for wc in range(4):
                        kb.mm(up[:, :N], wb_[:, n * 4 + wc, :], brs[:, n * 4 + wc, t0:t0 + N], wc == 0, wc == 3, [bwb_, bbg[gi]], [bup])
                    kb.act(sg_[:, :N], pg[:, :N], AF.Sigmoid, [bpg], [bsg_])
                    if n == 0:
                        kb.tt(macc[:, :N], sg_[:, :N], up[:, :N], ALU.mult, [bsg_, bup], [bmacc])
                    else:
                        kb.tt(tmp[:, :N], sg_[:, :N], up[:, :N], ALU.mult, [bsg_, bup], [btmp])
                        if n < 3:
                            kb.tt(macc[:, :N], macc[:, :N], tmp[:, :N], ALU.add, [bmacc, btmp], [bmacc])
                        else:
                            kb.tt(m_[:, :N], macc[:, :N], tmp[:, :N], ALU.add, [bmacc, btmp], [bm_])
                kb.ld(d["mT_d"][:, k, t0:t0 + N], m_[:, :N], [bm_], [P.B("mT_d")], q="act")
    kb.barrier()
    with contextlib.ExitStack() as ph:
        def sb(name, shape, dt):
            return ph.enter_context(nc.sbuf_tensor(f"o{i}_{name}", list(shape), dt)), Buf(name)
        wo, bwo = sb("wo", [128, KC, D], BF16)
        g1bc, bg1 = sb("g1bc", [128, 2, D], F32)
        mTg = [sb(f"mTg{j}", [128, KC, 512], BF16) for j in range(2)]
        xt = [sb(f"xt{j}", [128, D], F32) for j in range(2)]
        xo = [sb(f"xo{j}", [128, D], F32) for j in range(2)]
        wosrc = d["w_out"][i].rearrange("(kc p) c -> p kc c", p=128)
        for dg in range(4):
            kb.ld(wo[:, :, dg * 512:(dg + 1) * 512], wosrc[:, :, dg * 512:(dg + 1) * 512], [P.B("w_out")], [bwo], q="pool")
        for j in range(2):
            kb.ld(g1bc[:, j, :], d["modrow"][j:j + 1, 2 * D:3 * D].partition_broadcast(128), [P.B("modrow")], [bg1])
        for gi, (t0, N) in enumerate(TGROUPS):
            j = 0 if t0 < SEQ else 1
            m_, bm_ = mTg[gi % 2]
            kb.ld(m_[:, :, :N], d["mT_d"][:, :, t0:t0 + N], [P.B("mT_d")], [bm_])
            for tl in range(N // 128):
                r0 = t0 + tl * 128
                x_, bx_ = xt[tl % 2]
                o_, bo_ = xo[tl % 2]
                kb.ld(x_[:], d["xres"][r0:r0 + 128, :], [P.B("xres")], [bx_])
                for dg in range(4):
                    ps, bps = P.psf[dg % 4]
                    for k in range(KC):
                        kb.mm(ps[:, :], m_[:, k, tl * 128:(tl + 1) * 128], wo[:, k, dg * 512:(dg + 1) * 512], k == 0, k == KC - 1, [bm_, bwo], [bps])
                    kb.tt(o_[:, dg * 512:(dg + 1) * 512], ps[:, :], g1bc[:, j, dg * 512:(dg + 1) * 512], ALU.mult, [bps, bg1], [bo_])
                kb.tt(o_[:], o_[:], x_[:], ALU.add, [bo_, bx_], [bo_])
                kb.ld(d["xres"][r0:r0 + 128, :], o_[:], [bo_], [P.B("xres")], q="act")
    kb.barrier()


def moe_stage(P, i):
    kb, d, c, nc = P.kb, P.din, P.c, P.nc
    kb.barrier()
    NS = 288
    with contextlib.ExitStack() as ph:
        def sb(name, shape, dt):
            return ph.enter_context(nc.sbuf_tensor(f"e{i}_{name}", list(shape), dt)), Buf(name)
        wr, bwr = sb("wr", [128, KC, NE], BF16)
        affT, baffT = sb("affT", [16, NT], F32)
        kb.ld(wr[:], d["w_router"][i].rearrange("(k p) e -> p k e", p=128), [P.B("w_router")], [bwr], q="pool")
        w13 = [sb(f"w13_{j}", [128, KC, 2, 512], BF16) for j in range(2)]
        w2b = [sb(f"w2_{j}", [128, 8, 1024], BF16) for j in range(2)]

        def load13(e, fh):
            w_, bw_ = w13[fh]
            w1src = d["w1"][i, e].rearrange("(k p) f -> p k f", p=128)
            w3src = d["w3"][i, e].rearrange("(k p) f -> p k f", p=128)
            kb.ld(w_[:, :, 0, :], w1src[:, :, fh * 512:(fh + 1) * 512], [P.B("w1")], [bw_], q="pool")
            kb.ld(w_[:, :, 1, :], w3src[:, :, fh * 512:(fh + 1) * 512], [P.B("w3")], [bw_], q="pool")

        def load2(e, dh):
            w_, bw_ = w2b[dh]
            w2src = d["w2"][i, e].rearrange("(k p) c -> p k c", p=128)
            kb.ld(w_[:], w2src[:, :, dh * 1024:(dh + 1) * 1024], [P.B("w2")], [bw_], q="pool")

        load13(0, 0)
        load13(0, 1)
        load2(0, 0)
        load2(0, 1)
        with contextlib.ExitStack() as ph1:
            def sb1(name, shape, dt):
                return ph1.enter_context(nc.sbuf_tensor(f"e{i}_{name}", list(shape), dt)), Buf(name)
            sm = [sb1(f"sm{j}", [128, 4], F32) for j in range(2)]
            ee = [sb1(f"ee{j}", [128, NE], F32) for j in range(2)]

            def consume(t0, ntok, ht, bht):
                for tl in range(ntok // 128):
                    tt = t0 // 128 + tl
                    p = tt % 2
                    lg, blg = P.psf[p]
                    for k in range(KC):
                        kb.mm(lg[:, 0:NE], ht[:, k, tl * 128:(tl + 1) * 128], wr[:, k, :], k == 0, k == KC - 1, [bht, bwr], [blg])
                    s_, bs_ = sm[p]
                    e_, be_ = ee[p]
                    kb.op("dve", lambda g: g.reduce_max(out=s_[:, 0:1], in_=lg[:, 0:NE], axis=AX.X), [blg], [bs_])
                    kb.ts(s_[:, 1:2], s_[:, 0:1], -1.0, ALU.mult, [bs_], [bs_])
                    kb.op("dve", lambda g: g.memset(s_[:, 2:3], 0.0), [], [bs_])
                    kb.act(e_[:], lg[:, 0:NE], AF.Exp, [blg, bs_], [be_, bs_], bias=s_[:, 1:2], accum=s_[:, 2:3])
                    kb.op("dve", lambda g: g.reciprocal(out=s_[:, 3:4], in_=s_[:, 2:3]), [bs_], [bs_])
                    kb.ts(e_[:], e_[:], s_[:, 3:4], ALU.mult, [be_, bs_], [be_])
                    tp, btp = P.psf[2 + p]
                    kb.tr(tp[0:16, 0:128], e_[:, :], c["ident_f"][:, :], [be_, P.B("k_ident_f")], [btp])
                    kb.cp(affT[:, tt * 128:(tt + 1) * 128], tp[0:16, 0:128], [btp], [baffT], e="act")
            norm_stage(P, i, "g_ffnT", 2, 3, consume, xs_dram="xs2")
        kb.barrier()
        if "aff" in P.dbg:
            P.dump(f"aff{i}", affT[:], baffT, [16, NT])
        wa, bwa = sb("wa", [16, SEQ], F32)
        wb, bwb = sb("wb", [16, SEQ], F32)
        vals, bvals = sb("vals", [16, NS], F32)
        idxu, bidxu = sb("idxu", [16, NS], U32)
        idxf, bidxf = sb("idxf", [16, NS], F32)
        gT, bgT = sb("gT", [128, 3, NE], F32)
        idxT, bidxT = sb("idxT", [128, 3, NE], I32)
        for (toff, L, rounds, soff) in ((0, SEQ, 32, 0), (SEQ, CTX, 4, 256)):
            cur, bcur = affT[:, toff:toff + L], baffT
            for r in range(rounds):
                v8 = vals[:, soff + r * 8:soff + (r + 1) * 8]
                kb.op("dve", lambda g: g.max(out=v8, in_=cur), [bcur], [bvals])
                kb.op("dve", lambda g: g.max_index(out=idxu[:, soff + r * 8:soff + (r + 1) * 8], in_max=v8, in_values=cur), [bcur, bvals], [bidxu])
                nxt, bnxt = (wa, bwa) if r % 2 == 0 else (wb, bwb)
                if r < rounds - 1:
                    kb.op("dve", lambda g: g.match_replace(out=nxt[:, :L], in_to_replace=v8, in_values=cur, imm_value=-1.0), [bcur, bvals], [bnxt])
                    cur, bcur = nxt[:, :L], bnxt
        kb.cp(idxf[:], idxu[:], [bidxu], [bidxf])
        kb.ts(idxf[:, 256:NS], idxf[:, 256:NS], float(SEQ), ALU.add, [bidxf], [bidxf])
        kb.ts(idxf[:], idxf[:], 0.0, ALU.max, [bidxf], [bidxf], s2=float(NT - 1), op1=ALU.min)
        for ct, (c0, n) in enumerate(((0, 128), (128, 128), (256, 32))):
            tp, btp = P.psf[ct % 2]
            kb.tr(tp[0:n, 0:16], vals[:, c0:c0 + n], c["ident_f"][0:16, 0:16], [bvals, P.B("k_ident_f")], [btp])
            kb.cp(gT[0:n, ct, :], tp[0:n, 0:16], [btp], [bgT])
            tp2, btp2 = P.psf[2 + ct % 2]
            kb.tr(tp2[0:n, 0:16], idxf[:, c0:c0 + n], c["ident_f"][0:16, 0:16], [bidxf, P.B("k_ident_f")], [btp2])
            kb.cp(idxT[0:n, ct, :], tp2[0:n, 0:16], [btp2], [bidxT])
        if "idx" in P.dbg:
            P.dump(f"idx{i}", idxf[:], bidxf, [16, NS])
            P.dump(f"vals{i}", vals[:], bvals, [16, NS])
            P.dump(f"idxT{i}", idxT[:], bidxT, [128, 3, NE], I32)
        g2bc, bg2 = sb("g2bc", [128, 2, D], F32)
        for j in range(2):
            kb.ld(g2bc[:, j, :], d["modrow"][j:j + 1, 5 * D:6 * D].partition_broadcast(128), [P.B("modrow")], [bg2])
        xg = [sb(f"xg{j}", [128, 3, D], BF16) for j in range(1)]
        xgT, bxgT = sb("xgT", [128, KC, NS], BF16)
        hidT, bhid = sb("hidT", [128, 8, NS], BF16)
        sl, bsl = sb("sl", [128, NS], F32)
        yo, byo = sb("yo", [128, 3, D], F32)
        A, bA, modT, bm = c["A"], P.B("k_A"), c["modT"], P.B("k_modT")
        CT = ((0, 128, 0), (128, 128, 0), (256, 32, 1))
        for q in range(4):
            kb.ld(d[f"macc{q}"], d["xres"][:, q * 512:(q + 1) * 512], [P.B("xres")], [P.B(f"macc{q}")])
        x_, bx_ = xg[0]

        def gather(e):
            for ct, (c0, n, j) in enumerate(CT):
                kb.dma("pool", lambda g, ct=ct, n=n: g.indirect_dma_start(
                    out=x_[0:n, ct, :], out_offset=None, in_=d["xs2"][:, :],
                    in_offset=bass.IndirectOffsetOnAxis(ap=idxT[0:n, ct, e:e + 1], axis=0)), [P.B("xs2"), bidxT], [bx_])

        gather(0)
        for e in range(NE):
            for ct, (c0, n, j) in enumerate(CT):
                for k in range(KC):
                    ps, bps = P.psb[k % 2]
                    sl_ = ps[:, (k // 2 % 8) * 128:(k // 2 % 8) * 128 + n]
                    kb.tr(sl_, x_[0:n, ct, k * 128:(k + 1) * 128], c["ident_b"][0:n, 0:n], [bx_, P.B("k_ident_b")], [bps])
                    kb.act(xgT[:, k, c0:c0 + n], sl_, AF.Identity, [bps, bA, bm], [bxgT], scale=A[:, j, k:k + 1], bias=modT[:, 2, k, j:j + 1])
            if e + 1 < NE:
                gather(e + 1)
            for fh in range(2):
                w_, bw_ = w13[fh]
                for fi in range(4):
                    h1, bh1 = P.psf[fi % 2]
                    h3, bh3 = P.psf[2 + fi % 2]
                    for k in range(KC):
                        kb.mm(h1[:, :NS], w_[:, k, 0, fi * 128:(fi + 1) * 128], xgT[:, k, :], k == 0, k == KC - 1, [bw_, bxgT], [bh1])
                    for k in range(KC):
                        kb.mm(h3[:, :NS], w_[:, k, 1, fi * 128:(fi + 1) * 128], xgT[:, k, :], k == 0, k == KC - 1, [bw_, bxgT], [bh3])
                    kb.act(sl[:], h1[:, :NS], AF.Silu, [bh1], [bsl])
                    kb.tt(hidT[:, fh * 4 + fi, :], sl[:], h3[:, :NS], ALU.mult, [bsl, bh3], [bhid])
                if e + 1 < NE:
                    load13(e + 1, fh)
            for dh in range(2):
                w_, bw_ = w2b[dh]
                for ct, (c0, n, j) in enumerate(CT):
                    for dgi in range(2):
                        y, by = P.psf[4 + dgi]
                        for f in range(8):
                            kb.mm(y[0:n, :], hidT[:, f, c0:c0 + n], w_[:, f, dgi * 512:(dgi + 1) * 512], f == 0, f == 7, [bhid, bw_], [by])
                        col = dh * 1024 + dgi * 512
                        kb.stt(yo[0:n, ct, col:col + 512], y[0:n, :], gT[0:n, ct, e:e + 1], g2bc[0:n, j, col:col + 512], ALU.mult, ALU.mult, [by, bgT, bg2], [byo])
                if e + 1 < NE:
                    load2(e + 1, dh)
            for ct, (c0, n, j) in enumerate(CT):
                for q in range(4):
                    kb.dma("pool", lambda g, ct=ct, n=n, q=q: g.indirect_dma_start(
                        out=d[f"macc{q}"][:, :], out_offset=bass.IndirectOffsetOnAxis(ap=idxT[0:n, ct, e:e + 1], axis=0),
                        in_=yo[0:n, ct, q * 512:(q + 1) * 512], in_offset=None, compute_op=ALU.add), [byo, bidxT], [P.B(f"macc{q}")])
        for q in range(4):
            kb.ld(d["xres"][:, q * 512:(q + 1) * 512], d[f"macc{q}"], [P.B(f"macc{q}")], [P.B("xres")])
    kb.barrier()


def final_stage(P):
    kb, d, c, nc = P.kb, P.din, P.c, P.nc
    kb.barrier()
    with contextlib.ExitStack() as ph:
        def sb(name, shape, dt):
            return ph.enter_context(nc.sbuf_tensor(f"z_{name}", list(shape), dt)), Buf(name)
        gbc, bg = sb("gbc", [128, D], F32)
        kb.ld(gbc[:], d["g_final"][0:1, :].partition_broadcast(128), [P.B("g_final")], [bg])
        xt = [sb(f"xt{j}", [128, D], F32) for j in range(2)]
        ot = [sb(f"ot{j}", [128, D], F32) for j in range(2)]
        junk, bj = sb("junk", [128, D], BF16)
        st = [sb(f"st{j}", [128, 4], F32) for j in range(2)]
        for tt in range(SEQ // 128):
            p = tt % 2
            x_, bx_ = xt[p]
            o_, bo_ = ot[p]
            s_, bs_ = st[p]
            kb.ld(x_[:], d["xres"][tt * 128:(tt + 1) * 128, :], [P.B("xres")], [bx_])
            kb.op("dve", lambda g: g.memset(s_[:], 0.0), [], [bs_])
            kb.act(junk[:], x_[:], AF.Square, [bx_, bs_], [bj, bs_], accum=s_[:, 0:1])
            kb.act(s_[:, 1:2], s_[:, 0:1], AF.Sqrt, [bs_], [bs_], scale=1.0 / D, bias=EPS)
            kb.op("dve", lambda g: g.reciprocal(out=s_[:, 2:3], in_=s_[:, 1:2]), [bs_], [bs_])
            kb.stt(o_[:], x_[:], s_[:, 2:3], gbc[:], ALU.mult, ALU.mult, [bx_, bs_, bg], [bo_])
            kb.ld(d["out"][tt * 128:(tt + 1) * 128, :], o_[:], [bo_], [P.B("out")], q="act")
    kb.barrier()


def build_program(nlayers=DEPTH, dbg=None, upto=None):
    P = Prog(nlayers=nlayers, dbg=dbg)
    declare_io(P)
    load_consts(P)
    for i in range(nlayers):
        stage_mod(P, i)
        norm1_stage(P, i)
        attn_stage(P, i)
        hy_filters(P, i)
        hy_stage(P, i)
        conf_stage(P, i)
        if upto == "mixers":
            break
        merge_stage(P, i)
        if P.dbg.get("xmid") and i == 0:
            o = P.outp("dbg_xmid", [NT, D])
            P.kb.ld(o, P.din["xres"], [P.B("xres")], [P.B("dbg_xmid")])
        if upto == "merge":
            break
        moe_stage(P, i)
        if P.dbg.get("xmid") and i == 0:
            o = P.outp("dbg_xl0", [NT, D])
            P.kb.ld(o, P.din["xres"], [P.B("xres")], [P.B("dbg_xl0")])
    final_stage(P)
    return P


_PROG = {}


def kernel(**inputs):
    if "p" not in _PROG:
        _PROG["p"] = build_program(DEPTH)
    P = _PROG["p"]
    shared = prep_shared(inputs, DEPTH)
    in_maps = []
    for core in range(8):
        m = dict(shared)
        m.update(prep_core(inputs, core % 4))
        in_maps.append({k: v for k, v in m.items() if k in P.din})
    res = run_bass_kernel_spmd(P.nc, in_maps, core_ids=list(range(8)))
    out = np.stack([np.asarray(res.results[b]["out"], dtype=np.float32) for b in range(4)], axis=0)
    return out
```

```python
import contextlib
import math
import numpy as np
import concourse.bass as bass
import concourse.mybir as mybir
from concourse.bass_utils import run_bass_kernel_spmd

F32 = mybir.dt.float32
BF16 = mybir.dt.bfloat16
I32 = mybir.dt.int32
U32 = mybir.dt.uint32
AF = mybir.ActivationFunctionType
ALU = mybir.AluOpType
AX = mybir.AxisListType

D = 2048
SEQ = 2048
CTX = 256
NT = SEQ + CTX
NTILE = NT // 128
DEPTH = 4
KC = D // 128
IN_COLS = 12448
EPS = 1e-6
GRID_W = 64
NE = 16
FF = 1024
C_K, C_V, C_KVA, C_KPE, C_Q, C_QA, C_HY, C_CV, C_G = 0, 256, 512, 768, 800, 1312, 1696, 3232, 4256
TGROUPS = [(0, 512), (512, 512), (1024, 512), (1536, 512), (2048, 256)]


class Buf:
    __slots__ = ("name", "w", "r")

    def __init__(self, name):
        self.name = name
        self.w = None
        self.r = {}


class KB:
    ND = 8

    def __init__(self, nc, same_sync=True):
        self.nc = nc
        self.es = contextlib.ExitStack()
        self.eng = {"pe": nc.tensor, "act": nc.scalar, "dve": nc.vector, "pool": nc.gpsimd, "sp": nc.sync}
        self.sems = []
        self.csem = {}
        self.ccnt = {}
        for e in ("pe", "act", "dve", "pool"):
            self.csem[e] = self._sem("c_" + e)
            self.ccnt[e] = 0
        self.seen = {e: {} for e in self.eng}
        self.dq = {}
        self.dqn = {}
        self.dqcnt = {}
        for q in ("sp", "pool", "act"):
            self.dq[q] = [self._sem(f"d_{q}{i}") for i in range(self.ND)]
            self.dqn[q] = 0
            self.dqcnt[q] = [0] * self.ND
        self.same_sync = same_sync
        self.ninst = 0

    def _sem(self, name):
        h = self.es.enter_context(self.nc.semaphore(name))
        self.sems.append(h)
        return len(self.sems) - 1

    def sb(self, name, shape, dt):
        return self.es.enter_context(self.nc.sbuf_tensor(name, list(shape), dt))

    def _wait(self, e, ev):
        if ev is None:
            return
        si, val, src = ev
        if src == e and (e == "pe" or not self.same_sync):
            return
        if self.seen[e].get(si, 0) >= val:
            return
        self.eng[e].wait_ge(self.sems[si], val)
        self.seen[e][si] = val

    def _deps(self, e, reads, writes):
        for b in reads:
            self._wait(e, b.w)
        for b in writes:
            self._wait(e, b.w)
            for si, (val, src) in list(b.r.items()):
                self._wait(e, (si, val, src))

    def _commit(self, ev, reads, writes):
        for b in writes:
            b.w = ev
            b.r = {}
        for b in reads:
            si, val, src = ev
            b.r[si] = (val, src)

    def op(self, e, fn, reads=(), writes=()):
        self._deps(e, reads, writes)
        ins = fn(self.eng[e])
        self.ccnt[e] += 1
        ins.then_inc(self.sems[self.csem[e]], 1)
        ev = (self.csem[e], self.ccnt[e], e)
        self._commit(ev, reads, writes)
        self.ninst += 1
        return ev

    def dma(self, q, fn, reads=(), writes=()):
        e = q
        self._deps(e, reads, writes)
        slot = self.dqn[q] % self.ND
        self.dqn[q] += 1
        si = self.dq[q][slot]
        prev = 16 * self.dqcnt[q][slot]
        if prev > 0:
            self._wait(e, (si, prev, "dma"))
        ins = fn(self.eng[e])
        ins.then_inc(self.sems[si], 16)
        self.dqcnt[q][slot] += 1
        ev = (si, 16 * self.dqcnt[q][slot], "dma")
        self._commit(ev, reads, writes)
        self.ninst += 1
        return ev

    def barrier(self):
        evs = []
        for e in ("pe", "act", "dve", "pool"):
            if self.ccnt[e]:
                evs.append((self.csem[e], self.ccnt[e], "x"))
        for q in self.dq:
            for slot in range(self.ND):
                if self.dqcnt[q][slot]:
                    evs.append((self.dq[q][slot], 16 * self.dqcnt[q][slot], "x"))
        for e in self.eng:
            for ev in evs:
                self._wait(e, ev)

    def mm(self, ps, lhsT, rhs, start, stop, r, w):
        return self.op("pe", lambda g: g.matmul(ps, lhsT=lhsT, rhs=rhs, start=start, stop=stop), r, w)

    def tr(self, ps, in_, ident, r, w):
        return self.op("pe", lambda g: g.transpose(ps, in_, ident), r, w)

    def act(self, out, in_, func, r, w, scale=1.0, bias=0.0, accum=None, e="act"):
        if accum is None:
            return self.op(e, lambda g: g.activation(out=out, in_=in_, func=func, bias=bias, scale=scale), r, w)
        return self.op(e, lambda g: g.activation(out=out, in_=in_, func=func, bias=bias, scale=scale, accum_out=accum), r, w)

    def tt(self, out, a, b, op, r, w, e="dve"):
        return self.op(e, lambda g: g.tensor_tensor(out=out, in0=a, in1=b, op=op), r, w)

    def ts(self, out, a, s1, op0, r, w, s2=None, op1=None, e="dve"):
        if op1 is None:
            return self.op(e, lambda g: g.tensor_scalar(out=out, in0=a, scalar1=s1, scalar2=None, op0=op0), r, w)
        return self.op(e, lambda g: g.tensor_scalar(out=out, in0=a, scalar1=s1, scalar2=s2, op0=op0, op1=op1), r, w)

    def stt(self, out, a, s, b, op0, op1, r, w, e="dve"):
        return self.op(e, lambda g: g.scalar_tensor_tensor(out=out, in0=a, scalar=s, in1=b, op0=op0, op1=op1), r, w)

    def cp(self, out, in_, r, w, e="dve"):
        if e == "act":
            return self.op(e, lambda g: g.activation(out=out, in_=in_, func=AF.Copy), r, w)
        return self.op(e, lambda g: g.tensor_copy(out=out, in_=in_), r, w)

    def ld(self, out, in_, r, w, q="sp"):
        return self.dma(q, lambda g: g.dma_start(out=out, in_=in_), r, w)


SAME_SYNC = True


class Prog:
    def __init__(self, nlayers=DEPTH, dbg=None, same_sync=None):
        same_sync = SAME_SYNC if same_sync is None else same_sync
        self.dbg = dbg or {}
        self.nlayers = nlayers
        nc = bass.Bass("TRN2", target_bir_lowering=False)
        self.nc = nc
        self.kb = KB(nc, same_sync=same_sync)
        self.din = {}
        self.bufs = {}
        self.dbg_out = {}

    def inp(self, name, shape, dt=F32):
        t = self.nc.dram_tensor(name, list(shape), dt, kind="ExternalInput").ap()
        self.din[name] = t
        self.bufs[name] = Buf(name)
        return t

    def scratch(self, name, shape, dt):
        t = self.nc.dram_tensor(name, list(shape), dt, kind="Internal").ap()
        self.din[name] = t
        self.bufs[name] = Buf(name)
        return t

    def outp(self, name, shape, dt=F32):
        t = self.nc.dram_tensor(name, list(shape), dt, kind="ExternalOutput").ap()
        self.din[name] = t
        self.bufs[name] = Buf(name)
        return t

    def B(self, name):
        if name not in self.bufs:
            self.bufs[name] = Buf(name)
        return self.bufs[name]

    def dump(self, name, sb_ap, buf, shape, dt=F32):
        o = self.outp("dbg_" + name, shape, dt)
        self.kb.ld(o, sb_ap, [buf], [self.B("dbg_" + name)], q="sp")
        self.dbg_out[name] = "dbg_" + name


def declare_io(P):
    L = P.nlayers
    P.inp("xin", [NT, D])
    P.inp("cT", [128, KC, 2])
    P.inp("w_mod", [L, D, 6 * D])
    P.inp("b_mod2", [L, 2, 6 * D])
    P.inp("g_mixT", [L, 128, KC])
    P.inp("g_ffnT", [L, 128, KC])
    P.inp("w_in", [L, D, IN_COLS])
    P.inp("gqa_q_gain", [L, 128, 1])
    P.inp("gqa_k_gain", [L, 128, 1])
    P.inp("mla_q_a_gainT", [L, 128, 3])
    P.inp("mla_kv_a_gainT", [L, 128, 2])
    P.inp("mla_q_b", [L, 384, 384])
    P.inp("mla_kv_b", [L, 256, 768])
    P.inp("hy_conv_wT", [L, 128, 12, 3])
    P.inp("hy_conv_bT", [L, 128, 12])
    P.inp("hf_w1", [L, 33, 64])
    P.inp("hf_b1T", [L, 64, 1])
    P.inp("hf_w2", [L, 64, 64])
    P.inp("hf_b2T", [L, 64, 1])
    P.inp("hf_w3", [L, 64, 2048])
    P.inp("hf_freqT", [L, 64, 1])
    P.inp("hf_log_rate", [L, 1, 2048])
    P.inp("hy_biasT", [L, 128, 2, 4])
    P.inp("cv_wT", [L, 128, 4, 31])
    P.inp("cv_bT", [L, 128, 4])
    P.inp("cv_ln_gT", [L, 128, 4])
    P.inp("cv_ln_bT", [L, 128, 4])
    P.inp("w_brR", [L, KC, 128, 16, 128])
    P.inp("w_gateR", [L, D, KC, 512])
    P.inp("w_out", [L, D, D])
    P.inp("w_router", [L, D, NE])
    P.inp("w1", [L, NE, D, FF])
    P.inp("w3", [L, NE, D, FF])
    P.inp("w2", [L, NE, FF, D])
    P.inp("g_final", [1, D])
    P.inp("c_ident", [128, 128])
    P.inp("c_r128", [128, 128])
    P.inp("c_r32", [32, 32])
    P.inp("c_cos128", [128, NT])
    P.inp("c_sin128", [128, NT])
    P.inp("c_cos32", [32, NT])
    P.inp("c_sin32", [32, NT])
    P.inp("c_fc", [SEQ, SEQ])
    P.inp("c_fs", [SEQ, SEQ])
    P.inp("c_gc", [SEQ, SEQ])
    P.inp("c_gs", [SEQ, SEQ])
    P.inp("c_fc_c", [CTX, CTX])
    P.inp("c_fs_c", [CTX, CTX])
    P.inp("c_gc_c", [CTX, CTX])
    P.inp("c_gs_c", [CTX, CTX])
    P.inp("c_feat", [33, SEQ])
    P.inp("c_feat_c", [33, CTX])
    P.inp("c_t01", [128, 18])
    P.inp("c_iota", [128, 1])
    P.scratch("xres", [NT, D], F32)
    P.scratch("modrow", [2, 6 * D], F32)
    P.scratch("br", [4, 512, NT], BF16)
    P.scratch("xs2", [NT, D], BF16)
    P.scratch("hT_d", [128, KC, NT], BF16)
    P.scratch("mT_d", [128, KC, NT], BF16)
    for q in range(4):
        P.scratch(f"macc{q}", [NT, 512], F32)
    P.scratch("kf", [2, 16, 128, 2, 512], F32)
    P.scratch("kf_c", [2, 2, 128, 2, 512], F32)
    P.outp("out", [SEQ, D])


def load_consts(P):
    kb = P.kb
    d = P.din
    c = {}
    P.c = c

    def mk(name, shape, dt, src=None, q="sp"):
        t = kb.sb("k_" + name, shape, dt)
        c[name] = t
        b = P.B("k_" + name)
        if src is not None:
            kb.ld(t[:], d[src], [P.B(src)], [b], q=q)
        return t, b

    mk("ident_f", [128, 128], F32, "c_ident")
    mk("ident_b", [128, 128], BF16, "c_ident", q="pool")
    mk("r128", [128, 128], BF16, "c_r128", q="pool")
    mk("r32", [32, 32], BF16, "c_r32", q="pool")
    t, b = mk("ones_b", [128, 128], BF16)
    kb.op("dve", lambda g: g.memset(t[:], 1.0), [], [b])
    t2, b2 = mk("ones_f", [128, 128], F32)
    kb.op("dve", lambda g: g.memset(t2[:], 1.0), [], [b2])
    P.psf = []
    for i in range(6):
        P.psf.append((kb.es.enter_context(P.nc.psum_tensor(f"psf{i}", [128, 512], F32)), P.B(f"psf{i}")))
    P.psb = []
    for i in range(2):
        P.psb.append((kb.es.enter_context(P.nc.psum_tensor(f"psb{i}", [128, 1024], BF16)), P.B(f"psb{i}")))
    mk("modT", [128, 4, KC, 2], F32)
    mk("A", [128, 2, KC], F32)
    mk("gT", [128, KC], F32)
    kb.ld(d["xres"], d["xin"], [P.B("xin")], [P.B("xres")], q="sp")


def stage_mod(P, i):
    kb, d, c, nc = P.kb, P.din, P.c, P.nc
    kb.barrier()
    with contextlib.ExitStack() as ph:
        def sb(name, shape, dt):
            return ph.enter_context(nc.sbuf_tensor(f"m{i}_{name}", list(shape), dt))
        cT = sb("cT", [128, KC, 2], F32); bcT = Buf("cT")
        scT = sb("scT", [128, KC, 2], F32); bsc = Buf("scT")
        row = sb("row", [2, 6 * D], F32); brow = Buf("row")
        wt = [sb(f"wt{j}", [128, KC, 512], F32) for j in range(2)]
        bwt = [Buf("wt0"), Buf("wt1")]
        kb.ld(cT[:], d["cT"], [P.B("cT")], [bcT])
        kb.act(scT[:], cT[:], AF.Silu, [bcT], [bsc])
        kb.ld(row[:], d["b_mod2"][i], [P.B("b_mod2")], [brow])
        wsrc = d["w_mod"][i].rearrange("(k p) c -> p k c", p=128)
        for cg in range(24):
            w, bw = wt[cg % 2], bwt[cg % 2]
            kb.ld(w[:], wsrc[:, :, cg * 512:(cg + 1) * 512], [P.B("w_mod")], [bw])
            ps, bps = P.psf[cg % 2]
            for k in range(KC):
                kb.mm(ps[0:2, :], scT[:, k, :], w[:, k, :], k == 0, k == KC - 1, [bsc, bw], [bps])
            kb.tt(row[:, cg * 512:(cg + 1) * 512], ps[0:2, :], row[:, cg * 512:(cg + 1) * 512], ALU.add, [bps, brow], [brow])
        kb.ld(d["modrow"], row[:], [brow], [P.B("modrow")])
        ps, bps = P.psf[2]
        for vi, v in enumerate((0, 1, 3, 4)):
            for k in range(KC):
                col = (vi * KC + k) * 2
                kb.tr(ps[:, col:col + 2], row[0:2, v * D + k * 128: v * D + (k + 1) * 128], c["ident_f"][0:2, 0:2], [brow, P.B("k_ident_f")], [bps])
        kb.cp(c["modT"][:].rearrange("p v k j -> p (v k j)"), ps[:, 0:128], [bps], [P.B("k_modT")])
    kb.barrier()


def norm_stage(P, i, gname, vsh, vsc, consume, xs_dram=None):
    kb, d, c, nc = P.kb, P.din, P.c, P.nc
    with contextlib.ExitStack() as ph:
        def sb(name, shape, dt):
            return ph.enter_context(nc.sbuf_tensor(f"n{i}{gname}_{name}", list(shape), dt))
        gT, bg = c["gT"], P.B("k_gT")
        A, bA = c["A"], P.B("k_A")
        modT, bm = c["modT"], P.B("k_modT")
        kb.ld(gT[:], d[gname][i], [P.B(gname)], [bg])
        for j in range(2):
            kb.tt(A[:, j, :], gT[:], modT[:, vsc, :, j], ALU.mult, [bg, bm], [bA])
            kb.tt(A[:, j, :], A[:, j, :], gT[:], ALU.add, [bA, bg], [bA])
        xt = [sb(f"xt{j}", [128, D], F32) for j in range(2)]; bxt = [Buf("a"), Buf("b")]
        junk = sb("junk", [128, D], BF16); bj = Buf("junk")
        xs = [sb(f"xs{j}", [128, D], BF16) for j in range(8)]; bxs = [Buf(f"xs{j}") for j in range(8)]
        st = [sb(f"st{j}", [128, 4], F32) for j in range(2)]; bst = [Buf("a"), Buf("b")]
        hts = [sb(f"ht{j}", [128, KC, 512], BF16) for j in range(2)]; bht = [Buf("a"), Buf("b")]
        for gi, g4 in enumerate(range(0, NTILE, 4)):
            nt = min(4, NTILE - g4)
            j = 0 if g4 < 16 else 1
            for tl in range(nt):
                tt = g4 + tl
                p = tt % 2
                xi = (gi % 2) * 4 + tl
                kb.ld(xt[p][:], d["xres"][tt * 128:(tt + 1) * 128, :], [P.B("xres")], [bxt[p]])
                kb.op("dve", lambda g: g.memset(st[p][:], 0.0), [], [bst[p]])
                kb.act(junk[:], xt[p][:], AF.Square, [bxt[p], bst[p]], [bj, bst[p]], accum=st[p][:, 0:1])
                kb.act(st[p][:, 1:2], st[p][:, 0:1], AF.Sqrt, [bst[p]], [bst[p]], scale=1.0 / D, bias=EPS)
                kb.op("dve", lambda g: g.reciprocal(out=st[p][:, 2:3], in_=st[p][:, 1:2]), [bst[p]], [bst[p]])
                kb.ts(xs[xi][:], xt[p][:], st[p][:, 2:3], ALU.mult, [bxt[p], bst[p]], [bxs[xi]])
                if xs_dram is not None:
                    kb.ld(d[xs_dram][tt * 128:(tt + 1) * 128, :], xs[xi][:], [bxs[xi]], [P.B(xs_dram)], q="act")
            h_, bh_ = hts[gi % 2], bht[gi % 2]
            for k in range(KC):
                ps, bps = P.psb[k % 2]
                off = ((k // 2) % 2) * 512
                for tl in range(nt):
                    xi = (gi % 2) * 4 + tl
                    kb.tr(ps[:, off + tl * 128:off + (tl + 1) * 128], xs[xi][:, k * 128:(k + 1) * 128], c["ident_b"][:], [bxs[xi], P.B("k_ident_b")], [bps])
                kb.act(h_[:, k, :nt * 128], ps[:, off:off + nt * 128], AF.Identity, [bps, bA, bm], [bh_], scale=A[:, j, k:k + 1], bias=modT[:, vsh, k, j:j + 1])
            consume(g4 * 128, nt * 128, h_, bh_)


def _fm(v, k):
    sh = v.shape[:-1]
    return np.ascontiguousarray(np.swapaxes(v.reshape(*sh, k, 128), -1, -2))


_CONST_CACHE = {}


def const_tables():
    if _CONST_CACHE:
        return _CONST_CACHE
    c = {}
    c["c_ident"] = np.eye(128, dtype=np.float32)
    def perm(n):
        q = n // 4
        m = np.zeros((n, n), np.float32)
        for i in range(n):
            blk = i // q
            partner = i + q if blk % 2 == 0 else i - q
            m[partner, i] = 1.0
        return m
    c["c_r128"] = perm(128)
    c["c_r32"] = perm(32)
    rows = np.repeat(np.arange(SEQ // GRID_W), GRID_W).astype(np.float64)
    cols = np.tile(np.arange(GRID_W), SEQ // GRID_W).astype(np.float64)
    def rope_tab(hd):
        half = hd // 2
        m = half
        inv = 10000.0 ** (-np.arange(0, m, 2, dtype=np.float64) / m)
        cos = np.ones((hd, NT), np.float64)
        sin = np.zeros((hd, NT), np.float64)
        for p in range(hd):
            axis_pos = rows if p < half else cols
            q = p % half
            f = q % (m // 2)
            ang = axis_pos * inv[f]
            cos[p, :SEQ] = np.cos(ang)
            sgn = -1.0 if q < m // 2 else 1.0
            sin[p, :SEQ] = sgn * np.sin(ang)
        return cos.astype(np.float32), sin.astype(np.float32)
    c["c_cos128"], c["c_sin128"] = rope_tab(128)
    c["c_cos32"], c["c_sin32"] = rope_tab(32)
    def dft(L):
        N = 2 * L
        t = np.arange(L, dtype=np.float64)
        f = np.arange(L, dtype=np.float64)
        ang = 2.0 * np.pi * np.outer(t, f) / N
        fc = np.cos(ang)
        fs = -np.sin(ang)
        fs[:, 0] = (-1.0) ** t
        w = np.full(L, 2.0); w[0] = 1.0
        gc = (w[:, None] / N) * np.cos(ang.T)
        gs = -(2.0 / N) * np.sin(ang.T)
        gs[0, :] = (1.0 / N) * (-1.0) ** t
        return [a.astype(np.float32) for a in (fc, fs, gc, gs)]
    c["c_fc"], c["c_fs"], c["c_gc"], c["c_gs"] = dft(SEQ)
    c["c_fc_c"], c["c_fs_c"], c["c_gc_c"], c["c_gs_c"] = dft(CTX)
    def feats(L):
        pos = np.arange(L, dtype=np.float32)
        t01 = pos / np.float32(L - 1)
        bands = np.linspace(1e-4, 15, 16, dtype=np.float32)
        ang = (np.float32(2.0 * math.pi / L) * pos[:, None] * bands[None, :]).astype(np.float32)
        f = np.concatenate([t01[:, None], np.cos(ang), -np.sin(ang)], axis=-1).astype(np.float32)
        return np.ascontiguousarray(f.T), t01
    c["c_feat"], t01 = feats(SEQ)
    c["c_feat_c"], t01c = feats(CTX)
    c["c_t01"] = np.ascontiguousarray(np.concatenate([t01, t01c]).reshape(18, 128).T)
    c["c_iota"] = np.arange(128, dtype=np.float32).reshape(128, 1)
    _CONST_CACHE.update(c)
    return c


def prep_shared(inputs, nlayers):
    L = nlayers
    f = lambda n: np.ascontiguousarray(np.asarray(inputs[n], dtype=np.float32)[:L])
    s = {}
    s["w_mod"] = f("w_mod")
    s["b_mod2"] = np.ascontiguousarray(np.repeat(f("b_mod")[:, None, :], 2, axis=1))
    s["g_mixT"] = _fm(f("g_mix"), KC)
    s["g_ffnT"] = _fm(f("g_ffn"), KC)
    s["w_in"] = f("w_in")
    s["gqa_q_gain"] = f("gqa_q_gain").reshape(L, 128, 1)
    s["gqa_k_gain"] = f("gqa_k_gain").reshape(L, 128, 1)
    s["mla_q_a_gainT"] = _fm(f("mla_q_a_gain"), 3)
    s["mla_kv_a_gainT"] = _fm(f("mla_kv_a_gain"), 2)
    s["mla_q_b"] = f("mla_q_b")
    s["mla_kv_b"] = f("mla_kv_b")
    s["hy_conv_wT"] = np.ascontiguousarray(np.transpose(f("hy_conv_w").reshape(L, 3, 12, 128), (0, 3, 2, 1)))
    s["hy_conv_bT"] = _fm(f("hy_conv_b"), 12)
    s["hf_w1"] = f("hf_w1")
    s["hf_b1T"] = f("hf_b1").reshape(L, 64, 1)
    s["hf_w2"] = f("hf_w2")
    s["hf_b2T"] = f("hf_b2").reshape(L, 64, 1)
    s["hf_w3"] = f("hf_w3")
    s["hf_freqT"] = f("hf_freq").reshape(L, 64, 1)
    s["hf_log_rate"] = f("hf_log_rate").reshape(L, 1, 2048)
    s["hy_biasT"] = np.ascontiguousarray(np.transpose(f("hy_bias").reshape(L, 2, 4, 128), (0, 3, 1, 2)))
    s["cv_wT"] = np.ascontiguousarray(np.transpose(f("cv_w").reshape(L, 31, 4, 128), (0, 3, 2, 1)))
    s["cv_bT"] = _fm(f("cv_b"), 4)
    s["cv_ln_gT"] = _fm(f("cv_ln_g"), 4)
    s["cv_ln_bT"] = _fm(f("cv_ln_b"), 4)
    for n in ("w_out", "w_router", "w1", "w3", "w2"):
        s[n] = f(n)
    s["w_brR"] = np.ascontiguousarray(np.transpose(f("w_br").reshape(L, 4, 4, 128, KC, 128), (0, 4, 3, 1, 2, 5)).reshape(L, KC, 128, 16, 128))
    s["w_gateR"] = np.ascontiguousarray(np.transpose(s["w_in"][:, :, C_G:].reshape(L, D, 4, KC, 128), (0, 1, 3, 2, 4)).reshape(L, D, KC, 512))
    s["g_final"] = np.asarray(inputs["g_final"], np.float32).reshape(1, D)
    s.update(const_tables())
    return s


def prep_core(inputs, b):
    m = {}
    m["xin"] = np.ascontiguousarray(np.concatenate([np.asarray(inputs["x"][b], np.float32), np.asarray(inputs["ctx"][b], np.float32)], axis=0))
    cv = np.stack([np.asarray(inputs["c"][b], np.float32), np.asarray(inputs["c_ctx"], np.float32)], axis=0)
    m["cT"] = np.ascontiguousarray(np.transpose(cv.reshape(2, KC, 128), (2, 1, 0)))
    return m


def rms_fm(P, chunks, N, gains, dim, outs, bout, wk):
    kb, c = P.kb, P.c
    sq, bsq, rs, brs = wk["sq"], wk["bsq"], wk["rs"], wk["brs"]
    ss, bss = P.psf[3]
    n = chunks[0][0].shape[0]
    for ci, (ps, bps) in enumerate(chunks):
        kb.act(sq[:n, ci, :N], ps, AF.Square, [bps], [bsq])
    for ci in range(len(chunks)):
        kb.mm(ss[:, :N], c["ones_b"][:n, :], sq[:n, ci, :N], ci == 0, ci == len(chunks) - 1, [bsq, P.B("k_ones_b")], [bss])
    kb.act(rs[:, :N], ss[:, :N], AF.Ln, [bss], [brs], scale=1.0 / dim, bias=EPS)
    kb.act(rs[:, :N], rs[:, :N], AF.Exp, [brs], [brs], scale=-0.5)
    for ci, (ps, bps) in enumerate(chunks):
        gap, bg = gains[ci]
        kb.stt(outs[ci], ps, gap, rs[:n, :N], ALU.mult, ALU.mult, [bps, bg, brs], [bout])


def rope_fm(P, xn, bxn, n, N, cos, sin, bcs, out, bout, wk):
    kb, c = P.kb, P.c
    rot, brot = P.psf[4]
    R = c["r128"] if n == 128 else c["r32"]
    bR = P.B("k_r128" if n == 128 else "k_r32")
    kb.mm(rot[:n, :N], R[:, :], xn, True, True, [bxn, bR], [brot])
    t1, bt1, t2, bt2 = wk["t1"], wk["bt1"], wk["t2"], wk["bt2"]
    kb.tt(t1[:n, :N], xn, cos, ALU.mult, [bxn, bcs], [bt1])
    kb.tt(t2[:n, :N], rot[:n, :N], sin, ALU.mult, [brot, bcs], [bt2])
    kb.tt(out, t1[:n, :N], t2[:n, :N], ALU.add, [bt1, bt2], [bout])


def proj_fm(P, ps, bps, W, bW, col0, ncols, hTg, bh, N):
    for k in range(KC):
        P.kb.mm(ps[:ncols, :N], W[:, k, col0:col0 + ncols], hTg[:, k, :N], k == 0, k == KC - 1, [bW, bh], [bps])


def attn_stage(P, i):
    kb, d, c, nc = P.kb, P.din, P.c, P.nc
    kb.barrier()
    with contextlib.ExitStack() as ph:
        def sb(name, shape, dt):
            return ph.enter_context(nc.sbuf_tensor(f"a{i}_{name}", list(shape), dt)), Buf(name)
        W, bW = sb("W", [128, KC, 896], BF16)
        kvb, bkvb = sb("kvb", [128, 2, 768], BF16)
        qb, bqb = sb("qb", [128, 3, 384], BF16)
        gq, bgq = sb("gq", [128, 1], F32)
        gk, bgk = sb("gk", [128, 1], F32)
        gqa, bgqa = sb("gqa", [128, 3], F32)
        gkva, bgkva = sb("gkva", [128, 2], F32)
        kT, bkT = sb("kT", [128, 2, NT], BF16)
        vg, bvg = sb("vg", [128, NTILE, 256], BF16)
        kcat, bkcat = sb("kcat", [128, 4, NT], BF16)
        vm, bvm = sb("vm", [128, NTILE, 512], BF16)
        hTg = [sb(f"hTg{j}", [128, KC, 512], BF16) for j in range(2)]
        cs128 = [sb(f"cs128_{j}", [128, 2, 512], F32) for j in range(2)]
        cs32 = [sb(f"cs32_{j}", [32, 2, 512], F32) for j in range(2)]
        wk = {}
        wk["sq"], wk["bsq"] = sb("sq", [128, 3, 512], BF16)
        wk["rs"], wk["brs"] = sb("rs", [128, 512], F32)
        wk["t1"], wk["bt1"] = sb("t1", [128, 512], F32)
        wk["t2"], wk["bt2"] = sb("t2", [128, 512], F32)
        xn, bxn = sb("xn", [128, 3, 512], BF16)
        kp, bkp = sb("kp", [32, 512], BF16)
        qT, bqT = sb("qT", [128, 4, 512], BF16)
        qcat, bqcat = sb("qcat", [128, 4, 512], BF16)
        pT = [sb(f"pT{j}", [128, 512], BF16) for j in range(3)]
        rd, brd = sb("rd", [128, 512], F32)
        dacc = [sb(f"dacc{j}", [128, 512], F32) for j in range(4)]
        oT = [sb(f"oT{j}", [128, 512], BF16) for j in range(2)]

        kb.op("dve", lambda g: g.memset(kcat[:], 0.0), [], [bkcat])
        kb.op("dve", lambda g: g.memset(qcat[:], 0.0), [], [bqcat])
        kb.ld(gq[:], d["gqa_q_gain"][i], [P.B("gqa_q_gain")], [bgq])
        kb.ld(gk[:], d["gqa_k_gain"][i], [P.B("gqa_k_gain")], [bgk])
        kb.ld(gqa[:], d["mla_q_a_gainT"][i], [P.B("mla_q_a_gainT")], [bgqa])
        kb.ld(gkva[:], d["mla_kv_a_gainT"][i], [P.B("mla_kv_a_gainT")], [bgkva])
        kb.ld(kvb[:], d["mla_kv_b"][i].rearrange("(k p) c -> p k c", p=128), [P.B("mla_kv_b")], [bkvb], q="pool")
        kb.ld(qb[:], d["mla_q_b"][i].rearrange("(k p) c -> p k c", p=128), [P.B("mla_q_b")], [bqb], q="pool")
        wsrc = d["w_in"][i].rearrange("(k p) c -> p k c", p=128)
        hsrc = d["hT_d"]

        def load_group(gi):
            t0, N = TGROUPS[gi]
            h, bh = hTg[gi % 2]
            kb.ld(h[:, :, :N], hsrc[:, :, t0:t0 + N], [P.B("hT_d")], [bh])
            a, ba = cs128[gi % 2]
            kb.ld(a[:, 0, :N], d["c_cos128"][:, t0:t0 + N], [P.B("c_cos128")], [ba])
            kb.ld(a[:, 1, :N], d["c_sin128"][:, t0:t0 + N], [P.B("c_sin128")], [ba])
            a2, ba2 = cs32[gi % 2]
            kb.ld(a2[:, 0, :N], d["c_cos32"][:, t0:t0 + N], [P.B("c_cos32")], [ba2])
            kb.ld(a2[:, 1, :N], d["c_sin32"][:, t0:t0 + N], [P.B("c_sin32")], [ba2])
            return h, bh, a, ba, a2, ba2

        for k in range(KC):
            kb.ld(W[:, k, 0:800], wsrc[:, k, 0:800], [P.B("w_in")], [bW], q="pool")
        for gi, (t0, N) in enumerate(TGROUPS):
            h, bh, a, ba, a2, ba2 = load_group(gi)
            for g in range(2):
                ps, bps = P.psf[g]
                proj_fm(P, ps, bps, W, bW, C_K + g * 128, 128, h, bh, N)

            def vtile(tl):
                tt = t0 // 128 + tl
                ps, bps = P.psf[2] if tl % 2 == 0 else P.psf[5]
                for k in range(KC):
                    kb.mm(ps[:, 0:256], h[:, k, tl * 128:(tl + 1) * 128], W[:, k, C_V:C_V + 256], k == 0, k == KC - 1, [bh, bW], [bps])
                kb.act(vg[:, tt, :], ps[:, 0:256], AF.Copy, [bps], [bvg])
            ntl = N // 128
            for g in range(2):
                ps, bps = P.psf[g]
                rms_fm(P, [(ps[:, :N], bps)], N, [(gk[:, 0:1], bgk)], 128, [xn[:, g, :N]], bxn, wk)
                for tl in range(g * ntl // 2, (g + 1) * ntl // 2):
                    vtile(tl)
                rope_fm(P, xn[:, g, :N], bxn, 128, N, a[:, 0, :N], a[:, 1, :N], ba, kT[:, g, t0:t0 + N], bkT, wk)
            chunks = []
            for cc in range(2):
                ps, bps = P.psf[cc]
                proj_fm(P, ps, bps, W, bW, C_KVA + cc * 128, 128, h, bh, N)
                chunks.append((ps[:, :N], bps))
            rms_fm(P, chunks, N, [(gkva[:, cc:cc + 1], bgkva) for cc in range(2)], 256, [xn[:, cc, :N] for cc in range(2)], bxn, wk)
            for hh in range(4):
                ps, bps = P.psf[hh % 2]
                for cc in range(2):
                    kb.mm(ps[:64, :N], kvb[:, cc, hh * 192:hh * 192 + 64], xn[:, cc, :N], cc == 0, cc == 1, [bkvb, bxn], [bps])
                kb.act(kcat[0:64, hh, t0:t0 + N], ps[:64, :N], AF.Copy, [bps], [bkcat])
            for tl in range(N // 128):
                tt = t0 // 128 + tl
                ps, bps = P.psf[tl % 2]
                for hh in range(4):
                    for cc in range(2):
                        kb.mm(ps[:, hh * 128:(hh + 1) * 128], xn[:, cc, tl * 128:(tl + 1) * 128], kvb[:, cc, hh * 192 + 64:hh * 192 + 192], cc == 0, cc == 1, [bkvb, bxn], [bps])
                kb.act(vm[:, tt, :], ps[:, :], AF.Copy, [bps], [bvm])
            ps, bps = P.psf[0]
            proj_fm(P, ps, bps, W, bW, C_KPE, 32, h, bh, N)
            kb.act(kp[:, :N], ps[:32, :N], AF.Copy, [bps], [bkp])
            rope_fm(P, kp[:, :N], bkp, 32, N, a2[:, 0, :N], a2[:, 1, :N], ba2, kcat[64:96, 0, t0:t0 + N], bkcat, wk)
            for hh in range(1, 4):
                kb.cp(kcat[64:96, hh, t0:t0 + N], kcat[64:96, 0, t0:t0 + N], [bkcat], [bkcat])

        for k in range(KC):
            kb.ld(W[:, k, 0:896], wsrc[:, k, C_Q:C_Q + 896], [P.B("w_in")], [bW], q="pool")
        for gi, (t0, N) in enumerate(TGROUPS):
            h, bh, a, ba, a2, ba2 = load_group(gi)
            QB = (0, 1, 2)
            ps, bps = P.psf[QB[0]]
            proj_fm(P, ps, bps, W, bW, 0, 128, h, bh, N)
            for hh in range(4):
                if hh + 1 < 4:
                    psn, bpsn = P.psf[QB[(hh + 1) % 3]]
                    proj_fm(P, psn, bpsn, W, bW, (hh + 1) * 128, 128, h, bh, N)
                ps, bps = P.psf[QB[hh % 3]]
                rms_fm(P, [(ps[:, :N], bps)], N, [(gq[:, 0:1], bgq)], 128, [xn[:, hh % 3, :N]], bxn, wk)
                rope_fm(P, xn[:, hh % 3, :N], bxn, 128, N, a[:, 0, :N], a[:, 1, :N], ba, qT[:, hh, :N], bqT, wk)
            chunks = []
            for cc in range(3):
                ps, bps = P.psf[cc]
                proj_fm(P, ps, bps, W, bW, 512 + cc * 128, 128, h, bh, N)
                chunks.append((ps[:, :N], bps))
            rms_fm(P, chunks, N, [(gqa[:, cc:cc + 1], bgqa) for cc in range(3)], 384, [xn[:, cc, :N] for cc in range(3)], bxn, wk)
            for hh in range(4):
                ps, bps = P.psf[hh % 2]
                for cc in range(3):
                    kb.mm(ps[:64, :N], qb[:, cc, hh * 96:hh * 96 + 64], xn[:, cc, :N], cc == 0, cc == 2, [bqb, bxn], [bps])
                kb.act(qcat[0:64, hh, :N], ps[:64, :N], AF.Copy, [bps], [bqcat])
                ps2, bps2 = P.psf[2]
                for cc in range(3):
                    kb.mm(ps2[:32, :N], qb[:, cc, hh * 96 + 64:hh * 96 + 96], xn[:, cc, :N], cc == 0, cc == 2, [bqb, bxn], [bps2])
                kb.act(kp[:, :N], ps2[:32, :N], AF.Copy, [bps2], [bkp])
                rope_fm(P, kp[:, :N], bkp, 32, N, a2[:, 0, :N], a2[:, 1, :N], ba2, qcat[64:96, hh, :N], bqcat, wk)
            kts = list(range(NTILE)) if gi < 4 else [16, 17]
            for br_i, nh in ((1, 4), (2, 4)):
                for hh in range(nh):
                    o_ps, bo = P.psf[2 + 2 * (hh % 2)]
                    d_ps, bd = P.psf[5]
                    SB = (0, 1, 3)
                    def emit_s(ki):
                        kt = kts[ki]
                        s_ps, bs = P.psf[SB[ki % 3]]
                        if br_i == 1:
                            kb.mm(s_ps[:, :N], kT[:, hh // 2, kt * 128:(kt + 1) * 128], qT[:, hh, :N], True, True, [bkT, bqT], [bs])
                        else:
                            kb.mm(s_ps[:, :N], kcat[:, hh, kt * 128:(kt + 1) * 128], qcat[:, hh, :N], True, True, [bkcat, bqcat], [bs])
                        p_, bp = pT[ki % 3]
                        kb.act(p_[:, :N], s_ps[:, :N], AF.Exp, [bs], [bp], scale=(128.0 ** -0.5 if br_i == 1 else 96.0 ** -0.5))
                    emit_s(0)
                    emit_s(1)
                    for ki, kt in enumerate(kts):
                        if ki + 2 < len(kts):
                            emit_s(ki + 2)
                        if br_i == 1:
                            vv = vg[:, kt, (hh // 2) * 128:(hh // 2 + 1) * 128]
                            bv = bvg
                        else:
                            vv = vm[:, kt, hh * 128:(hh + 1) * 128]
                            bv = bvm
                        p_, bp = pT[ki % 3]
                        kb.mm(o_ps[:, :N], vv, p_[:, :N], ki == 0, ki == len(kts) - 1, [bv, bp], [bo])
                        da, bda = dacc[2 * (hh % 2) + ki % 2]
                        eng = "dve" if ki % 2 == 0 else "pool"
                        if ki < 2:
                            kb.cp(da[:, :N], p_[:, :N], [bp], [bda], e=eng)
                        else:
                            kb.tt(da[:, :N], da[:, :N], p_[:, :N], ALU.add, [bda, bp], [bda], e=eng)
                    for half in range(2):
                        da, bda = dacc[2 * (hh % 2) + half]
                        kb.mm(d_ps[:, :N], c["ones_f"][:, :], da[:, :N], half == 0, half == 1, [P.B("k_ones_f"), bda], [bd])
                    kb.act(rd[:, :N], d_ps[:, :N], AF.Ln, [bd], [brd])
                    kb.act(rd[:, :N], rd[:, :N], AF.Exp, [brd], [brd], scale=-1.0)
                    o_, bo_ = oT[hh % 2]
                    kb.tt(o_[:, :N], o_ps[:, :N], rd[:, :N], ALU.mult, [bo, brd], [bo_])
                    kb.ld(d["br"][br_i, hh * 128:(hh + 1) * 128, t0:t0 + N], o_[:, :N], [bo_], [P.B("br")], q="act")
    kb.barrier()


def norm1_stage(P, i):
    kb, d = P.kb, P.din
    kb.barrier()

    def consume(t0, ntok, ht, bht):
        kb.ld(d["hT_d"][:, :, t0:t0 + ntok], ht[:, :, :ntok], [bht], [P.B("hT_d")], q="act")
    norm_stage(P, i, "g_mixT", 0, 1, consume)
    kb.barrier()


SEGS = [("lat", 0, SEQ, 16, "", 0), ("ctx", SEQ, CTX, 2, "_c", 16)]


def sin3(P, out, arg, n, N, bufs, wk):
    kb = P.kb
    s, bs, s2, bs2 = wk["s"], wk["bs"], wk["s2"], wk["bs2"]
    kb.act(s[:n, :N], arg, AF.Sin, bufs, [bs], scale=1.0 / 3.0)
    kb.tt(s2[:n, :N], s[:n, :N], s[:n, :N], ALU.mult, [bs], [bs2])
    kb.ts(s2[:n, :N], s2[:n, :N], -4.0, ALU.mult, [bs2], [bs2], s2=3.0, op1=ALU.add)
    return kb.tt(out, s[:n, :N], s2[:n, :N], ALU.mult, [bs, bs2], wk["outb"])


def hy_filters(P, i):
    kb, d, c, nc = P.kb, P.din, P.c, P.nc
    kb.barrier()
    with contextlib.ExitStack() as ph:
        def sb(name, shape, dt):
            return ph.enter_context(nc.sbuf_tensor(f"f{i}_{name}", list(shape), dt)), Buf(name)
        w1, bw1 = sb("w1", [33, 64], F32)
        w2, bw2 = sb("w2", [64, 64], F32)
        w3, bw3 = sb("w3", [64, 2048], F32)
        b1, bb1 = sb("b1", [64, 1], F32)
        b2, bb2 = sb("b2", [64, 1], F32)
        fq, bfq = sb("fq", [64, 1], F32)
        rate, brate = sb("rate", [128, 2048], F32)
        t01, bt01 = sb("t01", [128, 18], F32)
        feat, bfeat = sb("feat", [33, SEQ], F32)
        h1, bh1 = sb("h1", [64, SEQ], F32)
        h2, bh2 = sb("h2", [64, SEQ], F32)
        arg, barg = sb("arg", [64, 512], F32)
        wk = {}
        wk["s"], wk["bs"] = sb("s", [64, 512], F32)
        wk["s2"], wk["bs2"] = sb("s2", [64, 512], F32)
        dec, bdec = sb("dec", [128, 2048], F32)
        filt, bfilt = sb("filt", [128, 16, 2048], BF16)
        tab = [sb(f"tab{j}", [128, 16, 128], BF16) for j in range(4)]
        res = [sb(f"res{j}", [128, 2, 2, 512], F32) for j in range(2)]
        tmp, btmp = sb("tmp", [128, 512], F32)
        kb.ld(w1[:], d["hf_w1"][i], [P.B("hf_w1")], [bw1])
        kb.ld(w2[:], d["hf_w2"][i], [P.B("hf_w2")], [bw2])
        kb.ld(w3[:], d["hf_w3"][i], [P.B("hf_w3")], [bw3])
        kb.ld(b1[:], d["hf_b1T"][i], [P.B("hf_b1T")], [bb1])
        kb.ld(b2[:], d["hf_b2T"][i], [P.B("hf_b2T")], [bb2])
        kb.ld(fq[:], d["hf_freqT"][i], [P.B("hf_freqT")], [bfq])
        kb.ld(rate[:], d["hf_log_rate"][i].partition_broadcast(128), [P.B("hf_log_rate")], [brate])
        kb.act(rate[:], rate[:], AF.Exp, [brate], [brate])
        kb.ld(t01[:], d["c_t01"], [P.B("c_t01")], [bt01])
        kb.ts(t01[:], t01[:], -1.0, ALU.mult, [bt01], [bt01])
        for (sname, toff, L, npt, suf, ttoff) in SEGS:
            kb.ld(feat[:, :L], d["c_feat" + suf], [P.B("c_feat" + suf)], [bfeat])
            G = min(L, 512)
            for g0 in range(0, L, G):
                ps, bps = P.psf[0]
                kb.mm(ps[:64, :G], w1[:, :], feat[:, g0:g0 + G], True, True, [bw1, bfeat], [bps])
                kb.ts(arg[:, :G], ps[:64, :G], b1[:, 0:1], ALU.add, [bps, bb1, bfq], [barg], s2=fq[:, 0:1], op1=ALU.mult)
                wk["outb"] = [bh1]
                sin3(P, h1[:, g0:g0 + G], arg[:, :G], 64, G, [barg], wk)
                ps, bps = P.psf[1]
                kb.mm(ps[:64, :G], w2[:, :], h1[:, g0:g0 + G], True, True, [bw2, bh1], [bps])
                kb.ts(arg[:, :G], ps[:64, :G], b2[:, 0:1], ALU.add, [bps, bb2, bfq], [barg], s2=fq[:, 0:1], op1=ALU.mult)
                wk["outb"] = [bh2]
                sin3(P, h2[:, g0:g0 + G], arg[:, :G], 64, G, [barg], wk)
            for pt in range(npt):
                kb.act(dec[:], rate[:], AF.Exp, [brate, bt01], [bdec], scale=t01[:, ttoff + pt:ttoff + pt + 1])
                for cg in range(4):
                    ps, bps = P.psf[cg % 2]
                    kb.mm(ps[:, :], h2[:, pt * 128:(pt + 1) * 128], w3[:, cg * 512:(cg + 1) * 512], True, True, [bh2, bw3], [bps])
                    kb.tt(filt[:, pt, cg * 512:(cg + 1) * 512], ps[:, :], dec[:, cg * 512:(cg + 1) * 512], ALU.mult, [bps, bdec], [bfilt])
            kb.op("dve", lambda g: g.memset(filt[0:1, 0, 512:1024], 0.0), [], [bfilt])
            kb.op("dve", lambda g: g.memset(filt[0:1, 0, 1536:2048], 0.0), [], [bfilt])
            for pt in range(npt):
                for o in range(2):
                    f_ = filt[:, pt, o * 1024:o * 1024 + 512]
                    b_ = filt[:, pt, o * 1024 + 512:o * 1024 + 1024]
                    kb.tt(b_, f_, b_, ALU.subtract, [bfilt], [bfilt])
                    kb.stt(f_, f_, 2.0, b_, ALU.mult, ALU.subtract, [bfilt], [bfilt])
            kf = d["kf" + suf]
            for ft in range(npt):
                r_, br_ = res[ft % 2]
                for cs, tn in enumerate(("c_fc", "c_fs")):
                    tb, btb = tab[2 * (ft % 2) + cs]
                    src = d[tn + suf].rearrange("(pt p) f -> p pt f", p=128)
                    kb.ld(tb[:, :npt, :], src[:, :, ft * 128:(ft + 1) * 128], [P.B(tn + suf)], [btb], q="pool")
                    for o in range(2):
                        pX, bX = P.psf[2 * o + cs]
                        c0 = o * 1024 + cs * 512
                        for pt in range(npt):
                            kb.mm(pX[:, :], tb[:, pt, :], filt[:, pt, c0:c0 + 512], pt == 0, pt == npt - 1, [btb, bfilt], [bX])
                        kb.cp(r_[:, cs, o, :], pX[:, :], [bX], [br_], e="act")
                        if cs == 1 and ft == 0:
                            pN, bN = P.psf[4]
                            for pt in range(npt):
                                kb.mm(pN[0:1, :], tb[:, pt, 0:1], filt[:, pt, o * 1024:o * 1024 + 512], pt == 0, pt == npt - 1, [btb, bfilt], [bN])
                            kb.cp(r_[0:1, 1, o, :], pN[0:1, :], [bN], [br_], e="act")
                kb.ld(kf[0, ft], r_[:, 0], [br_], [P.B("kf" + suf)], q="act")
                kb.ld(kf[1, ft], r_[:, 1], [br_], [P.B("kf" + suf)], q="act")
    kb.barrier()


def hy_stage(P, i):
    kb, d, c, nc = P.kb, P.din, P.c, P.nc
    kb.barrier()
    PADW = NT + 4
    with contextlib.ExitStack() as ph:
        def sb(name, shape, dt):
            return ph.enter_context(nc.sbuf_tensor(f"h{i}_{name}", list(shape), dt)), Buf(name)
        u, bu = sb("u", [128, 12, NT], BF16)
        cw, bcw = sb("cw", [128, 12, 3], F32)
        cb, bcb = sb("cb", [128, 12], F32)
        hb, bhb = sb("hb", [128, 2, 4], F32)
        kb.ld(cw[:], d["hy_conv_wT"][i], [P.B("hy_conv_wT")], [bcw])
        kb.ld(cb[:], d["hy_conv_bT"][i], [P.B("hy_conv_bT")], [bcb])
        kb.ld(hb[:], d["hy_biasT"][i], [P.B("hy_biasT")], [bhb])
        wsrc = d["w_in"][i].rearrange("(k p) c -> p k c", p=128)
        with contextlib.ExitStack() as ph2:
            def sb2(name, shape, dt):
                return ph2.enter_context(nc.sbuf_tensor(f"h{i}_{name}", list(shape), dt)), Buf(name)
            Ws = [sb2(f"W{j}", [128, KC, 512], BF16) for j in range(2)]
            hTg = [sb2(f"hTg{j}", [128, KC, 512], BF16) for j in range(2)]
            praw, bpraw = sb2("praw", [128, 4, PADW], F32)
            acc, bacc = sb2("acc", [128, SEQ], F32)
            kb.op("dve", lambda g: g.memset(praw[:], 0.0), [], [bpraw])
            for part in range(3):
                W, bW = Ws[part % 2]
                for k in range(KC):
                    kb.ld(W[:, k, :], wsrc[:, k, C_HY + part * 512:C_HY + (part + 1) * 512], [P.B("w_in")], [bW], q="pool")
                for gi, (t0, N) in enumerate(TGROUPS):
                    h, bh = hTg[gi % 2]
                    kb.ld(h[:, :, :N], d["hT_d"][:, :, t0:t0 + N], [P.B("hT_d")], [bh])
                    off = 1 + t0 if t0 < SEQ else 3 + t0
                    for cc in range(4):
                        ps, bps = P.psf[cc % 2]
                        proj_fm(P, ps, bps, W, bW, cc * 128, 128, h, bh, N)
                        kb.act(praw[:, cc, off:off + N], ps[:, :N], AF.Copy, [bps], [bpraw])
                for cc in range(4):
                    ch = part * 4 + cc
                    for (toff, L, poff) in ((0, SEQ, 1), (SEQ, CTX, SEQ + 3)):
                        kb.ts(acc[:, :L], praw[:, cc, poff - 1:poff - 1 + L], cw[:, ch, 0:1], ALU.mult, [bpraw, bcw, bcb], [bacc], s2=cb[:, ch:ch + 1], op1=ALU.add)
                        kb.stt(acc[:, :L], praw[:, cc, poff:poff + L], cw[:, ch, 1:2], acc[:, :L], ALU.mult, ALU.add, [bpraw, bcw, bacc], [bacc])
                        kb.stt(u[:, ch, toff:toff + L], praw[:, cc, poff + 1:poff + 1 + L], cw[:, ch, 2:3], acc[:, :L], ALU.mult, ALU.add, [bpraw, bcw, bacc], [bu])
        kb.barrier()
        if "hy_u" in P.dbg:
            P.dump("hy_u", u[:], bu, [128, 12, NT], BF16)
        z, bz = sb("z", [128, 4, SEQ], BF16)
        zt, bzt = sb("zt", [128, 16, 512], BF16)
        Yr, bYr = sb("Yr", [128, 16, 512], BF16)
        Yi, bYi = sb("Yi", [128, 16, 512], BF16)
        tabF = [sb(f"tabF{j}", [128, 16, 128], BF16) for j in range(4)]
        tabG = [sb(f"tabG{j}", [128, 16, 512], BF16) for j in range(2)]
        kfr, bkfr = sb("kfr", [128, 512], F32)
        kfi, bkfi = sb("kfi", [128, 512], F32)
        t1, bt1 = sb("t1", [128, 512], F32)
        t2, bt2 = sb("t2", [128, 512], F32)
        ob, bob = sb("ob", [128, 512], BF16)
        for (sname, toff, L, npt, suf, ttoff) in SEGS:
            kf = d["kf" + suf]
            for n in range(2):
                src_ap = (lambda cc, a, b: u[:, cc, toff + a:toff + b]) if n == 0 else (lambda cc, a, b: z[:, cc, a:b])
                bsrc = bu if n == 0 else bz
                for pt in range(npt):
                    ps, bps = P.psb[pt % 2]
                    for cc in range(4):
                        kb.tr(ps[:, cc * 128:(cc + 1) * 128], src_ap(cc, pt * 128, (pt + 1) * 128), c["ident_b"][:], [bsrc, P.B("k_ident_b")], [bps])
                    kb.cp(zt[:, pt, :], ps[:, 0:512], [bps], [bzt], e="act")
                for ft in range(npt):
                    tf = [tabF[2 * (ft % 2)], tabF[2 * (ft % 2) + 1]]
                    for cs, tn in enumerate(("c_fc", "c_fs")):
                        tb, btb = tf[cs]
                        src = d[tn + suf].rearrange("(pt p) f -> p pt f", p=128)
                        kb.ld(tb[:, :npt, :], src[:, :, ft * 128:(ft + 1) * 128], [P.B(tn + suf)], [btb], q="pool")
                    kb.ld(kfr[:], kf[0, ft, :, n, :], [P.B("kf" + suf)], [bkfr])
                    kb.ld(kfi[:], kf[1, ft, :, n, :], [P.B("kf" + suf)], [bkfi])
                    zr, bzr = P.psf[2 * (ft % 2)]
                    zi, bzi = P.psf[2 * (ft % 2) + 1]
                    for pt in range(npt):
                        kb.mm(zr[:, :], tf[0][0][:, pt, :], zt[:, pt, :], pt == 0, pt == npt - 1, [tf[0][1], bzt], [bzr])
                    for pt in range(npt):
                        kb.mm(zi[:, :], tf[1][0][:, pt, :], zt[:, pt, :], pt == 0, pt == npt - 1, [tf[1][1], bzt], [bzi])
                    kb.tt(t1[:], zr[:, :], kfi[:], ALU.mult, [bzr, bkfi], [bt1])
                    kb.tt(t2[:], zi[:, :], kfr[:], ALU.mult, [bzi, bkfr], [bt2])
                    kb.tt(Yi[:, ft, :], t1[:], t2[:], ALU.add, [bt1, bt2], [bYi])
                    kb.tt(t1[:], zr[:, :], kfr[:], ALU.mult, [bzr, bkfr], [bt1])
                    kb.tt(t2[:], zi[:, :], kfi[:], ALU.mult, [bzi, bkfi], [bt2])
                    kb.tt(Yr[:, ft, :], t1[:], t2[:], ALU.subtract, [bt1, bt2], [bYr])
                    if ft == 0:
                        kb.cp(Yr[0:1, 0, :], t1[0:1, :], [bt1], [bYr])
                        kb.cp(Yi[0:1, 0, :], t2[0:1, :], [bt2], [bYi])
                G = min(L, 512)
                for gidx, g0 in enumerate(range(0, L, G)):
                    for cs, tn in enumerate(("c_gc", "c_gs")):
                        tb, btb = tabG[cs]
                        src = d[tn + suf].rearrange("(ft p) t -> p ft t", p=128)
                        kb.ld(tb[:, :npt, :G], src[:, :, g0:g0 + G], [P.B(tn + suf)], [btb], q="pool")
                    for cc in range(4):
                        ps, bps = P.psf[2 + cc]
                        for ft in range(npt):
                            kb.mm(ps[:, :G], Yr[:, ft, cc * 128:(cc + 1) * 128], tabG[0][0][:, ft, :G], ft == 0, False, [bYr, tabG[0][1]], [bps])
                    for cc in range(4):
                        ps, bps = P.psf[2 + cc]
                        for ft in range(npt):
                            kb.mm(ps[:, :G], Yi[:, ft, cc * 128:(cc + 1) * 128], tabG[1][0][:, ft, :G], False, ft == npt - 1, [bYi, tabG[1][1]], [bps])
                    for cc in range(4):
                        ps, bps = P.psf[2 + cc]
                        kb.stt(t1[:, :G], src_ap(cc, g0, g0 + G), hb[:, n, cc:cc + 1], ps[:, :G], ALU.mult, ALU.add, [bsrc, bhb, bps], [bt1])
                        gate = u[:, 4 * (n + 1) + cc, toff + g0:toff + g0 + G]
                        if n == 0:
                            kb.tt(z[:, cc, g0:g0 + G], t1[:, :G], gate, ALU.mult, [bt1, bu], [bz])
                        else:
                            kb.tt(ob[:, :G], t1[:, :G], gate, ALU.mult, [bt1, bu], [bob])
                            kb.ld(d["br"][0, cc * 128:(cc + 1) * 128, toff + g0:toff + g0 + G], ob[:, :G], [bob], [P.B("br")], q="act")
                    if n == 0:
                        pass
                if n == 0:
                    pass
    kb.barrier()


def conf_stage(P, i):
    kb, d, c, nc = P.kb, P.din, P.c, P.nc
    kb.barrier()
    LOFF, COFF, TOT = 15, SEQ + 45, SEQ + 45 + CTX + 15
    with contextlib.ExitStack() as ph:
        def sb(name, shape, dt):
            return ph.enter_context(nc.sbuf_tensor(f"c{i}_{name}", list(shape), dt)), Buf(name)
        W, bW = sb("W", [128, KC, 1024], BF16)
        hTg = [sb(f"hTg{j}", [128, KC, 512], BF16) for j in range(2)]
        glu, bglu = sb("glu", [128, 4, TOT], BF16)
        dgm, bdgm = sb("dgm", [128, 4, 31, 128], BF16)
        uu, buu = sb("uu", [128, 4, NT], F32)
        buus = [Buf(f"uu{j}") for j in range(4)]
        cw, bcw = sb("cw", [128, 4, 31], F32)
        cb, bcb = sb("cb", [128, 4], F32)
        lg, blg = sb("lg", [128, 4], F32)
        lb, blb = sb("lb", [128, 4], F32)
        sg, bsg = sb("sg", [128, 512], F32)
        usq, busq = sb("usq", [128, 4, 512], F32)
        mean, bmean = sb("mean", [128, 512], F32)
        var, bvar = sb("var", [128, 512], F32)
        y, by = sb("y", [128, 512], F32)
        ob, bob = sb("ob", [128, 512], BF16)
        kb.ld(cw[:], d["cv_wT"][i], [P.B("cv_wT")], [bcw])
        kb.ld(cb[:], d["cv_bT"][i], [P.B("cv_bT")], [bcb])
        kb.ld(lg[:], d["cv_ln_gT"][i], [P.B("cv_ln_gT")], [blg])
        kb.ld(lb[:], d["cv_ln_bT"][i], [P.B("cv_ln_bT")], [blb])
        wsrc = d["w_in"][i].rearrange("(k p) c -> p k c", p=128)
        for k in range(KC):
            kb.ld(W[:, k, :], wsrc[:, k, C_CV:C_CV + 1024], [P.B("w_in")], [bW], q="pool")
        kb.op("dve", lambda g: g.memset(glu[:], 0.0), [], [bglu])
        for gi, (t0, N) in enumerate(TGROUPS):
            h, bh = hTg[gi % 2]
            kb.ld(h[:, :, :N], d["hT_d"][:, :, t0:t0 + N], [P.B("hT_d")], [bh])
            off = LOFF + t0 if t0 < SEQ else COFF
            for cc in range(4):
                pa, bpa = P.psf[0]
                pb, bpb = P.psf[1]
                proj_fm(P, pa, bpa, W, bW, cc * 128, 128, h, bh, N)
                proj_fm(P, pb, bpb, W, bW, 512 + cc * 128, 128, h, bh, N)
                kb.act(sg[:, :N], pb[:, :N], AF.Sigmoid, [bpb], [bsg])
                kb.tt(glu[:, cc, off:off + N], pa[:, :N], sg[:, :N], ALU.mult, [bpa, bsg], [bglu])
        for cc in range(4):
            for j in range(31):
                kb.ts(dgm[:, cc, j, :], c["ident_b"][:], cw[:, cc, j:j + 1], ALU.mult, [P.B("k_ident_b"), bcw], [bdgm])
        it = 0
        for cc in range(4):
            for (toff, L, poff) in ((0, SEQ, LOFF), (SEQ, CTX, COFF)):
                G = min(L, 512)
                for g0 in range(0, L, G):
                    ps, bps = P.psf[2 + it % 4]
                    it += 1
                    for j in range(31):
                        a0 = poff - 15 + j + g0
                        kb.mm(ps[:, :G], dgm[:, cc, j, :], glu[:, cc, a0:a0 + G], j == 0, j == 30, [bdgm, bglu], [bps])
                    kb.act(uu[:, cc, toff + g0:toff + g0 + G], ps[:, :G], AF.Identity, [bps, bcb], [buus[cc]], bias=cb[:, cc:cc + 1])
        for gi, (t0, N) in enumerate(TGROUPS):
            s_ps, bs = P.psf[0]
            q_ps, bq = P.psf[1]
            for cc in range(4):
                kb.act(usq[:, cc, :N], uu[:, cc, t0:t0 + N], AF.Square, [buus[cc]], [busq])
            for cc in range(4):
                kb.mm(s_ps[:, :N], c["ones_f"][:, :], uu[:, cc, t0:t0 + N], cc == 0, cc == 3, [P.B("k_ones_f"), buus[cc]], [bs])
            for cc in range(4):
                kb.mm(q_ps[:, :N], c["ones_f"][:, :], usq[:, cc, :N], cc == 0, cc == 3, [P.B("k_ones_f"), busq], [bq])
            kb.ts(mean[:, :N], s_ps[:, :N], 1.0 / 512, ALU.mult, [bs], [bmean])
            kb.tt(var[:, :N], mean[:, :N], mean[:, :N], ALU.mult, [bmean], [bvar])
            kb.stt(var[:, :N], q_ps[:, :N], 1.0 / 512, var[:, :N], ALU.mult, ALU.subtract, [bq, bvar], [bvar])
            kb.act(var[:, :N], var[:, :N], AF.Ln, [bvar], [bvar], bias=EPS)
            kb.act(var[:, :N], var[:, :N], AF.Exp, [bvar], [bvar], scale=-0.5)
            for cc in range(4):
                kb.tt(y[:, :N], uu[:, cc, t0:t0 + N], mean[:, :N], ALU.subtract, [buus[cc], bmean], [by])
                kb.tt(y[:, :N], y[:, :N], var[:, :N], ALU.mult, [by, bvar], [by])
                kb.act(ob[:, :N], y[:, :N], AF.Silu, [by, blg, blb], [bob], scale=lg[:, cc:cc + 1], bias=lb[:, cc:cc + 1])
                kb.ld(d["br"][3, cc * 128:(cc + 1) * 128, t0:t0 + N], ob[:, :N], [bob], [P.B("br")], q="act")
    kb.barrier()


def merge_stage(P, i):
    kb, d, c, nc = P.kb, P.din, P.c, P.nc
    kb.barrier()
    with contextlib.ExitStack() as ph:
        def sb(name, shape, dt):
            return ph.enter_context(nc.sbuf_tensor(f"g{i}_{name}", list(shape), dt)), Buf(name)
        hT, bh = sb("hT", [128, KC, NT], BF16)
        brs, bbr = sb("brs", [128, 16, NT], BF16)
        Wg = [sb(f"Wg{j}", [128, KC, 512], BF16) for j in range(2)]
        wbr = [sb(f"wbr{j}", [128, 16, 128], BF16) for j in range(2)]
        sgt = [sb(f"sgt{j}", [128, 512], F32) for j in range(2)]
        macc, bmacc = sb("macc", [128, 512], F32)
        tmp, btmp = sb("tmp", [128, 512], F32)
        mTk = [sb(f"mTk{j}", [128, 512], BF16) for j in range(2)]
        bhg = [Buf(f"hTg{g}") for g in range(len(TGROUPS))]
        bbg = [Buf(f"brg{g}") for g in range(len(TGROUPS))]
        for gi, (t0, N) in enumerate(TGROUPS):
            kb.ld(hT[:, :, t0:t0 + N], d["hT_d"][:, :, t0:t0 + N], [P.B("hT_d")], [bhg[gi]])
            for n in range(4):
                kb.ld(brs[:, n * 4:(n + 1) * 4, t0:t0 + N], d["br"][n, :, t0:t0 + N].rearrange("(wc p) t -> p wc t", p=128), [P.B("br")], [bbg[gi]])
        wgsrc = d["w_gateR"][i].rearrange("(kc p) k c -> p kc k c", p=128)
        it = 0
        for k in range(KC):
            W_, bW_ = Wg[k % 2]
            wb_, bwb_ = wbr[k % 2]
            kb.ld(W_[:], wgsrc[:, :, k, :], [P.B("w_gateR")], [bW_], q="pool")
            kb.ld(wb_[:], d["w_brR"][i, k], [P.B("w_brR")], [bwb_], q="pool")
            for gi, (t0, N) in enumerate(TGROUPS):
                m_, bm_ = mTk[it % 2]
                it += 1
                for n in range(4):
                    pg, bpg = P.psf[n % 2]
                    up, bup = P.psf[2 + n % 2]
                    sg_, bsg_ = sgt[n % 2]
                    for kc in range(KC):
                        kb.mm(pg[:, :N], W_[:, kc, n * 128:(n + 1) * 128], hT[:, kc, t0:t0 + N], kc == 0, kc == KC - 1, [bW_, bhg[gi]], [bpg])
                    for wc in range(4):
                        kb.mm(up[:, :N], wb_[:, n * 4 + wc, :], brs[:, n * 4 + wc, t0:t0 + N], wc == 0, wc == 3, [bwb_, bbg[gi]], [bup])
                    kb.act(sg_[:, :N], pg[:, :N], AF.Sigmoid, [bpg], [bsg_])
                    if n == 0:
                        kb.tt(macc[:, :N], sg_[:, :N], up[:, :N], ALU.mult, [bsg_, bup], [bmacc])
                    else:
                        kb.tt(tmp[:, :N], sg_[:, :N], up[:, :N], ALU.mult, [bsg_, bup], [btmp])
                        if n < 3:
                            kb.tt(macc[:, :N], macc[:, :N], tmp[:, :N], ALU.add, [bmacc, btmp], [bmacc])
                        else:
                            kb.tt(m_[:, :N], macc[:, :N], tmp[:, :N], ALU.add, [bmacc, btmp], [bm_])
                kb.ld(d["mT_d"][:, k, t0:t0 + N], m_[:, :N], [bm_], [P.B("mT_d")], q="act")
    kb.barrier()
    with contextlib.ExitStack() as ph:
        def sb(name, shape, dt):
            return ph.enter_context(nc.sbuf_tensor(f"o{i}_{name}", list(shape), dt)), Buf(name)
        wo, bwo = sb("wo", [128, KC, D], BF16)
        g1bc, bg1 = sb("g1bc", [128, 2, D], F32)
        mTg = [sb(f"mTg{j}", [128, KC, 512], BF16) for j in range(2)]
        xt = [sb(f"xt{j}", [128, D], F32) for j in range(2)]
        xo = [sb(f"xo{j}", [128, D], F32) for j in range(2)]
        wosrc = d["w_out"][i].rearrange("(kc p) c -> p kc c", p=128)
        for dg in range(4):
            kb.ld(wo[:, :, dg * 512:(dg + 1) * 512], wosrc[:, :, dg * 512:(dg + 1) * 512], [P.B("w_out")], [bwo], q="pool")
        for j in range(2):
            kb.ld(g1bc[:, j, :], d["modrow"][j:j + 1, 2 * D:3 * D].partition_broadcast(128), [P.B("modrow")], [bg1])
        for gi, (t0, N) in enumerate(TGROUPS):
            j = 0 if t0 < SEQ else 1
            m_, bm_ = mTg[gi % 2]
            kb.ld(m_[:, :, :N], d["mT_d"][:, :, t0:t0 + N], [P.B("mT_d")], [bm_])
            for tl in range(N // 128):
                r0 = t0 + tl * 128
                x_, bx_ = xt[tl % 2]
                o_, bo_ = xo[tl % 2]
                kb.ld(x_[:], d["xres"][r0:r0 + 128, :], [P.B("xres")], [bx_])
                for dg in range(4):
                    ps, bps = P.psf[dg % 4]
                    for k in range(KC):
                        kb.mm(ps[:, :], m_[:, k, tl * 128:(tl + 1) * 128], wo[:, k, dg * 512:(dg + 1) * 512], k == 0, k == KC - 1, [bm_, bwo], [bps])
                    kb.tt(o_[:, dg * 512:(dg + 1) * 512], ps[:, :], g1bc[:, j, dg * 512:(dg + 1) * 512], ALU.mult, [bps, bg1], [bo_])
                kb.tt(o_[:], o_[:], x_[:], ALU.add, [bo_, bx_], [bo_])
                kb.ld(d["xres"][r0:r0 + 128, :], o_[:], [bo_], [P.B("xres")], q="act")
    kb.barrier()


def moe_stage(P, i):
    kb, d, c, nc = P.kb, P.din, P.c, P.nc
    kb.barrier()
    NS = 288
    with contextlib.ExitStack() as ph:
        def sb(name, shape, dt):
            return ph.enter_context(nc.sbuf_tensor(f"e{i}_{name}", list(shape), dt)), Buf(name)
        wr, bwr = sb("wr", [128, KC, NE], BF16)
        affT, baffT = sb("affT", [16, NT], F32)
        kb.ld(wr[:], d["w_router"][i].rearrange("(k p) e -> p k e", p=128), [P.B("w_router")], [bwr], q="pool")
        w13 = [sb(f"w13_{j}", [128, KC, 2, 512], BF16) for j in range(2)]
        w2b = [sb(f"w2_{j}", [128, 8, 1024], BF16) for j in range(2)]

        def load13(e, fh):
            w_, bw_ = w13[fh]
            w1src = d["w1"][i, e].rearrange("(k p) f -> p k f", p=128)
            w3src = d["w3"][i, e].rearrange("(k p) f -> p k f", p=128)
            kb.ld(w_[:, :, 0, :], w1src[:, :, fh * 512:(fh + 1) * 512], [P.B("w1")], [bw_], q="pool")
            kb.ld(w_[:, :, 1, :], w3src[:, :, fh * 512:(fh + 1) * 512], [P.B("w3")], [bw_], q="pool")

        def load2(e, dh):
            w_, bw_ = w2b[dh]
            w2src = d["w2"][i, e].rearrange("(k p) c -> p k c", p=128)
            kb.ld(w_[:], w2src[:, :, dh * 1024:(dh + 1) * 1024], [P.B("w2")], [bw_], q="pool")

        load13(0, 0)
        load13(0, 1)
        load2(0, 0)
        load2(0, 1)
        with contextlib.ExitStack() as ph1:
            def sb1(name, shape, dt):
                return ph1.enter_context(nc.sbuf_tensor(f"e{i}_{name}", list(shape), dt)), Buf(name)
            sm = [sb1(f"sm{j}", [128, 4], F32) for j in range(2)]
            ee = [sb1(f"ee{j}", [128, NE], F32) for j in range(2)]

            def consume(t0, ntok, ht, bht):
                for tl in range(ntok // 128):
                    tt = t0 // 128 + tl
                    p = tt % 2
                    lg, blg = P.psf[p]
                    for k in range(KC):
                        kb.mm(lg[:, 0:NE], ht[:, k, tl * 128:(tl + 1) * 128], wr[:, k, :], k == 0, k == KC - 1, [bht, bwr], [blg])
                    s_, bs_ = sm[p]
                    e_, be_ = ee[p]
                    kb.op("dve", lambda g: g.reduce_max(out=s_[:, 0:1], in_=lg[:, 0:NE], axis=AX.X), [blg], [bs_])
                    kb.ts(s_[:, 1:2], s_[:, 0:1], -1.0, ALU.mult, [bs_], [bs_])
                    kb.op("dve", lambda g: g.memset(s_[:, 2:3], 0.0), [], [bs_])
                    kb.act(e_[:], lg[:, 0:NE], AF.Exp, [blg, bs_], [be_, bs_], bias=s_[:, 1:2], accum=s_[:, 2:3])
                    kb.op("dve", lambda g: g.reciprocal(out=s_[:, 3:4], in_=s_[:, 2:3]), [bs_], [bs_])
                    kb.ts(e_[:], e_[:], s_[:, 3:4], ALU.mult, [be_, bs_], [be_])
                    tp, btp = P.psf[2 + p]
                    kb.tr(tp[0:16, 0:128], e_[:, :], c["ident_f"][:, :], [be_, P.B("k_ident_f")], [btp])
                    kb.cp(affT[:, tt * 128:(tt + 1) * 128], tp[0:16, 0:128], [btp], [baffT], e="act")
            norm_stage(P, i, "g_ffnT", 2, 3, consume, xs_dram="xs2")
        kb.barrier()
        if "aff" in P.dbg:
            P.dump(f"aff{i}", affT[:], baffT, [16, NT])
        wa, bwa = sb("wa", [16, SEQ], F32)
        wb, bwb = sb("wb", [16, SEQ], F32)
        vals, bvals = sb("vals", [16, NS], F32)
        idxu, bidxu = sb("idxu", [16, NS], U32)
        idxf, bidxf = sb("idxf", [16, NS], F32)
        gT, bgT = sb("gT", [128, 3, NE], F32)
        idxT, bidxT = sb("idxT", [128, 3, NE], I32)
        for (toff, L, rounds, soff) in ((0, SEQ, 32, 0), (SEQ, CTX, 4, 256)):
            cur, bcur = affT[:, toff:toff + L], baffT
            for r in range(rounds):
                v8 = vals[:, soff + r * 8:soff + (r + 1) * 8]
                kb.op("dve", lambda g: g.max(out=v8, in_=cur), [bcur], [bvals])
                kb.op("dve", lambda g: g.max_index(out=idxu[:, soff + r * 8:soff + (r + 1) * 8], in_max=v8, in_values=cur), [bcur, bvals], [bidxu])
                nxt, bnxt = (wa, bwa) if r % 2 == 0 else (wb, bwb)
                if r < rounds - 1:
                    kb.op("dve", lambda g: g.match_replace(out=nxt[:, :L], in_to_replace=v8, in_values=cur, imm_value=-1.0), [bcur, bvals], [bnxt])
                    cur, bcur = nxt[:, :L], bnxt
        kb.cp(idxf[:], idxu[:], [bidxu], [bidxf])
        kb.ts(idxf[:, 256:NS], idxf[:, 256:NS], float(SEQ), ALU.add, [bidxf], [bidxf])
        kb.ts(idxf[:], idxf[:], 0.0, ALU.max, [bidxf], [bidxf], s2=float(NT - 1), op1=ALU.min)
        for ct, (c0, n) in enumerate(((0, 128), (128, 128), (256, 32))):
            tp, btp = P.psf[ct % 2]
            kb.tr(tp[0:n, 0:16], vals[:, c0:c0 + n], c["ident_f"][0:16, 0:16], [bvals, P.B("k_ident_f")], [btp])
            kb.cp(gT[0:n, ct, :], tp[0:n, 0:16], [btp], [bgT])
            tp2, btp2 = P.psf[2 + ct % 2]
            kb.tr(tp2[0:n, 0:16], idxf[:, c0:c0 + n], c["ident_f"][0:16, 0:16], [bidxf, P.B("k_ident_f")], [btp2])
            kb.cp(idxT[0:n, ct, :], tp2[0:n, 0:16], [btp2], [bidxT])
        if "idx" in P.dbg:
            P.dump(f"idx{i}", idxf[:], bidxf, [16, NS])
            P.dump(f"vals{i}", vals[:], bvals, [16, NS])
            P.dump(f"idxT{i}", idxT[:], bidxT, [128, 3, NE], I32)
        g2bc, bg2 = sb("g2bc", [128, 2, D], F32)
        for j in range(2):
            kb.ld(g2bc[:, j, :], d["modrow"][j:j + 1, 5 * D:6 * D].partition_broadcast(128), [P.B("modrow")], [bg2])
        xg = [sb(f"xg{j}", [128, 3, D], BF16) for j in range(1)]
        xgT, bxgT = sb("xgT", [128, KC, NS], BF16)
        hidT, bhid = sb("hidT", [128, 8, NS], BF16)
        sl, bsl = sb("sl", [128, NS], F32)
        yo, byo = sb("yo", [128, 3, D], F32)
        A, bA, modT, bm = c["A"], P.B("k_A"), c["modT"], P.B("k_modT")
        CT = ((0, 128, 0), (128, 128, 0), (256, 32, 1))
        for q in range(4):
            kb.ld(d[f"macc{q}"], d["xres"][:, q * 512:(q + 1) * 512], [P.B("xres")], [P.B(f"macc{q}")])
        x_, bx_ = xg[0]

        def gather(e):
            for ct, (c0, n, j) in enumerate(CT):
                kb.dma("pool", lambda g, ct=ct, n=n: g.indirect_dma_start(
                    out=x_[0:n, ct, :], out_offset=None, in_=d["xs2"][:, :],
                    in_offset=bass.IndirectOffsetOnAxis(ap=idxT[0:n, ct, e:e + 1], axis=0)), [P.B("xs2"), bidxT], [bx_])

        gather(0)
        for e in range(NE):
            for ct, (c0, n, j) in enumerate(CT):
                for k in range(KC):
                    ps, bps = P.psb[k % 2]
                    sl_ = ps[:, (k // 2 % 8) * 128:(k // 2 % 8) * 128 + n]
                    kb.tr(sl_, x_[0:n, ct, k * 128:(k + 1) * 128], c["ident_b"][0:n, 0:n], [bx_, P.B("k_ident_b")], [bps])
                    kb.act(xgT[:, k, c0:c0 + n], sl_, AF.Identity, [bps, bA, bm], [bxgT], scale=A[:, j, k:k + 1], bias=modT[:, 2, k, j:j + 1])
            if e + 1 < NE:
                gather(e + 1)
            for fh in range(2):
                w_, bw_ = w13[fh]
                for fi in range(4):
                    h1, bh1 = P.psf[fi % 2]
                    h3, bh3 = P.psf[2 + fi % 2]
                    for k in range(KC):
                        kb.mm(h1[:, :NS], w_[:, k, 0, fi * 128:(fi + 1) * 128], xgT[:, k, :], k == 0, k == KC - 1, [bw_, bxgT], [bh1])
                    for k in range(KC):
                        kb.mm(h3[:, :NS], w_[:, k, 1, fi * 128:(fi + 1) * 128], xgT[:, k, :], k == 0, k == KC - 1, [bw_, bxgT], [bh3])
                    kb.act(sl[:], h1[:, :NS], AF.Silu, [bh1], [bsl])
                    kb.tt(hidT[:, fh * 4 + fi, :], sl[:], h3[:, :NS], ALU.mult, [bsl, bh3], [bhid])
                if e + 1 < NE:
                    load13(e + 1, fh)
            for dh in range(2):
                w_, bw_ = w2b[dh]
                for ct, (c0, n, j) in enumerate(CT):
                    for dgi in range(2):
                        y, by = P.psf[4 + dgi]
                        for f in range(8):
                            kb.mm(y[0:n, :], hidT[:, f, c0:c0 + n], w_[:, f, dgi * 512:(dgi + 1) * 512], f == 0, f == 7, [bhid, bw_], [by])
                        col = dh * 1024 + dgi * 512
                        kb.stt(yo[0:n, ct, col:col + 512], y[0:n, :], gT[0:n, ct, e:e + 1], g2bc[0:n, j, col:col + 512], ALU.mult, ALU.mult, [by, bgT, bg2], [byo])
                if e + 1 < NE:
                    load2(e + 1, dh)
            for ct, (c0, n, j) in enumerate(CT):
                for q in range(4):
                    kb.dma("pool", lambda g, ct=ct, n=n, q=q: g.indirect_dma_start(
                        out=d[f"macc{q}"][:, :], out_offset=bass.IndirectOffsetOnAxis(ap=idxT[0:n, ct, e:e + 1], axis=0),
                        in_=yo[0:n, ct, q * 512:(q + 1) * 512], in_offset=None, compute_op=ALU.add), [byo, bidxT], [P.B(f"macc{q}")])
        for q in range(4):
            kb.ld(d["xres"][:, q * 512:(q + 1) * 512], d[f"macc{q}"], [P.B(f"macc{q}")], [P.B("xres")])
    kb.barrier()


def final_stage(P):
    kb, d, c, nc = P.kb, P.din, P.c, P.nc
    kb.barrier()
    with contextlib.ExitStack() as ph:
        def sb(name, shape, dt):
            return ph.enter_context(nc.sbuf_tensor(f"z_{name}", list(shape), dt)), Buf(name)
        gbc, bg = sb("gbc", [128, D], F32)
        kb.ld(gbc[:], d["g_final"][0:1, :].partition_broadcast(128), [P.B("g_final")], [bg])
        xt = [sb(f"xt{j}", [128, D], F32) for j in range(2)]
        ot = [sb(f"ot{j}", [128, D], F32) for j in range(2)]
        junk, bj = sb("junk", [128, D], BF16)
        st = [sb(f"st{j}", [128, 4], F32) for j in range(2)]
        for tt in range(SEQ // 128):
            p = tt % 2
            x_, bx_ = xt[p]
            o_, bo_ = ot[p]
            s_, bs_ = st[p]
            kb.ld(x_[:], d["xres"][tt * 128:(tt + 1) * 128, :], [P.B("xres")], [bx_])
            kb.op("dve", lambda g: g.memset(s_[:], 0.0), [], [bs_])
            kb.act(junk[:], x_[:], AF.Square, [bx_, bs_], [bj, bs_], accum=s_[:, 0:1])
            kb.act(s_[:, 1:2], s_[:, 0:1], AF.Sqrt, [bs_], [bs_], scale=1.0 / D, bias=EPS)
            kb.op("dve", lambda g: g.reciprocal(out=s_[:, 2:3], in_=s_[:, 1:2]), [bs_], [bs_])
            kb.stt(o_[:], x_[:], s_[:, 2:3], gbc[:], ALU.mult, ALU.mult, [bx_, bs_, bg], [bo_])
            kb.ld(d["out"][tt * 128:(tt + 1) * 128, :], o_[:], [bo_], [P.B("out")], q="act")
    kb.barrier()


def build_program(nlayers=DEPTH, dbg=None, upto=None):
    P = Prog(nlayers=nlayers, dbg=dbg)
    declare_io(P)
    load_consts(P)
    for i in range(nlayers):
        stage_mod(P, i)
        norm1_stage(P, i)
        attn_stage(P, i)
        hy_filters(P, i)
        hy_stage(P, i)
        conf_stage(P, i)
        if upto == "mixers":
            break
        merge_stage(P, i)
        if P.dbg.get("xmid") and i == 0:
            o = P.outp("dbg_xmid", [NT, D])
            P.kb.ld(o, P.din["xres"], [P.B("xres")], [P.B("dbg_xmid")])
        if upto == "merge":
            break
        moe_stage(P, i)
        if P.dbg.get("xmid") and i == 0:
            o = P.outp("dbg_xl0", [NT, D])
            P.kb.ld(o, P.din["xres"], [P.B("xres")], [P.B("dbg_xl0")])
    final_stage(P)
    return P


_PROG = {}


def kernel(**inputs):
    if "p" not in _PROG:
        _PROG["p"] = build_program(DEPTH)
    P = _PROG["p"]
    shared = prep_shared(inputs, DEPTH)
    in_maps = []
    for core in range(8):
        m = dict(shared)
        m.update(prep_core(inputs, core % 4))
        in_maps.append({k: v for k, v in m.items() if k in P.din})
    res = run_bass_kernel_spmd(P.nc, in_maps, core_ids=list(range(8)))
    out = np.stack([np.asarray(res.results[b]["out"], dtype=np.float32) for b in range(4)], axis=0)
    return out
```
